# Optimizing a Trainium2 kernel written in Bass

```python
import math
import jax, jax.numpy as jnp
from jax import lax
import numpy as np


D_MODEL = 1024
BATCH = 8
SEQ = 2048
DEPTH = 1

DELTA_HEAD_DIM = 128
DELTA_WIDTH = D_MODEL // 2
N_DELTA_HEADS = DELTA_WIDTH // DELTA_HEAD_DIM
SHORT_CONV = 5
CHUNK = 64
POOL_WINDOWS = (2, 4, 8, 16)
N_POOL_GROUPS = len(POOL_WINDOWS)
POOL_WIDTH = D_MODEL - DELTA_WIDTH
POOL_GROUP_DIM = POOL_WIDTH // N_POOL_GROUPS
MIX_WIDTH = DELTA_WIDTH + POOL_WIDTH
IN_COLS = 4 * DELTA_WIDTH + 4 * N_DELTA_HEADS + POOL_WIDTH
N_EXPERTS = 16
EC_CAPACITY = 2
EXPERT_FF = 2 * D_MODEL
RMS_EPS = 1e-6

kernel_name = 'hybrid_deltanet_pool_ecmoe_encoder'


def _rmsnorm(x, w):
    xf = x.astype(jnp.float32)
    y = xf * lax.rsqrt(jnp.mean(xf * xf, axis=-1, keepdims=True) + RMS_EPS)
    return (y * w.astype(jnp.float32)).astype(x.dtype)


def _l2norm(x):
    xf = x.astype(jnp.float32)
    return xf * lax.rsqrt(jnp.sum(xf * xf, axis=-1, keepdims=True) + RMS_EPS)


def _centred_dwconv(x, w):
    K = w.shape[0]
    return lax.conv_general_dilated(
        x, w[:, None, :].astype(x.dtype), window_strides=(1,),
        padding=[(K // 2, K // 2)], dimension_numbers=('NWC', 'WIO', 'NWC'),
        feature_group_count=x.shape[-1])


def _chunk_gated_delta_rule(q, k, v, g, beta):
    out_dtype = v.dtype
    q, k, v, g, beta = (a.astype(jnp.float32) for a in (q, k, v, g, beta))
    B, H, T, Dk = q.shape
    Dv = v.shape[-1]
    N = T // CHUNK
    q = q.reshape(B, H, N, CHUNK, Dk)
    k = k.reshape(B, H, N, CHUNK, Dk)
    v = v.reshape(B, H, N, CHUNK, Dv)
    beta = beta.reshape(B, H, N, CHUNK)
    g = jnp.cumsum(g.reshape(B, H, N, CHUNK), axis=-1)
    incl = jnp.tril(jnp.ones((CHUNK, CHUNK), dtype=bool))
    strict = jnp.tril(jnp.ones((CHUNK, CHUNK), dtype=bool), -1)
    diff = g[..., :, None] - g[..., None, :]
    decay = jnp.where(incl, jnp.exp(jnp.where(incl, diff, 0.0)), 0.0)
    kk = jnp.einsum('bhncd,bhnsd->bhncs', k, k)
    a_mat = jnp.where(strict, beta[..., :, None] * kk * decay, 0.0) + jnp.eye(CHUNK, dtype=jnp.float32)
    rhs = jnp.concatenate([v * beta[..., None], k * (beta * jnp.exp(g))[..., None]], axis=-1)
    sol = lax.linalg.triangular_solve(a_mat, rhs, left_side=True, lower=True, unit_diagonal=True)
    u, w = sol[..., :Dv], sol[..., Dv:]
    qk = jnp.where(incl, jnp.einsum('bhncd,bhnsd->bhncs', q, k) * decay, 0.0)
    q_dec = q * jnp.exp(g)[..., None]
    g_last = g[..., -1]
    k_dec = k * jnp.exp(g_last[..., None] - g)[..., None]

    def step(S, xs):
        q_c, k_c, u_c, w_c, qk_c, gl_c = xs
        v_new = u_c - jnp.einsum('bhcd,bhde->bhce', w_c, S)
        o_c = jnp.einsum('bhcd,bhde->bhce', q_c, S) + jnp.einsum('bhcs,bhse->bhce', qk_c, v_new)
        S = S * jnp.exp(gl_c)[..., None, None] + jnp.einsum('bhcd,bhce->bhde', k_c, v_new)
        return S, o_c

    xs = tuple(jnp.moveaxis(a, 2, 0) for a in (q_dec, k_dec, u, w, qk, g_last))
    _, o = lax.scan(step, jnp.zeros((B, H, Dk, Dv), jnp.float32), xs)
    return jnp.moveaxis(o, 0, 2).reshape(B, H, T, Dv).astype(out_dtype)


def _log_decay(a, a_log, dt_bias):
    gl = -jnp.exp(a_log.astype(jnp.float32)) * jax.nn.softplus(a.astype(jnp.float32) + dt_bias.astype(jnp.float32))
    return jnp.swapaxes(gl, 1, 2)


def _centred_mean_pool(u, window):
    B, T, C = u.shape
    lo = window // 2
    hi = window - lo - 1
    csum = jnp.concatenate([jnp.zeros((B, 1, C), jnp.float32), jnp.cumsum(u.astype(jnp.float32), axis=1)], axis=1)
    t = jnp.arange(T)
    start = jnp.maximum(t - lo, 0)
    end = jnp.minimum(t + hi + 1, T)
    total = csum[:, end] - csum[:, start]
    count = (end - start).astype(jnp.float32)
    return (total / count[None, :, None]).astype(u.dtype)


def _hybrid_mixer(xn, w_in, conv_w, a_log_fwd, dt_bias_fwd, a_log_bwd, dt_bias_bwd,
                  head_norm_w, pool_w, pool_scale, w_out):
    B, T, _ = xn.shape
    H, Dh = N_DELTA_HEADS, DELTA_HEAD_DIM
    proj = jnp.einsum('btd,dc->btc', xn, w_in)
    qkv, z, ab, u = jnp.split(proj, [3 * DELTA_WIDTH, 4 * DELTA_WIDTH, 4 * DELTA_WIDTH + 4 * H], axis=-1)
    qkv = jax.nn.silu(_centred_dwconv(qkv, conv_w))
    q, k, v = jnp.split(qkv, 3, axis=-1)
    to_heads = lambda a: jnp.swapaxes(a.reshape(B, T, H, Dh), 1, 2)
    q = (_l2norm(to_heads(q)) * (Dh ** -0.5)).astype(xn.dtype)
    k = _l2norm(to_heads(k)).astype(xn.dtype)
    v = to_heads(v)
    a_f, b_f, a_b, b_b = jnp.split(ab, 4, axis=-1)
    g_f = _log_decay(a_f, a_log_fwd, dt_bias_fwd)
    g_b = _log_decay(a_b, a_log_bwd, dt_bias_bwd)
    beta_f = jnp.swapaxes(jax.nn.sigmoid(b_f.astype(jnp.float32)), 1, 2)
    beta_b = jnp.swapaxes(jax.nn.sigmoid(b_b.astype(jnp.float32)), 1, 2)
    o_fwd = _chunk_gated_delta_rule(q, k, v, g_f, beta_f)
    rev = lambda a: jnp.flip(a, axis=2)
    o_bwd = rev(_chunk_gated_delta_rule(rev(q), rev(k), rev(v), rev(g_b), rev(beta_b)))
    o = jnp.swapaxes(o_fwd + o_bwd, 1, 2)
    o = _rmsnorm(o, head_norm_w) * jax.nn.silu(z.reshape(B, T, H, Dh))
    o_delta = o.reshape(B, T, DELTA_WIDTH)
    ug = u.reshape(B, T, N_POOL_GROUPS, POOL_GROUP_DIM)
    pooled = jnp.stack([_centred_mean_pool(ug[:, :, i], w) for i, w in enumerate(POOL_WINDOWS)], axis=2)
    o_pool = jnp.einsum('btgc,gcd->btgd', pooled - ug, pool_w).reshape(B, T, POOL_WIDTH) * pool_scale
    return jnp.einsum('btc,cd->btd', jnp.concatenate([o_delta, o_pool], axis=-1), w_out)


def _expert_choice_ffn(xn, router_w, w_gate, w_up, w_down):
    B, T, D = xn.shape
    cap = EC_CAPACITY * T // N_EXPERTS
    probs = jax.nn.softmax(jnp.einsum('btd,de->bte', xn, router_w).astype(jnp.float32), axis=-1)
    gate, idx = lax.top_k(jnp.swapaxes(probs, 1, 2), cap)
    bidx = jnp.arange(B)[:, None, None]
    xg = xn[bidx, idx]
    h = jax.nn.silu(jnp.einsum('becd,edf->becf', xg, w_gate)) * jnp.einsum('becd,edf->becf', xg, w_up)
    y = jnp.einsum('becf,efd->becd', h, w_down) * gate[..., None].astype(xn.dtype)
    return jnp.zeros_like(xn).at[bidx, idx].add(y)


def setup_inputs(seed: int = 0) -> dict:
    key = jax.random.key(seed)
    ks = jax.random.split(key, 20)
    H = N_DELTA_HEADS
    nrm = lambda k, shape, fan_in: jax.random.normal(k, shape, jnp.float32) * (fan_in ** -0.5)
    gain = lambda k, shape: 1.0 + 0.02 * jax.random.normal(k, shape, jnp.float32)
    def dt_bias(k):
        dt = jnp.exp(jax.random.uniform(k, (DEPTH, H), jnp.float32, math.log(1e-3), math.log(1e-1)))
        return dt + jnp.log(-jnp.expm1(-dt))
    return {
        'x': jax.random.normal(ks[0], (BATCH, SEQ, D_MODEL), jnp.float32),
        'norm_mix_w': gain(ks[1], (DEPTH, D_MODEL)),
        'w_in': nrm(ks[2], (DEPTH, D_MODEL, IN_COLS), D_MODEL),
        'conv_w': nrm(ks[3], (DEPTH, SHORT_CONV, 3 * DELTA_WIDTH), SHORT_CONV),
        'a_log_fwd': jnp.log(jax.random.uniform(ks[4], (DEPTH, H), jnp.float32, 1.0, 16.0)),
        'dt_bias_fwd': dt_bias(ks[5]),
        'a_log_bwd': jnp.log(jax.random.uniform(ks[6], (DEPTH, H), jnp.float32, 1.0, 16.0)),
        'dt_bias_bwd': dt_bias(ks[7]),
        'head_norm_w': gain(ks[8], (DEPTH, DELTA_HEAD_DIM)),
        'pool_w': nrm(ks[9], (DEPTH, N_POOL_GROUPS, POOL_GROUP_DIM, POOL_GROUP_DIM), POOL_GROUP_DIM),
        'pool_scale': gain(ks[10], (DEPTH, POOL_WIDTH)),
        'w_out': nrm(ks[11], (DEPTH, MIX_WIDTH, D_MODEL), MIX_WIDTH),
        'norm_ffn_w': gain(ks[12], (DEPTH, D_MODEL)),
        'router_w': nrm(ks[13], (DEPTH, D_MODEL, N_EXPERTS), D_MODEL),
        'expert_w_gate': nrm(ks[14], (DEPTH, N_EXPERTS, D_MODEL, EXPERT_FF), D_MODEL),
        'expert_w_up': nrm(ks[15], (DEPTH, N_EXPERTS, D_MODEL, EXPERT_FF), D_MODEL),
        'expert_w_down': nrm(ks[16], (DEPTH, N_EXPERTS, EXPERT_FF, D_MODEL), EXPERT_FF),
        'norm_final_w': gain(ks[17], (D_MODEL,)),
    }


def reference(x, norm_mix_w, w_in, conv_w, a_log_fwd, dt_bias_fwd, a_log_bwd, dt_bias_bwd,
              head_norm_w, pool_w, pool_scale, w_out, norm_ffn_w, router_w,
              expert_w_gate, expert_w_up, expert_w_down, norm_final_w):
    h = x
    for i in range(DEPTH):
        h = h + _hybrid_mixer(_rmsnorm(h, norm_mix_w[i]), w_in[i], conv_w[i],
                              a_log_fwd[i], dt_bias_fwd[i], a_log_bwd[i], dt_bias_bwd[i],
                              head_norm_w[i], pool_w[i], pool_scale[i], w_out[i])
        h = h + _expert_choice_ffn(_rmsnorm(h, norm_ffn_w[i]), router_w[i],
                                   expert_w_gate[i], expert_w_up[i], expert_w_down[i])
    return _rmsnorm(h, norm_final_w)
```

```python
import numpy as np
from contextlib import ExitStack
import concourse.bass as bass
import concourse.mybir as mybir
from concourse.bass_utils import run_bass_kernel_spmd

F32 = mybir.dt.float32
BF16 = mybir.dt.bfloat16
U32 = mybir.dt.uint32
F32R = mybir.dt.float32r
ALU = mybir.AluOpType
AF = mybir.ActivationFunctionType
AX = mybir.AxisListType

T = 2048
D = 1024
NTC = 16
H = 4
E = 16
CAP = 256
FF = 2048
INC = 2576
EPS = 1e-6
BIG = 30000.0
ENGS = ("pe", "act", "dve", "pool", "sp")


class Tk:
    __slots__ = ("sem", "val", "snap")

    def __init__(s, sem, val, snap):
        s.sem = sem
        s.val = val
        s.snap = snap


class R:
    __slots__ = ("w", "rs")

    def __init__(s):
        s.w = None
        s.rs = {}


class DS:
    def __init__(s, sem):
        s.sem = sem
        s.count = 0


class Prog:
    def __init__(s, psem):
        s.q = {e: [] for e in ENGS}
        s.cnt = {e: 0 for e in ENGS}
        s.vc = {e: {} for e in ENGS}
        s.psem = psem
        s.dma_toks = []

    def _waits(s, eng, reads, writes):
        vc = s.vc[eng]
        need = {}
        toks = []
        for r in reads:
            if r.w is not None:
                toks.append(r.w)
        for r in writes:
            if r.w is not None:
                toks.append(r.w)
            toks.extend(r.rs.values())
        for t in toks:
            if eng == "pe" and t.sem is s.psem["pe"]:
                continue
            k = id(t.sem)
            if vc.get(k, 0) >= t.val:
                continue
            if k not in need or need[k].val < t.val:
                need[k] = t
        return list(need.values())

    def _absorb(s, eng, waits):
        vc = s.vc[eng]
        for t in waits:
            for k, v in t.snap.items():
                if vc.get(k, 0) < v:
                    vc[k] = v
            k = id(t.sem)
            if vc.get(k, 0) < t.val:
                vc[k] = t.val

    def op(s, eng, fn, reads=(), writes=()):
        waits = s._waits(eng, reads, writes)
        s._absorb(eng, waits)
        s.cnt[eng] += 1
        sem = s.psem[eng]
        tok = Tk(sem, s.cnt[eng], dict(s.vc[eng]))
        wl = [(t.sem, t.val) for t in waits]

        def emit(e):
            for sm, v in wl:
                e.wait_ge(sm, v)
            fn(e).then_inc(sem, 1)

        s.q[eng].append(emit)
        for r in reads:
            r.rs[id(sem)] = tok
        for r in writes:
            r.w = tok
            r.rs = {}
        return tok

    def dma(s, eng, ds, fn, reads=(), writes=()):
        waits = s._waits(eng, reads, writes)
        s._absorb(eng, waits)
        ds.count += 16
        tok = Tk(ds.sem, ds.count, dict(s.vc[eng]))
        wl = [(t.sem, t.val) for t in waits]
        sem = ds.sem

        def emit(e):
            for sm, v in wl:
                e.wait_ge(sm, v)
            fn(e).then_inc(sem, 16)

        s.q[eng].append(emit)
        for r in reads:
            r.rs[id(sem)] = tok
        for r in writes:
            r.w = tok
            r.rs = {}
        s.dma_toks.append(tok)
        return tok

    def barrier(s):
        toks = [Tk(s.psem[e], s.cnt[e], dict(s.vc[e])) for e in ENGS if s.cnt[e] > 0 and e != "sp"]
        toks += s.dma_toks
        s.dma_toks = []
        for eng in ENGS:
            vc = s.vc[eng]
            need = {}
            for t in toks:
                k = id(t.sem)
                if vc.get(k, 0) >= t.val:
                    continue
                if k not in need or need[k].val < t.val:
                    need[k] = t
            waits = list(need.values())
            s._absorb(eng, waits)
            wl = [(t.sem, t.val) for t in waits]
            if wl:
                def emit(e, wl=wl):
                    for sm, v in wl:
                        e.wait_ge(sm, v)
                s.q[eng].append(emit)

    def wait_all(s, eng, toks):
        wl = [(t.sem, t.val) for t in toks]

        def emit(e):
            for sm, v in wl:
                e.wait_ge(sm, v)
        s.q[eng].append(emit)


def host_consts():
    p = np.arange(128)[:, None]
    f = np.arange(128)[None, :]
    c = {}
    c["ident"] = (p == f).astype(np.float32)
    c["ut_f"] = (p <= f).astype(np.float32)
    c["ut_b"] = (p >= f).astype(np.float32)
    c["sl_f"] = (p > f).astype(np.float32)
    c["sl_b"] = (p < f).astype(np.float32)
    c["neg_f"] = (-BIG * (f < p)).astype(np.float32)
    c["neg_b"] = (-BIG * (f > p)).astype(np.float32)
    c["str_f"] = (f > p).astype(np.float32)
    c["str_b"] = (f < p).astype(np.float32)
    c["ones"] = np.ones((128, 128), np.float32)
    c["iota"] = np.tile(np.arange(256, dtype=np.float32)[None, :], (128, 1))
    tok = np.zeros((128, 16, 2), np.float32)
    tok[:, :, 0] = np.arange(16, dtype=np.float32)[None, :]
    tok[:, :, 1] = np.arange(128, dtype=np.float32)[:, None]
    c["tok"] = tok.reshape(128, 32)
    invc = np.zeros((128, 4, 16), np.float32)
    for g, w in enumerate((2, 4, 8, 16)):
        lo = w // 2
        hi = w - lo - 1
        for t in range(lo):
            invc[:, g, t] = 1.0 / (t + hi + 1)
        for i in range(hi):
            t = T - hi + i
            invc[:, g, 8 + i] = 1.0 / (T - t + lo)
    c["invc"] = invc.reshape(128, 64)
    return c


CONST_NAMES = ["ident", "ut_f", "ut_b", "sl_f", "sl_b", "neg_f", "neg_b", "str_f", "str_b", "ones"]


def build_nc():
    nc = bass.Bass("TRN2", target_bir_lowering=False)

    def din(name, shape, dt=F32):
        return nc.dram_tensor(name, list(shape), dt, kind="ExternalInput").ap()

    x_d = din("x", [T, D])
    win_d = din("w_in", [D, INC])
    cw_d = din("conv_wP", [128, 60])
    abp_d = din("abp", [128, 16])
    nw_d = [din("nw%d" % i, [128, D]) for i in range(3)]
    hnw_d = din("hnw", [128, 128])
    pw_d = din("pool_wP", [128, 512])
    psc_d = din("pool_scT", [128, 4])
    wout_d = din("w_out", [D, D])
    rw_d = din("router_wP", [128, 128])
    wg_d = din("wg", [E, D, FF])
    wu_d = din("wu", [E, D, FF])
    wd_d = din("wd", [E, FF, D])
    cst_d = {n: din("c_" + n, [128, 128]) for n in CONST_NAMES}
    iota_d = din("c_iota", [128, 256])
    invc_d = din("c_invc", [128, 64])
    out_d = nc.dram_tensor("out", [T, D], F32, kind="ExternalOutput").ap()
    xn2_d = nc.dram_tensor("xn2_scr", [T, D], BF16, kind="Internal").ap()
    tok_d = din("c_tok", [128, 32])

    es = ExitStack()
    with es:
        def sb(name, shape, dt):
            return es.enter_context(nc.sbuf_tensor(name, list(shape), dt))

        def pstile(name):
            return es.enter_context(nc.psum_tensor(name, [128, 512], F32))

        psem = {e: es.enter_context(nc.semaphore("ps_" + e)) for e in ENGS}
        P = Prog(psem)
        out_toks = []

        def newds(name):
            return DS(es.enter_context(nc.semaphore(name)))

        cst = {n: sb("k_" + n, [128, 128], F32) for n in CONST_NAMES}
        identb = sb("identb", [128, 128], BF16)
        onesb = sb("onesb", [128, 128], BF16)
        ltb = sb("ltb", [128, 128], BF16)
        iota = sb("iota", [128, 256], F32)
        tokf = sb("tokf", [128, 32], F32)
        tokb = sb("tokb", [128, 16, 2], BF16)
        invc = sb("invc", [128, 64], F32)
        cw = sb("cw", [128, 12, 5], F32)
        abp = sb("abp_s", [128, 16], F32)
        hnw = sb("hnw_s", [128, 128], F32)
        psc = sb("psc", [128, 4], F32)
        pwb = sb("pwb", [128, 4, 128], BF16)
        rws = sb("rws", [128, 8, 16], F32)
        epsc = sb("epsc", [128, 1], F32)
        onec = sb("onec", [128, 1], F32)
        mhalf = sb("mhalf", [128, 1], F32)
        small = sb("small", [128, 64], F32)
        gsm = sb("gsm", [128, 16, 8], F32)
        bet = sb("bet", [128, 16, 8], F32)
        nbet = sb("nbet", [128, 16, 8], F32)
        exps = sb("exps", [128, 16, 24], F32)
        prob = sb("prob", [128, 16, 16], F32)
        sel = sb("sel", [128, 16, 16], F32)
        selb = sb("selb", [128, 16, 16], BF16)
        posm = sb("posm", [128, 16, 16], F32)
        R_c = R()
        R_g = R()
        R_exps = R()
        R_prob = R()
        R_sel = R()
        R_posm = R()
        R_small = R()

        ARENA_B = 190 * 1024
        arena = sb("arena", [128, ARENA_B // 2], BF16)

        def view(off, shape, dt):
            n = 1
            for s_ in shape[1:]:
                n *= s_
            nb = n * (4 if dt in (F32, U32) else 2)
            assert off % 4 == 0 and off + nb <= ARENA_B, (off, nb)
            v = arena[:, off // 2:(off + nb) // 2]
            if dt in (F32, U32):
                v = v.bitcast(dt)
            if len(shape) == 2:
                return v
            if len(shape) == 3:
                return v.rearrange("p (a b) -> p a b", b=shape[2])
            if len(shape) == 4:
                return v.rearrange("p (a b c) -> p a b c", b=shape[2], c=shape[3])
            raise ValueError

        K = 1024
        OFF_A, OFF_B, OFF_C, OFF_D, OFF_E, OFF_F = 0, 64 * K, 96 * K, 128 * K, 144 * K, 160 * K

        ps = [pstile("ps%d" % i) for i in range(8)]
        R_ps = [R() for _ in range(8)]

        def psb(i, dt=F32):
            return ps[i][:, :] if dt == F32 else ps[i][:, :].bitcast(BF16)

        ds_c = newds("ds_c")
        ds_cp = newds("ds_cp")
        ds_x = [newds("ds_x0"), newds("ds_x1")]
        ds_x4 = ds_x + [newds("ds_x2"), newds("ds_x3")]
        ds_w = [newds("ds_w%d" % i) for i in range(12)]
        ds_o = [newds("ds_o0"), newds("ds_o1")]
        ds_s = [newds("ds_s0"), newds("ds_s1")]
        ds_g = [newds("ds_g0"), newds("ds_g1")]

        for n in CONST_NAMES:
            P.dma("sp", ds_c, lambda e, n=n: e.dma_start(out=cst[n][:, :], in_=cst_d[n]), writes=[R_c])
        P.dma("sp", ds_c, lambda e: e.dma_start(out=iota[:, :], in_=iota_d), writes=[R_c])
        P.dma("sp", ds_c, lambda e: e.dma_start(out=tokf[:, :], in_=tok_d), writes=[R_c])
        P.dma("sp", ds_c, lambda e: e.dma_start(out=invc[:, :], in_=invc_d), writes=[R_c])
        P.dma("sp", ds_c, lambda e: e.dma_start(out=cw[:, :, :].rearrange("p c j -> p (c j)"), in_=cw_d), writes=[R_c])
        P.dma("sp", ds_c, lambda e: e.dma_start(out=abp[:, :], in_=abp_d), writes=[R_c])
        P.dma("sp", ds_c, lambda e: e.dma_start(out=hnw[:, :], in_=hnw_d), writes=[R_c])
        P.dma("sp", ds_c, lambda e: e.dma_start(out=psc[:, :], in_=psc_d), writes=[R_c])
        P.dma("sp", ds_c, lambda e: e.dma_start(out=rws[:, :, :].rearrange("p c e -> p (c e)"), in_=rw_d), writes=[R_c])
        P.dma("pool", ds_cp, lambda e: e.dma_start(out=pwb[:, :, :].rearrange("p g d -> p (g d)"), in_=pw_d), writes=[R_c])
        P.op("pool", lambda e: e.tensor_copy(out=identb[:, :], in_=cst["ident"][:, :]), reads=[R_c], writes=[R_c])
        P.op("pool", lambda e: e.tensor_copy(out=onesb[:, :], in_=cst["ones"][:, :]), reads=[R_c], writes=[R_c])
        P.op("pool", lambda e: e.tensor_copy(out=ltb[:, :], in_=cst["sl_b"][:, :]), reads=[R_c], writes=[R_c])
        P.op("pool", lambda e: e.tensor_copy(out=tokb[:, :, :].rearrange("p a b -> p (a b)"), in_=tokf[:, :]), reads=[R_c], writes=[R_c])
        P.op("pool", lambda e: e.memset(epsc[:, :], EPS), writes=[R_c])
        P.op("pool", lambda e: e.memset(onec[:, :], 1.0), writes=[R_c])
        P.op("pool", lambda e: e.memset(mhalf[:, :], -0.5), writes=[R_c])
        P.op("act", lambda e: e.activation(out=small[:, 0:8], in_=abp[:, 0:8], func=AF.Exp), reads=[R_c], writes=[R_c])
        P.op("dve", lambda e: e.tensor_scalar(out=small[:, 0:8], in0=small[:, 0:8], scalar1=-1.0, scalar2=None, op0=ALU.mult), reads=[R_c], writes=[R_c])
        P.barrier()
        RC = [R_c]

        ident = cst["ident"]

        XNT = view(OFF_A, [128, 8, T], BF16)
        WINP = [view(OFF_A + 32 * K + i * 8704, [128, 8, 528], BF16) for i in range(2)]
        RAW = view(OFF_A + 32 * K + 17408, [128, 2056], F32)
        RAWS = [RAW, view(OFF_D, [128, 2056], F32)]
        NWBC1 = view(OFF_A + 32 * K + 17408 + 8224, [128, D], F32)
        SB2 = view(OFF_A + 32 * K + 17408, [128, T + 32], F32)
        QT = view(OFF_B, [128, H, T], BF16)
        KT = view(OFF_B + 16 * K, [128, H, T], BF16)
        KTOK = view(OFF_C, [128, NTC, H, 128], BF16)
        VTOK = view(OFF_C + 16 * K, [128, NTC, H, 128], BF16)
        ZS = view(OFF_D, [128, NTC, 512], BF16)
        OPT = view(OFF_E, [128, 4, T], BF16)
        SQ = view(OFF_E, [128, T], BF16)
        VTMP = view(OFF_E, [128, T], BF16)
        RSQ = view(OFF_E + 4 * K, [128, T], F32)
        XT = [view(OFF_F + i * 4 * K, [128, D], F32) for i in range(2)]
        XT4 = XT + [view(OFF_F + 25 * K, [128, D], F32), view(OFF_D + 11 * K, [128, D], F32)]
        XN = view(OFF_F + 8 * K, [128, D], BF16)
        CONVT = view(OFF_F + 10 * K, [128, T], F32)
        CONVS = [CONVT, view(OFF_F + 21 * K, [128, T], F32)]
        UB = view(OFF_F + 10 * K, [128, T + 32], F32)
        ZF = view(OFF_F + 10 * K, [128, 512], F32)
        ABS = view(OFF_F + 19 * K, [128, 16, 16], F32)
        TM1 = view(OFF_F + 20 * K, [128, 16, 8], F32)
        PB3 = view(OFF_F + 21 * K, [128, T + 32], F32)
        DIFB = view(OFF_F, [128, T], BF16)
        R_xnt = [R() for _ in range(NTC)]
        R_xt = [R(), R()]
        R_xt4 = R_xt + [R(), R()]
        R_xn = R()
        R_winp = [R(), R()]
        R_raw = R()
        R_acc = R()
        R_raws = [R_raw, R()]
        R_accs = [R_acc, R()]
        R_nw = R()
        R_qt = [R() for _ in range(H)]
        R_kt = [R() for _ in range(H)]
        R_ktok = R()
        R_vtok = R()
        R_zs = [R() for _ in range(NTC)]
        R_opt = [R() for _ in range(4)]
        R_abs = R()
        R_pb3 = R()
        R_difb = R()

        P.dma("sp", ds_c, lambda e: e.dma_start(out=NWBC1[:, :], in_=nw_d[0]), writes=[R_nw])
        P.op("pool", lambda e: e.memset(RAW[:, :], 0.0), writes=[R_raw])
        P.op("pool", lambda e: e.memset(RAWS[1][:, :], 0.0), writes=[R_raws[1]])

        XNS = [XN, view(OFF_D + 9 * K, [128, D], BF16)]
        R_xns = [R_xn, R()]
        R_sm1 = [R(), R()]
        JUNK = view(OFF_F + 21 * K, [128, D], F32)
        R_junk = R()
        def front_1a(tc):
            b = tc % 2
            sc = 8 + 3 * b
            xb = tc % 4
            P.dma("sp", ds_x4[xb], lambda e: e.dma_start(out=XT4[xb][:, :], in_=x_d[tc * 128:(tc + 1) * 128, :]), writes=[R_xt4[xb]])
            P.op("act", lambda e: e.activation(out=JUNK[:, :], in_=XT4[xb][:, :], func=AF.Square, accum_out=small[:, sc:sc + 1]),
                 reads=[R_xt4[xb]], writes=[R_junk, R_sm1[b]])
            P.op("act", lambda e: e.activation(out=small[:, sc + 1:sc + 2], in_=small[:, sc:sc + 1], func=AF.Sqrt, bias=epsc[:, :], scale=1.0 / D),
                 reads=[R_c], writes=[R_sm1[b]])
            P.op("dve", lambda e: e.reciprocal(out=small[:, sc + 2:sc + 3], in_=small[:, sc + 1:sc + 2]), reads=[], writes=[R_sm1[b]])
            P.op("dve", lambda e: e.scalar_tensor_tensor(out=XNS[b][:, :], in0=XT4[xb][:, :], scalar=small[:, sc + 2:sc + 3], in1=NWBC1[:, :],
                                                         op0=ALU.mult, op1=ALU.mult),
                 reads=[R_xt4[xb], R_sm1[b], R_nw], writes=[R_xns[b]])

        def back_1a(tc):
            b = tc % 2
            pb = tc % 2
            for dc in range(8):
                P.op("pe", lambda e, dc=dc: e.transpose(psb(pb, BF16)[:, dc * 128:(dc + 1) * 128], XNS[b][:, dc * 128:(dc + 1) * 128], identb[:, :]),
                     reads=[R_xns[b], R_c], writes=[R_ps[pb]])
            P.op("act", lambda e: e.activation(out=XNT[:, :, tc * 128:(tc + 1) * 128],
                                               in_=psb(pb, BF16)[:, 0:1024].rearrange("p (a b) -> p a b", b=128), func=AF.Copy),
                 reads=[], writes=[R_ps[pb], R_xnt[tc]])

        front_1a(0)
        for tc in range(NTC):
            if tc + 1 < NTC:
                front_1a(tc + 1)
            back_1a(tc)

        wpi = [0]

        def load_winp(col0, ncol):
            i = wpi[0] % 2
            wpi[0] += 1
            P.dma("pool", ds_w[i], lambda e, i=i: e.dma_start(out=WINP[i][:, :, 0:ncol],
                                                              in_=win_d.rearrange("(c p) n -> p c n", p=128)[:, :, col0:col0 + ncol]),
                  writes=[R_winp[i]])
            return i

        def proj_chunk(wi, lcol, evac):
            for tb in range(4):
                pb = 2 + tb % 2
                for dc in range(8):
                    P.op("pe", lambda e, dc=dc, tb=tb, pb=pb: e.matmul(psb(pb), WINP[wi][:, dc, lcol:lcol + 128], XNT[:, dc, tb * 512:(tb + 1) * 512],
                                                                      start=(dc == 0), stop=(dc == 7)),
                         reads=[R_winp[wi]] + R_xnt[tb * 4:(tb + 1) * 4], writes=[R_ps[pb]])
                evac(tb, pb)

        def mk_evac_raw(RAWB, R_rawb):
            def evac_raw(tb, pb):
                P.op("act", lambda e, tb=tb, pb=pb: e.activation(out=RAWB[:, 2 + tb * 512: 2 + (tb + 1) * 512], in_=psb(pb), func=AF.Copy),
                     reads=[], writes=[R_ps[pb], R_rawb])
            return evac_raw

        RSQS = [RSQ, RSQ]
        R_rsqs = [R_opt[1], R_opt[1]]
        chunks = [(grp, kind, hh) for grp, kind in enumerate(("q", "k", "v")) for hh in range(H)]
        wis = {}

        def stage_a(ci):
            grp, kind, hh = chunks[ci]
            if hh == 0:
                wis[grp] = load_winp(grp * 512, 512)
            wi = wis[grp]
            cc = grp * 4 + hh
            RAWB, R_rawb = RAWS[cc % 2], R_raws[cc % 2]
            CONVB, R_accb = CONVS[cc % 2], R_accs[cc % 2]
            proj_chunk(wi, hh * 128, mk_evac_raw(RAWB, R_rawb))

        def stage_c(ci):
            grp, kind, hh = chunks[ci]
            cc = grp * 4 + hh
            RAWB, R_rawb = RAWS[cc % 2], R_raws[cc % 2]
            CONVB, R_accb = CONVS[cc % 2], R_accs[cc % 2]
            P.op("dve", lambda e: e.tensor_scalar(out=CONVB[:, :], in0=RAWB[:, 0:T], scalar1=cw[:, cc, 0:1], scalar2=None, op0=ALU.mult),
                 reads=[R_rawb, R_c], writes=[R_accb])
            for j in range(1, 5):
                P.op("dve", lambda e, j=j: e.scalar_tensor_tensor(out=CONVB[:, :], in0=RAWB[:, j:j + T], scalar=cw[:, cc, j:j + 1], in1=CONVB[:, :],
                                                                  op0=ALU.mult, op1=ALU.add),
                     reads=[R_rawb, R_c], writes=[R_accb])
            if kind == "v":
                P.op("act", lambda e: e.activation(out=VTMP[:, :], in_=CONVB[:, :], func=AF.Silu), reads=[R_accb], writes=[R_opt[0]])
                for half in range(2):
                    pb = 4 + half
                    for t8 in range(8):
                        tc = half * 8 + t8
                        P.op("pe", lambda e, tc=tc, t8=t8, pb=pb: e.transpose(psb(pb, BF16)[:, t8 * 128:(t8 + 1) * 128], VTMP[:, tc * 128:(tc + 1) * 128], identb[:, :]),
                             reads=[R_opt[0], R_c], writes=[R_ps[pb]])
                    P.op("act", lambda e, half=half, pb=pb: e.activation(out=VTOK[:, half * 8:(half + 1) * 8, hh, :],
                                                                        in_=psb(pb, BF16)[:, 0:1024].rearrange("p (a b) -> p a b", b=128), func=AF.Copy),
                         reads=[], writes=[R_ps[pb], R_vtok])
            else:
                P.op("act", lambda e: e.activation(out=CONVB[:, :], in_=CONVB[:, :], func=AF.Silu), reads=[], writes=[R_accb])
                P.op("pool", lambda e: e.tensor_tensor(out=SQ[:, :], in0=CONVB[:, :], in1=CONVB[:, :], op=ALU.mult), reads=[R_accb], writes=[R_opt[0]])

        def stage_a2(ci):
            grp, kind, hh = chunks[ci]
            cc = grp * 4 + hh
            RSQB, R_rsqb = RSQS[cc % 2], R_rsqs[cc % 2]
            if kind != "v":
                for tb in range(4):
                    pb = 4 + tb % 2
                    P.op("pe", lambda e, tb=tb, pb=pb: e.matmul(psb(pb), onesb[:, :], SQ[:, tb * 512:(tb + 1) * 512], start=True, stop=True),
                         reads=[R_opt[0], R_c], writes=[R_ps[pb]])
                    P.op("act", lambda e, tb=tb, pb=pb: e.activation(out=RSQB[:, tb * 512:(tb + 1) * 512], in_=psb(pb), func=AF.Ln, bias=epsc[:, :], scale=1.0),
                         reads=[R_c], writes=[R_ps[pb], R_rsqb])

        def stage_b(ci):
            grp, kind, hh = chunks[ci]
            if kind == "v":
                return
            cc = grp * 4 + hh
            CONVB, R_accb = CONVS[cc % 2], R_accs[cc % 2]
            RSQB, R_rsqb = RSQS[cc % 2], R_rsqs[cc % 2]
            P.op("act", lambda e: e.activation(out=RSQB[:, :], in_=RSQB[:, :], func=AF.Exp, scale=-0.5), reads=[], writes=[R_rsqb])
            dst = QT if kind == "q" else KT
            rdst = R_qt[hh] if kind == "q" else R_kt[hh]
            scl = 128.0 ** -0.5 if kind == "q" else 1.0
            P.op("dve", lambda e: e.scalar_tensor_tensor(out=dst[:, hh, :], in0=CONVB[:, :], scalar=scl, in1=RSQB[:, :], op0=ALU.mult, op1=ALU.mult),
                 reads=[R_accb, R_rsqb], writes=[rdst])
            if kind == "k":
                for half in range(2):
                    pb = 6 + half
                    for t8 in range(8):
                        tc = half * 8 + t8
                        P.op("pe", lambda e, tc=tc, t8=t8, pb=pb: e.transpose(psb(pb, BF16)[:, t8 * 128:(t8 + 1) * 128], KT[:, hh, tc * 128:(tc + 1) * 128], identb[:, :]),
                             reads=[R_kt[hh], R_c], writes=[R_ps[pb]])
                    P.op("act", lambda e, half=half, pb=pb: e.activation(out=KTOK[:, half * 8:(half + 1) * 8, hh, :],
                                                                        in_=psb(pb, BF16)[:, 0:1024].rearrange("p (a b) -> p a b", b=128), func=AF.Copy),
                         reads=[], writes=[R_ps[pb], R_ktok])

        NCH = len(chunks)
        stage_a(0)
        for ci in range(NCH + 1):
            if ci + 1 < NCH:
                stage_a(ci + 1)
            if ci < NCH:
                stage_c(ci)
            if ci >= 1:
                stage_b(ci - 1)
            if ci < NCH:
                stage_a2(ci)
        P.barrier()

        wi = load_winp(1536, 528)
        for tc in range(NTC):
            pb = 2 + tc % 2
            for dc in range(8):
                P.op("pe", lambda e, dc=dc, tc=tc, pb=pb, wi=wi: e.matmul(psb(pb), XNT[:, dc, tc * 128:(tc + 1) * 128], WINP[wi][:, dc, 0:512], start=(dc == 0), stop=(dc == 7)),
                     reads=[R_winp[wi], R_xnt[tc]], writes=[R_ps[pb]])
            P.op("act", lambda e, pb=pb: e.activation(out=ZF[:, :], in_=psb(pb), func=AF.Silu), reads=[], writes=[R_ps[pb], R_acc])
            P.op("dve", lambda e, tc=tc: e.tensor_tensor(out=ZS[:, tc, :].rearrange("p (h d) -> p h d", d=128), in0=ZF[:, :].rearrange("p (h d) -> p h d", d=128),
                                                         in1=hnw[:, :].unsqueeze(1).to_broadcast([128, 4, 128]), op=ALU.mult),
                 reads=[R_acc, R_c], writes=[R_zs[tc]])
        ABV = psb(4)[:, 0:256].rearrange("p (a b) -> p a b", b=16)
        for tc in range(NTC):
            for dc in range(8):
                P.op("pe", lambda e, dc=dc, tc=tc, wi=wi: e.matmul(ABV[:, tc, :], XNT[:, dc, tc * 128:(tc + 1) * 128], WINP[wi][:, dc, 512:528], start=(dc == 0), stop=(dc == 7)),
                     reads=[R_winp[wi], R_xnt[tc]], writes=[R_ps[4]])
        P.op("act", lambda e: e.activation(out=ABS[:, :, :], in_=ABV, func=AF.Copy), reads=[], writes=[R_ps[4], R_abs])
        ABS5 = ABS[:, :, :].rearrange("p a (d k h) -> p a d k h", d=2, k=2)
        for d_ in range(2):
            P.op("dve", lambda e, d_=d_: e.tensor_tensor(out=TM1[:, :, d_ * 4:(d_ + 1) * 4], in0=ABS5[:, :, d_, 0, :],
                                                         in1=abp[:, 8 + d_ * 4: 12 + d_ * 4].unsqueeze(1).to_broadcast([128, 16, 4]), op=ALU.add),
                 reads=[R_abs, R_c], writes=[R_g])
            P.op("act", lambda e, d_=d_: e.activation(out=bet[:, :, d_ * 4:(d_ + 1) * 4], in_=ABS5[:, :, d_, 1, :], func=AF.Sigmoid),
                 reads=[R_abs], writes=[R_g])
        P.op("act", lambda e: e.activation(out=TM1[:, :, :], in_=TM1[:, :, :], func=AF.Exp), reads=[], writes=[R_g])
        P.op("act", lambda e: e.activation(out=TM1[:, :, :], in_=TM1[:, :, :], func=AF.Ln, bias=onec[:, :], scale=1.0), reads=[R_c], writes=[R_g])
        P.op("dve", lambda e: e.tensor_tensor(out=gsm[:, :, :], in0=TM1[:, :, :], in1=small[:, 0:8].unsqueeze(1).to_broadcast([128, 16, 8]), op=ALU.mult),
             reads=[R_c], writes=[R_g])
        P.op("dve", lambda e: e.tensor_scalar(out=nbet[:, :, :], in0=bet[:, :, :], scalar1=-1.0, scalar2=None, op0=ALU.mult), reads=[], writes=[R_g])

        wi = load_winp(2064, 512)
        TW = T + 8
        P.op("pool", lambda e: e.memset(UB[:, :], 0.0), reads=[], writes=[R_acc])
        P.op("pool", lambda e: e.memset(SB2[:, :], 0.0), reads=[], writes=[R_raw, R_nw])
        P.op("pool", lambda e: e.memset(PB3[:, :], 0.0), reads=[], writes=[R_pb3])

        def evac_u(tb, pb):
            P.op("act", lambda e, tb=tb, pb=pb: e.activation(out=UB[:, 16 + tb * 512: 16 + (tb + 1) * 512], in_=psb(pb), func=AF.Copy),
                 reads=[], writes=[R_ps[pb], R_acc])

        for g, w in enumerate((2, 4, 8, 16)):
            lo = w // 2
            hi = w - lo - 1
            proj_chunk(wi, g * 128, evac_u)
            s_prev, r_prev = UB, R_acc
            sh = 1
            for lv in range({2: 1, 4: 2, 8: 3, 16: 4}[w]):
                dstb, rdst = (SB2, R_raw) if lv % 2 == 0 else (PB3, R_pb3)
                P.op("dve", lambda e, s_prev=s_prev, dstb=dstb, sh=sh: e.tensor_tensor(out=dstb[:, 16:16 + TW], in0=s_prev[:, 16:16 + TW], in1=s_prev[:, 16 - sh:16 - sh + TW], op=ALU.add),
                     reads=[r_prev], writes=[rdst])
                s_prev, r_prev = dstb, rdst
                sh *= 2
            P.op("dve", lambda e, s_prev=s_prev, hi=hi, w=w: e.scalar_tensor_tensor(out=DIFB[:, :], in0=s_prev[:, 16 + hi:16 + hi + T], scalar=1.0 / w, in1=UB[:, 16:16 + T],
                                                                                   op0=ALU.mult, op1=ALU.subtract),
                 reads=[r_prev, R_acc], writes=[R_difb])
            P.op("dve", lambda e, s_prev=s_prev, hi=hi, lo=lo, g=g: e.tensor_tensor(out=small[:, 16:16 + lo], in0=s_prev[:, 16 + hi:16 + hi + lo], in1=invc[:, g * 16:g * 16 + lo], op=ALU.mult),
                 reads=[r_prev, R_c], writes=[R_small])
            P.op("dve", lambda e, lo=lo: e.tensor_tensor(out=DIFB[:, 0:lo], in0=small[:, 16:16 + lo], in1=UB[:, 16:16 + lo], op=ALU.subtract),
                 reads=[R_small, R_acc], writes=[R_difb])
            if hi > 0:
                P.op("dve", lambda e, s_prev=s_prev, hi=hi, g=g: e.tensor_tensor(out=small[:, 32:32 + hi], in0=s_prev[:, 16 + T:16 + T + hi], in1=invc[:, g * 16 + 8:g * 16 + 8 + hi], op=ALU.mult),
                     reads=[r_prev, R_c], writes=[R_small])
                P.op("dve", lambda e, hi=hi: e.tensor_tensor(out=DIFB[:, T - hi:T], in0=small[:, 32:32 + hi], in1=UB[:, 16 + T - hi:16 + T], op=ALU.subtract),
                     reads=[R_small, R_acc], writes=[R_difb])
            for tb in range(4):
                pb = 4 + tb % 2
                P.op("pe", lambda e, tb=tb, pb=pb, g=g: e.matmul(psb(pb), pwb[:, g, :], DIFB[:, tb * 512:(tb + 1) * 512], start=True, stop=True),
                     reads=[R_difb, R_c], writes=[R_ps[pb]])
                P.op("act", lambda e, tb=tb, pb=pb, g=g: e.activation(out=OPT[:, g, tb * 512:(tb + 1) * 512], in_=psb(pb), func=AF.Identity, scale=psc[:, g:g + 1]),
                     reads=[R_c], writes=[R_ps[pb], R_opt[g]])
        P.barrier()

        if DEBUG:
            ds_dbg = newds("ds_dbg")
            for name, ap, shape, dt in (("d_qt", QT, [128, H * T], BF16), ("d_kt", KT, [128, H * T], BF16),
                                        ("d_ktok", KTOK, [128, NTC * H * 128], BF16), ("d_vtok", VTOK, [128, NTC * H * 128], BF16),
                                        ("d_zs", ZS, [128, NTC * 512], BF16), ("d_opt", OPT, [128, 4 * T], BF16)):
                dd = nc.dram_tensor(name, shape, dt, kind="ExternalOutput").ap()
                flat = ap
                if len(ap.shape) == 3:
                    flat = ap.rearrange("p a b -> p (a b)")
                elif len(ap.shape) == 4:
                    flat = ap.rearrange("p a b c -> p (a b c)")
                out_toks.append(P.dma("sp", ds_dbg, lambda e, dd=dd, flat=flat: e.dma_start(out=dd, in_=flat)))
            for name, tl in (("d_gsm", gsm), ("d_bet", bet)):
                dd = nc.dram_tensor(name, [128, 128], F32, kind="ExternalOutput").ap()
                out_toks.append(P.dma("sp", ds_dbg, lambda e, dd=dd, tl=tl: e.dma_start(out=dd, in_=tl[:, :, :].rearrange("p a b -> p (a b)"))))

        NEU = F32
        OACC = view(OFF_A, [128, NTC, H, 128], F32)

        def make_ctx(base, limit, banks):
            o2 = [base]

            def tv(shape, dt):
                n = 1
                for s_ in shape[1:]:
                    n *= s_
                nb = n * (4 if dt == F32 else 2)
                off = o2[0]
                o2[0] += nb
                assert o2[0] <= limit, (o2[0], limit)
                return view(off, shape, dt)
            c = {}
            for nm in ("MG", "EM", "EMS", "BYV"):
                c[nm] = tv([128, 4, 128], F32)
            for nm in ("AK", "ATK", "RRK"):
                c[nm] = [tv([128, 4, 128], NEU) for _ in range(2)]
            for nm in ("QKT", "NTB", "EK", "KDEC", "YWT", "VNEW"):
                c[nm] = tv([128, 4, 128], BF16)
            for nm in ("MG", "EM", "EMS", "BYV", "QKT", "NTB", "EK", "KDEC", "YWT", "VNEW"):
                c["R_" + nm] = R()
            for nm in ("AK", "ATK", "RRK"):
                c["R_" + nm] = [R(), R()]
            c["banks"] = banks
            c["end"] = o2[0]
            return c

        ctxs = [make_ctx(OFF_A + 32 * K, OFF_A + 64 * K, (0, 1, 2, 3)), make_ctx(OFF_F, ARENA_B, (4, 5, 6, 7))]
        SST = view(ctxs[0]["end"], [128, 8, 128], F32)
        SBF = view(ctxs[0]["end"] + 4 * K, [128, 8, 128], BF16)
        assert ctxs[0]["end"] + 6 * K <= OFF_A + 64 * K
        R_oacc = [R() for _ in range(NTC)]
        R_s = [R(), R()]
        R_sbf = [R(), R()]

        for n in range(NTC):
            P.op("pool", lambda e, n=n: e.memset(OACC[:, n, :, :], 0.0), writes=[R_oacc[n]])
        P.op("pool", lambda e: e.memset(SST[:, :, :], 0.0), writes=R_s)
        P.op("pool", lambda e: e.memset(SBF[:, :, :], 0.0), writes=R_sbf)

        EV = psb(0)[:, 0:384].rearrange("p (a b) -> p a b", b=24)
        for d_ in range(2):
            sfx = "_f" if d_ == 0 else "_b"
            for j, lh in enumerate((cst["ut" + sfx], cst["sl" + sfx], cst["ones"])):
                c0 = j * 8 + d_ * 4
                P.op("pe", lambda e, d_=d_, lh=lh, c0=c0: e.matmul(EV[:, :, c0:c0 + 4], lh[:, :], gsm[:, :, d_ * 4:(d_ + 1) * 4], start=True, stop=True),
                     reads=[R_c, R_g], writes=[R_ps[0]])
        P.op("act", lambda e: e.activation(out=exps[:, :, :], in_=EV, func=AF.Exp), reads=[], writes=[R_ps[0], R_exps])

        def v4(i):
            return psb(i).rearrange("p (h c) -> p h c", h=4)

        identbc = ident[:, :].unsqueeze(1).to_broadcast([128, 4, 128])

        def group(n, d_, c):
            sfx = "_f" if d_ == 0 else "_b"
            ut, sl, neg, st = cst["ut" + sfx], cst["sl" + sfx], cst["neg" + sfx], cst["str" + sfx]
            ns = slice(n * 128, (n + 1) * 128)
            c4 = d_ * 4
            b0, b1, b2, b3 = c["banks"]
            MG, EM, EMS, BYV, AK, ATK, RRK = c["MG"], c["EM"], c["EMS"], c["BYV"], c["AK"], c["ATK"], c["RRK"]
            QKT, NTB, EK, KDEC, YWT, VNEW = c["QKT"], c["NTB"], c["EK"], c["KDEC"], c["YWT"], c["VNEW"]
            R_mg, R_em, R_ems, R_byv, R_ak, R_atk, R_rrk = c["R_MG"], c["R_EM"], c["R_EMS"], c["R_BYV"], c["R_AK"], c["R_ATK"], c["R_RRK"]
            R_qkt, R_ntb, R_ek, R_kdec, R_ywt, R_vnew = c["R_QKT"], c["R_NTB"], c["R_EK"], c["R_KDEC"], c["R_YWT"], c["R_VNEW"]
            for h in range(4):
                P.op("pool", lambda e, h=h: e.tensor_scalar(out=MG[:, h, :], in0=sl[:, :], scalar1=gsm[:, n, c4 + h:c4 + h + 1], scalar2=1.0, op0=ALU.mult, op1=ALU.mult),
                     reads=[R_c, R_g], writes=[R_mg])
            yield
            DV = v4(b1)
            for h in range(4):
                P.op("pe", lambda e, h=h: e.matmul(DV[:, h, :], MG[:, h, :], ut[:, :], start=True, stop=False), reads=[R_mg, R_c], writes=[R_ps[b1]])
                P.op("pe", lambda e, h=h: e.matmul(DV[:, h, :], ident[:, :], neg[:, :], start=False, stop=True), reads=[R_c], writes=[R_ps[b1]])
            yield
            P.op("act", lambda e: e.activation(out=EM[:, :, :], in_=DV, func=AF.Exp), reads=[], writes=[R_ps[b1], R_em])
            P.op("pool", lambda e: e.tensor_tensor(out=EMS[:, :, :], in0=EM[:, :, :], in1=st[:, :].unsqueeze(1).to_broadcast([128, 4, 128]), op=ALU.mult),
                 reads=[R_em, R_c], writes=[R_ems])
            yield
            for hp in range(2):
                V4 = psb(b0).rearrange("p (h k c) -> p h k c", h=2, k=2)
                for hl in range(2):
                    h = hp * 2 + hl
                    P.op("pe", lambda e, h=h, hl=hl: e.matmul(V4[:, hl, 0, :], KT[:, h, ns], KT[:, h, ns], start=True, stop=True), reads=[R_kt[h]], writes=[R_ps[b0]])
                    P.op("pe", lambda e, h=h, hl=hl: e.matmul(V4[:, hl, 1, :], KT[:, h, ns], QT[:, h, ns], start=True, stop=True), reads=[R_kt[h], R_qt[h]], writes=[R_ps[b0]])
                yield
                for hl in range(2):
                    h = hp * 2 + hl
                    P.op("dve", lambda e, h=h, hl=hl: e.scalar_tensor_tensor(out=AK[0][:, h, :], in0=V4[:, hl, 0, :], scalar=nbet[:, n, c4 + h:c4 + h + 1], in1=EMS[:, h, :],
                                                                             op0=ALU.mult, op1=ALU.mult),
                         reads=[R_g, R_ems], writes=[R_ps[b0], R_ak[0]])
                P.op("dve", lambda e, hp=hp: e.tensor_tensor(out=QKT[:, 2 * hp:2 * hp + 2, :], in0=V4[:, :, 1, :], in1=EM[:, 2 * hp:2 * hp + 2, :], op=ALU.mult),
                     reads=[R_em], writes=[R_ps[b0], R_qkt])
                yield
            TV = psb(b1, BF16)[:, 0:512].rearrange("p (h c) -> p h c", h=4) if NEU == BF16 else v4(b1)
            idn = identb if NEU == BF16 else ident
            for h in range(4):
                P.op("pe", lambda e, h=h: e.transpose(TV[:, h, :], AK[0][:, h, :], idn[:, :]), reads=[R_ak[0], R_c], writes=[R_ps[b1]])
            yield
            P.op("act", lambda e: e.activation(out=ATK[0][:, :, :], in_=TV, func=AF.Copy), reads=[], writes=[R_ps[b1], R_atk[0]])
            P.op("pool", lambda e: e.tensor_tensor(out=RRK[0][:, :, :], in0=AK[0][:, :, :], in1=identbc, op=ALU.add), reads=[R_ak[0], R_c], writes=[R_rrk[0]])
            yield
            cur = 0
            AV, ATV, RV = v4(b2), v4(b3), v4(b1)
            for k in range(1, 7):
                nxt = 1 - cur
                for h in range(4):
                    P.op("pe", lambda e, h=h, cur=cur: e.matmul(ATV[:, h, :], AK[cur][:, h, :], ATK[cur][:, h, :], start=True, stop=True),
                         reads=[R_ak[cur], R_atk[cur]], writes=[R_ps[b3]])
                if k < 6:
                    for h in range(4):
                        P.op("pe", lambda e, h=h, cur=cur: e.matmul(AV[:, h, :], ATK[cur][:, h, :], AK[cur][:, h, :], start=True, stop=True),
                             reads=[R_ak[cur], R_atk[cur]], writes=[R_ps[b2]])
                yield
                P.op("act", lambda e, nxt=nxt: e.activation(out=ATK[nxt][:, :, :], in_=ATV, func=AF.Copy), reads=[], writes=[R_ps[b3], R_atk[nxt]])
                if k < 6:
                    P.op("dve", lambda e, nxt=nxt: e.tensor_copy(out=AK[nxt][:, :, :], in_=AV), reads=[], writes=[R_ps[b2], R_ak[nxt]])
                yield
                for h in range(4):
                    P.op("pe", lambda e, h=h, cur=cur, nxt=nxt: e.matmul(RV[:, h, :], ATK[nxt][:, h, :], RRK[cur][:, h, :], start=True, stop=True),
                         reads=[R_atk[nxt], R_rrk[cur]], writes=[R_ps[b1]])
                yield
                if k < 6:
                    P.op("dve", lambda e, cur=cur, nxt=nxt: e.tensor_tensor(out=RRK[nxt][:, :, :], in0=RV, in1=RRK[cur][:, :, :], op=ALU.add),
                         reads=[R_rrk[cur]], writes=[R_ps[b1], R_rrk[nxt]])
                else:
                    P.op("dve", lambda e, cur=cur: e.tensor_tensor(out=NTB[:, :, :], in0=RV, in1=RRK[cur][:, :, :], op=ALU.add),
                         reads=[R_rrk[cur]], writes=[R_ps[b1], R_ntb])
                cur = nxt
                yield
            for h in range(4):
                P.op("act", lambda e, h=h: e.activation(out=EK[:, h, :], in_=KTOK[:, n, h, :], func=AF.Identity, scale=exps[:, n, c4 + h:c4 + h + 1]),
                     reads=[R_ktok, R_exps], writes=[R_ek])
                P.op("act", lambda e, h=h: e.activation(out=KDEC[:, h, :], in_=KTOK[:, n, h, :], func=AF.Identity, scale=exps[:, n, 8 + c4 + h:8 + c4 + h + 1]),
                     reads=[R_ktok, R_exps], writes=[R_kdec])
            yield
            YVV, YWV = v4(b0), v4(b2)
            for h in range(4):
                P.op("pe", lambda e, h=h: e.matmul(YVV[:, h, :], NTB[:, h, :], VTOK[:, n, h, :], start=True, stop=True), reads=[R_ntb, R_vtok], writes=[R_ps[b0]])
            for h in range(4):
                P.op("pe", lambda e, h=h: e.matmul(YWV[:, h, :], EK[:, h, :], NTB[:, h, :], start=True, stop=True), reads=[R_ntb, R_ek], writes=[R_ps[b2]])
            yield
            for h in range(4):
                P.op("act", lambda e, h=h: e.activation(out=BYV[:, h, :], in_=YVV[:, h, :], func=AF.Identity, scale=bet[:, n, c4 + h:c4 + h + 1]),
                     reads=[R_g], writes=[R_ps[b0], R_byv])
            P.op("act", lambda e: e.activation(out=YWT[:, :, :], in_=YWV, func=AF.Copy), reads=[], writes=[R_ps[b2], R_ywt])
            yield
            P1V, O1V, O2V, SUV = v4(b1), v4(b3), v4(b2), v4(b0)
            for h in range(4):
                P.op("pe", lambda e, h=h: e.matmul(P1V[:, h, :], YWT[:, h, :], SBF[:, c4 + h, :], start=True, stop=True), reads=[R_ywt, R_sbf[d_]], writes=[R_ps[b1]])
            for h in range(4):
                P.op("pe", lambda e, h=h: e.matmul(O1V[:, h, :], QT[:, h, ns], SBF[:, c4 + h, :], start=True, stop=True), reads=[R_qt[h], R_sbf[d_]], writes=[R_ps[b3]])
            yield
            for h in range(4):
                P.op("dve", lambda e, h=h: e.scalar_tensor_tensor(out=VNEW[:, h, :], in0=P1V[:, h, :], scalar=nbet[:, n, c4 + h:c4 + h + 1], in1=BYV[:, h, :],
                                                                  op0=ALU.mult, op1=ALU.add),
                     reads=[R_g, R_byv], writes=[R_ps[b1], R_vnew])
            yield
            for h in range(4):
                P.op("pe", lambda e, h=h: e.matmul(O2V[:, h, :], QKT[:, h, :], VNEW[:, h, :], start=True, stop=True), reads=[R_qkt, R_vnew], writes=[R_ps[b2]])
            for h in range(4):
                P.op("pe", lambda e, h=h: e.matmul(SUV[:, h, :], KDEC[:, h, :], VNEW[:, h, :], start=True, stop=True), reads=[R_kdec, R_vnew], writes=[R_ps[b0]])
            yield
            for h in range(4):
                P.op("dve", lambda e, h=h: e.scalar_tensor_tensor(out=SST[:, c4 + h, :], in0=SST[:, c4 + h, :], scalar=exps[:, n, 16 + c4 + h:16 + c4 + h + 1], in1=SUV[:, h, :],
                                                                  op0=ALU.mult, op1=ALU.add),
                     reads=[R_exps], writes=[R_ps[b0], R_s[d_]])
            P.op("act", lambda e: e.activation(out=SBF[:, c4:c4 + 4, :], in_=SST[:, c4:c4 + 4, :], func=AF.Copy), reads=[R_s[d_]], writes=[R_sbf[d_]])
            yield
            for h in range(4):
                P.op("dve", lambda e, h=h: e.scalar_tensor_tensor(out=OACC[:, n, h, :], in0=O1V[:, h, :], scalar=exps[:, n, c4 + h:c4 + h + 1], in1=OACC[:, n, h, :],
                                                                  op0=ALU.mult, op1=ALU.add),
                     reads=[R_exps], writes=[R_ps[b3], R_oacc[n]])
            P.op("dve", lambda e: e.tensor_tensor(out=OACC[:, n, :, :], in0=O2V, in1=OACC[:, n, :, :], op=ALU.add), reads=[], writes=[R_ps[b2], R_oacc[n]])
            yield

        for i in range(NTC):
            gens = [group(i, 0, ctxs[0]), group(NTC - 1 - i, 1, ctxs[1])]
            while gens:
                for g_ in list(gens):
                    try:
                        next(g_)
                    except StopIteration:
                        gens.remove(g_)
        P.barrier()

        OT = view(OFF_B, [128, 4, T], BF16)
        TMPO = view(OFF_A + 32 * K, [128, 4, 128], F32)
        OGB = view(OFF_A + 34 * K, [128, 512], BF16)
        R_ot = [R() for _ in range(NTC)]
        R_tmpo = R()
        R_ogb = R()
        for n in range(NTC):
            P.op("dve", lambda e, n=n: e.tensor_tensor(out=TMPO[:, :, :], in0=OACC[:, n, :, :], in1=OACC[:, n, :, :], op=ALU.mult), reads=[R_oacc[n]], writes=[R_tmpo])
            P.op("dve", lambda e: e.tensor_reduce(out=small[:, 40:44], in_=TMPO[:, :, :], axis=AX.X, op=ALU.add), reads=[R_tmpo], writes=[R_small])
            P.op("act", lambda e: e.activation(out=small[:, 44:48], in_=small[:, 40:44], func=AF.Sqrt, bias=epsc[:, :], scale=1.0 / 128), reads=[R_c], writes=[R_small])
            P.op("dve", lambda e: e.reciprocal(out=small[:, 48:52], in_=small[:, 44:48]), reads=[], writes=[R_small])
            P.op("dve", lambda e, n=n: e.tensor_tensor(out=TMPO[:, :, :], in0=OACC[:, n, :, :], in1=small[:, 48:52].unsqueeze(2).to_broadcast([128, 4, 128]), op=ALU.mult),
                 reads=[R_oacc[n], R_small], writes=[R_tmpo])
            P.op("dve", lambda e, n=n: e.tensor_tensor(out=OGB[:, :], in0=TMPO[:, :, :].rearrange("p h d -> p (h d)"), in1=ZS[:, n, :], op=ALU.mult),
                 reads=[R_tmpo, R_zs[n]], writes=[R_ogb])
            pb = n % 2
            for h in range(4):
                P.op("pe", lambda e, h=h, pb=pb: e.transpose(psb(pb, BF16)[:, h * 128:(h + 1) * 128], OGB[:, h * 128:(h + 1) * 128], identb[:, :]),
                     reads=[R_ogb, R_c], writes=[R_ps[pb]])
            P.op("act", lambda e, n=n, pb=pb: e.activation(out=OT[:, :, n * 128:(n + 1) * 128], in_=psb(pb, BF16)[:, 0:512].rearrange("p (a b) -> p a b", b=128), func=AF.Copy),
                 reads=[], writes=[R_ps[pb], R_ot[n]])
        P.barrier()
        if DEBUG:
            dd = nc.dram_tensor("d_ot", [128, 4 * T], BF16, kind="ExternalOutput").ap()
            out_toks.append(P.dma("sp", ds_dbg, lambda e, dd=dd: e.dma_start(out=dd, in_=OT[:, :, :].rearrange("p a b -> p (a b)"))))

        WOUT = view(OFF_B + 16 * K, [128, 8, D], BF16)
        HACC = view(OFF_A, [128, NTC, D], F32)
        XN2 = view(OFF_C, [128, NTC, D], BF16)
        NWBC2 = view(OFF_D, [128, D], F32)
        XN2F = view(OFF_D + 4 * K, [128, D], F32)
        XN2T = view(OFF_D + 8 * K, [128, 8, 128], F32)
        R_wout = R()
        R_hacc = [R() for _ in range(NTC)]
        R_xn2 = [R() for _ in range(NTC)]
        R_nw2 = R()
        R_xn2f = R()
        R_xn2t = R()
        P.dma("pool", ds_w[2], lambda e: e.dma_start(out=WOUT[:, :, :], in_=wout_d.rearrange("(c p) n -> p c n", p=128)), writes=[R_wout])
        P.dma("sp", ds_c, lambda e: e.dma_start(out=NWBC2[:, :], in_=nw_d[1]), writes=[R_nw2])
        XN2Fs = [XN2F, view(OFF_F + 8 * K, [128, D], F32)]
        XN2Ts = [XN2T, view(OFF_F + 12 * K, [128, 8, 128], F32)]
        R_xn2fs = [R_xn2f, R()]
        R_xn2ts = [R_xn2t, R()]

        scr_toks = []
        R_sma = [R(), R()]
        R_smb = R()
        def s3_a(tc):
            b = tc % 2
            ts_ = slice(tc * 128, (tc + 1) * 128)
            XF, R_xf = XN2Fs[b], R_xn2fs[b]
            XTt, R_xt2 = XN2Ts[b], R_xn2ts[b]
            P.dma("sp", ds_x[b], lambda e: e.dma_start(out=XT[b][:, :], in_=x_d[tc * 128:(tc + 1) * 128, :]), writes=[R_xt[b]])
            for half in range(2):
                pb = 2 * (tc % 2) + half
                for cc in range(8):
                    lh = OT[:, cc, ts_] if cc < 4 else OPT[:, cc - 4, ts_]
                    rr = R_ot[tc] if cc < 4 else R_opt[cc - 4]
                    P.op("pe", lambda e, cc=cc, half=half, pb=pb, lh=lh: e.matmul(psb(pb), lh, WOUT[:, cc, half * 512:(half + 1) * 512], start=(cc == 0), stop=(cc == 7)),
                         reads=[rr, R_wout], writes=[R_ps[pb]])
                P.op("dve", lambda e, half=half, pb=pb: e.tensor_tensor(out=HACC[:, tc, half * 512:(half + 1) * 512], in0=psb(pb), in1=XT[b][:, half * 512:(half + 1) * 512], op=ALU.add),
                     reads=[R_xt[b]], writes=[R_ps[pb], R_hacc[tc]])

        def s3_n(tc):
            if not MOE:
                return
            b = tc % 2
            XF, R_xf = XN2Fs[b], R_xn2fs[b]
            XTt, R_xt2 = XN2Ts[b], R_xn2ts[b]
            sc = 56 + 3 * b
            P.op("act", lambda e: e.activation(out=XF[:, :], in_=HACC[:, tc, :], func=AF.Square, accum_out=small[:, sc:sc + 1]),
                 reads=[R_hacc[tc]], writes=[R_xf, R_sma[b]])
            P.op("act", lambda e: e.activation(out=small[:, sc + 1:sc + 2], in_=small[:, sc:sc + 1], func=AF.Sqrt, bias=epsc[:, :], scale=1.0 / D), reads=[R_c], writes=[R_sma[b]])
            P.op("dve", lambda e: e.reciprocal(out=small[:, sc + 2:sc + 3], in_=small[:, sc + 1:sc + 2]), reads=[], writes=[R_sma[b]])
            P.op("dve", lambda e: e.scalar_tensor_tensor(out=XF[:, :], in0=HACC[:, tc, :], scalar=small[:, sc + 2:sc + 3], in1=NWBC2[:, :], op0=ALU.mult, op1=ALU.mult),
                 reads=[R_hacc[tc], R_sma[b], R_nw2], writes=[R_xf])
            P.op("pool", lambda e: e.tensor_copy(out=XN2[:, tc, :], in_=XF[:, :]), reads=[R_xf], writes=[R_xn2[tc]])
            scr_toks.append(P.dma("sp", ds_s[b], lambda e: e.dma_start(out=xn2_d[tc * 128:(tc + 1) * 128, :], in_=XN2[:, tc, :]), reads=[R_xn2[tc]]))
            for dc in range(8):
                pb = 4 + dc // 4
                P.op("pe", lambda e, dc=dc, pb=pb: e.transpose(psb(pb)[:, (dc % 4) * 128:(dc % 4 + 1) * 128], XF[:, dc * 128:(dc + 1) * 128], ident[:, :]),
                     reads=[R_xf, R_c], writes=[R_ps[pb]])
            for hb in range(2):
                P.op("act", lambda e, hb=hb: e.activation(out=XTt[:, hb * 4:(hb + 1) * 4, :], in_=psb(4 + hb).rearrange("p (a b) -> p a b", b=128), func=AF.Copy),
                     reads=[], writes=[R_ps[4 + hb], R_xt2])

        def s3_b(tc):
            if not MOE:
                return
            b = tc % 2
            XTt, R_xt2 = XN2Ts[b], R_xn2ts[b]
            LG = psb(6 + b)[:, 0:16]
            for dc in range(8):
                P.op("pe", lambda e, dc=dc: e.matmul(LG, XTt[:, dc, :], rws[:, dc, :], start=(dc == 0), stop=(dc == 7)), reads=[R_xt2, R_c], writes=[R_ps[6 + b]])
            P.op("dve", lambda e: e.tensor_reduce(out=small[:, 52:53], in_=LG, axis=AX.X, op=ALU.max), reads=[], writes=[R_ps[6 + b], R_smb])
            P.op("dve", lambda e: e.tensor_scalar(out=small[:, 53:54], in0=small[:, 52:53], scalar1=-1.0, scalar2=None, op0=ALU.mult), reads=[], writes=[R_smb])
            P.op("act", lambda e: e.activation(out=prob[:, tc, :], in_=LG, func=AF.Exp, bias=small[:, 53:54], scale=1.0, accum_out=small[:, 54:55]),
                 reads=[R_smb], writes=[R_ps[6 + b], R_prob, R_smb])
            P.op("dve", lambda e: e.reciprocal(out=small[:, 55:56], in_=small[:, 54:55]), reads=[], writes=[R_smb])
            P.op("dve", lambda e: e.tensor_scalar(out=prob[:, tc, :], in0=prob[:, tc, :], scalar1=small[:, 55:56], scalar2=None, op0=ALU.mult), reads=[R_smb], writes=[R_prob])

        s3_a(0)
        for tc in range(NTC + 1):
            if tc + 1 < NTC:
                s3_a(tc + 1)
            if tc < NTC:
                s3_n(tc)
            if tc >= 1:
                s3_b(tc - 1)
        P.barrier()
        if MOE:
            PF = view(OFF_B, [128, T], F32)
            WORK = view(OFF_B + 8 * K, [128, T], F32)
            SELT = view(OFF_B + 16 * K, [128, T], F32)
            M8 = view(OFF_B + 24 * K, [128, 8], F32)
            TH = view(OFF_B + 24 * K + 64, [128, 1], F32)
            R_pf, R_work, R_selt, R_m8, R_th = R(), R(), R(), R(), R()
            slot_off = [OFF_F, OFF_F + 12 * K, OFF_E + 4 * K, OFF_B + 20 * K]
            NSLOT = len(slot_off)
            WGs = [view(o_, [128, 8, 256], BF16) for o_ in slot_off]
            WUs = [view(o_ + 4 * K, [128, 8, 256], BF16) for o_ in slot_off]
            WDs = [view(o_ + 8 * K, [128, 2, D], BF16) for o_ in slot_off]
            R_wg = [R() for _ in range(NSLOT)]
            R_wu = [R() for _ in range(NSLOT)]
            R_wd = [R() for _ in range(NSLOT)]

            def load_piece(pi):
                e_, pc = pi // 8, pi % 8
                sl_ = pi % NSLOT
                f0 = pc * 256
                P.dma("pool", ds_w[3 * sl_], lambda e, e_=e_, sl_=sl_, f0=f0: e.dma_start(out=WGs[sl_][:, :, :], in_=wg_d[e_].rearrange("(c p) f -> p c f", p=128)[:, :, f0:f0 + 256]),
                      writes=[R_wg[sl_]])
                P.dma("pool", ds_w[3 * sl_ + 1], lambda e, e_=e_, sl_=sl_, f0=f0: e.dma_start(out=WUs[sl_][:, :, :], in_=wu_d[e_].rearrange("(c p) f -> p c f", p=128)[:, :, f0:f0 + 256]),
                      writes=[R_wu[sl_]])
                P.dma("pool", ds_w[3 * sl_ + 2], lambda e, e_=e_, sl_=sl_, f0=f0: e.dma_start(out=WDs[sl_][:, :, :], in_=wd_d[e_, f0:f0 + 256, :].rearrange("(c p) d -> p c d", p=128)),
                      writes=[R_wd[sl_]])

            for pi in range(3):
                load_piece(pi)

            for tc in range(NTC):
                b = tc // 4
                P.op("pe", lambda e, tc=tc, b=b: e.transpose(psb(b)[0:16, (tc % 4) * 128:(tc % 4 + 1) * 128], prob[:, tc, :], ident[:, :]),
                     reads=[R_prob, R_c], writes=[R_ps[b]])
            for b in range(4):
                P.op("act", lambda e, b=b: e.activation(out=PF[0:16, b * 512:(b + 1) * 512], in_=psb(b)[0:16, :], func=AF.Copy), reads=[], writes=[R_ps[b], R_pf])
                P.op("dve", lambda e, b=b: e.tensor_copy(out=WORK[0:16, b * 512:(b + 1) * 512], in_=psb(b)[0:16, :]), reads=[], writes=[R_ps[b], R_work])
            for rnd in range(CAP // 8):
                P.op("dve", lambda e: e.max(out=M8[0:16, :], in_=WORK[0:16, :]), reads=[R_work], writes=[R_m8])
                if rnd < CAP // 8 - 1:
                    P.op("dve", lambda e: e.match_replace(out=WORK[0:16, :], in_to_replace=M8[0:16, :], in_values=WORK[0:16, :], imm_value=-1.0),
                         reads=[R_m8], writes=[R_work])
            P.op("dve", lambda e: e.tensor_reduce(out=TH[0:16, :], in_=M8[0:16, :], axis=AX.X, op=ALU.min), reads=[R_m8], writes=[R_th])
            P.op("dve", lambda e: e.tensor_scalar(out=SELT[0:16, :], in0=PF[0:16, :], scalar1=TH[0:16, 0:1], scalar2=None, op0=ALU.is_ge),
                 reads=[R_pf, R_th], writes=[R_selt])
            SV = psb(4)[:, 0:256].rearrange("p (a b) -> p a b", b=16)
            for tc in range(NTC):
                P.op("pe", lambda e, tc=tc: e.transpose(SV[:, tc, :], SELT[0:16, tc * 128:(tc + 1) * 128], ident[0:16, 0:16]),
                     reads=[R_selt, R_c], writes=[R_ps[4]])
            P.op("act", lambda e: e.activation(out=sel[:, :, :], in_=SV, func=AF.Copy), reads=[], writes=[R_ps[4], R_sel])
            P.op("dve", lambda e: e.tensor_copy(out=selb[:, :, :], in_=SV), reads=[], writes=[R_ps[4], R_sel])
            PV = psb(5)[:, 0:256].rearrange("p (a b) -> p a b", b=16)
            for tc in range(NTC):
                mms = [(onesb, t2) for t2 in range(tc)] + [(ltb, tc)]
                for i_, (lh, t2) in enumerate(mms):
                    P.op("pe", lambda e, tc=tc, lh=lh, t2=t2, i_=i_, nmm=len(mms): e.matmul(PV[:, tc, :], lh[:, :], selb[:, t2, :], start=(i_ == 0), stop=(i_ == nmm - 1)),
                         reads=[R_sel, R_c], writes=[R_ps[5]])
            P.op("dve", lambda e: e.scalar_tensor_tensor(out=posm[:, :, :], in0=PV, scalar=1.0, in1=sel[:, :, :], op0=ALU.add, op1=ALU.mult),
                 reads=[R_sel], writes=[R_ps[5], R_posm])
            P.op("dve", lambda e: e.tensor_scalar(out=posm[:, :, :], in0=posm[:, :, :], scalar1=-1.0, scalar2=None, op0=ALU.add), reads=[], writes=[R_posm])
            P.barrier()
            if DEBUG:
                for name, tl in (("d_prob", prob), ("d_sel", sel), ("d_posm", posm)):
                    dd = nc.dram_tensor(name, [128, 256], F32, kind="ExternalOutput").ap()
                    out_toks.append(P.dma("sp", ds_dbg, lambda e, dd=dd, tl=tl: e.dma_start(out=dd, in_=tl[:, :, :].rearrange("p a b -> p (a b)"))))

            load_piece(3)
            PE_ = view(OFF_B, [128, NTC, 256], BF16)
            PTE = view(OFF_B + 8 * K, [128, 2, T], BF16)
            XG = view(OFF_D, [128, 8, 256], BF16)
            HTT = view(OFF_D + 4 * K, [128, 16, 256], BF16)
            SGs = [view(OFF_D + 12 * K + i * K, [128, 256], F32) for i in range(2)]
            TMPS = [view(OFF_B + 16 * K + i * 2 * K, [128, 512], F32) for i in range(2)]
            R_tmps = [R(), R()]
            YG = view(OFF_E, [128, 2, D], BF16)
            R_pe, R_pte, R_xg, R_yg = R(), R(), R(), R()
            XGT = view(OFF_C, [128, 2, D], BF16)
            IDXF = view(OFF_C + 4 * K, [128, 2], F32)
            IDXU = view(OFF_C + 4 * K + 64, [128, 2], U32)
            IDX4 = view(OFF_C + 4 * K + 128, [128, 4], F32)
            R_xgt, R_idx = [R(), R()], R()
            R_scr = R()
            R_scr.w = scr_toks[-1] if False else None
            R_sgs = [R(), R()]
            R_htt = [R() for _ in range(16)]
            next_piece = [NSLOT]

            def build_p1(e_, tc):
                P.op("dve", lambda e, tc=tc, e_=e_: e.tensor_scalar(out=PE_[:, tc, :], in0=iota[:, :], scalar1=posm[:, tc, e_:e_ + 1], scalar2=None, op0=ALU.is_equal),
                     reads=[R_posm, R_c], writes=[R_pe])

            def build_p(e_):
                for tc in range(NTC):
                    build_p1(e_, tc)

            def gather_idx():
                IV = psb(7)[:, 0:4]
                for jc in range(2):
                    for tc in range(NTC):
                        P.op("pe", lambda e, jc=jc, tc=tc: e.matmul(IV[:, jc * 2:jc * 2 + 2], PE_[:, tc, jc * 128:(jc + 1) * 128], tokb[:, tc, :], start=(tc == 0), stop=(tc == NTC - 1)),
                             reads=[R_pe, R_c], writes=[R_ps[7]])
                P.op("dve", lambda e: e.tensor_copy(out=IDX4[:, :], in_=IV), reads=[], writes=[R_ps[7], R_idx])
                IV3 = IDX4[:, :].rearrange("p (j k) -> p j k", k=2)
                P.op("dve", lambda e: e.scalar_tensor_tensor(out=IDXF[:, :], in0=IV3[:, :, 0], scalar=128.0, in1=IV3[:, :, 1], op0=ALU.mult, op1=ALU.add),
                     reads=[], writes=[R_idx])
                P.op("dve", lambda e: e.tensor_copy(out=IDXU[:, :], in_=IDXF[:, :]), reads=[], writes=[R_idx])
                for jc in range(2):
                    P.dma("pool", ds_g[jc], lambda e, jc=jc: e.indirect_dma_start(out=XGT[:, jc, :], out_offset=None, in_=xn2_d[:, :],
                                                                                   in_offset=bass.IndirectOffsetOnAxis(ap=IDXU[:, jc:jc + 1], axis=0)),
                          reads=[R_idx], writes=[R_xgt[jc]])

            def gather():
                for jc in range(2):
                    bank = 6 + jc
                    for dc in range(8):
                        P.op("pe", lambda e, jc=jc, dc=dc, bank=bank: e.transpose(psb(bank, BF16)[:, dc * 128:(dc + 1) * 128], XGT[:, jc, dc * 128:(dc + 1) * 128], identb[:, :]),
                             reads=[R_xgt[jc], R_c], writes=[R_ps[bank]])
                    P.op("act", lambda e, jc=jc, bank=bank: e.activation(out=XG[:, :, jc * 128:(jc + 1) * 128], in_=psb(bank, BF16)[:, 0:1024].rearrange("p (a b) -> p a b", b=128), func=AF.Copy),
                         reads=[], writes=[R_ps[bank], R_xg])

            def transpose_p():
                for jc in range(2):
                    for th in range(2):
                        bank = 4 + (jc * 2 + th) % 2
                        for t8 in range(8):
                            tc = th * 8 + t8
                            P.op("pe", lambda e, jc=jc, tc=tc, t8=t8, bank=bank: e.transpose(psb(bank, BF16)[:, t8 * 128:(t8 + 1) * 128], PE_[:, tc, jc * 128:(jc + 1) * 128], identb[:, :]),
                                 reads=[R_pe, R_c], writes=[R_ps[bank]])
                        P.op("act", lambda e, jc=jc, th=th, bank=bank: e.activation(out=PTE[:, jc, th * 1024:(th + 1) * 1024], in_=psb(bank, BF16)[:, 0:1024], func=AF.Copy),
                             reads=[], writes=[R_ps[bank], R_pte])

            def down(e_, fc):
                sl_ = (e_ * 8 + fc // 2) % NSLOT
                sub = fc % 2
                for jc in range(2):
                    for half in range(2):
                        yb = jc * 2 + half
                        P.op("pe", lambda e, fc=fc, jc=jc, half=half, yb=yb, sl_=sl_, sub=sub: e.matmul(psb(yb), HTT[:, fc, jc * 128:(jc + 1) * 128], WDs[sl_][:, sub, half * 512:(half + 1) * 512],
                                                                                                    start=(fc == 0), stop=(fc == 15)),
                             reads=[R_htt[fc], R_wd[sl_]], writes=[R_ps[yb]])
                if sub == 1:
                    if next_piece[0] < E * 8:
                        load_piece(next_piece[0])
                        next_piece[0] += 1

            def ffn(e_):
                for fc in range(16):
                    sl_ = (e_ * 8 + fc // 2) % NSLOT
                    sub = fc % 2
                    hb = 4 + fc % 2
                    for wi_, (W, RW) in enumerate(((WGs, R_wg), (WUs, R_wu))):
                        for dc in range(8):
                            P.op("pe", lambda e, wi_=wi_, W=W, dc=dc, hb=hb, sl_=sl_, sub=sub: e.matmul(psb(hb)[:, wi_ * 256:(wi_ + 1) * 256], W[sl_][:, dc, sub * 128:(sub + 1) * 128], XG[:, dc, :],
                                                                                                    start=(dc == 0), stop=(dc == 7)),
                                 reads=[RW[sl_], R_xg], writes=[R_ps[hb]])
                    sgi = fc % 2
                    P.op("act", lambda e, hb=hb, sgi=sgi: e.activation(out=SGs[sgi][:, :], in_=psb(hb)[:, 0:256], func=AF.Silu), reads=[], writes=[R_ps[hb], R_sgs[sgi]])
                    P.op("dve", lambda e, hb=hb, fc=fc, sgi=sgi: e.tensor_tensor(out=HTT[:, fc, :], in0=SGs[sgi][:, :], in1=psb(hb)[:, 256:512], op=ALU.mult),
                         reads=[R_sgs[sgi]], writes=[R_ps[hb], R_htt[fc]])
                    if e_ + 1 < E:
                        build_p1(e_ + 1, fc)
                    if fc >= 2:
                        down(e_, fc - 2)
                down(e_, 14)
                down(e_, 15)
                for jc in range(2):
                    for half in range(2):
                        yb = jc * 2 + half
                        P.op("act", lambda e, jc=jc, half=half, yb=yb: e.activation(out=YG[:, jc, half * 512:(half + 1) * 512], in_=psb(yb), func=AF.Copy),
                             reads=[], writes=[R_ps[yb], R_yg])

            def scatter(e_):
                sbanks = (6, 7, 0, 1, 2, 3)
                for tc in range(NTC):
                    for half in range(2):
                        i_ = tc * 2 + half
                        bank = sbanks[i_ % len(sbanks)]
                        for jc in range(2):
                            P.op("pe", lambda e, tc=tc, half=half, jc=jc, bank=bank: e.matmul(psb(bank), PTE[:, jc, tc * 128:(tc + 1) * 128], YG[:, jc, half * 512:(half + 1) * 512],
                                                                                            start=(jc == 0), stop=(jc == 1)),
                                 reads=[R_pte, R_yg], writes=[R_ps[bank]])
                        if i_ % 2 == 0:
                            P.op("dve", lambda e, tc=tc, half=half, bank=bank, e_=e_: e.scalar_tensor_tensor(out=HACC[:, tc, half * 512:(half + 1) * 512], in0=psb(bank), scalar=prob[:, tc, e_:e_ + 1],
                                                                                                         in1=HACC[:, tc, half * 512:(half + 1) * 512], op0=ALU.mult, op1=ALU.add),
                                 reads=[R_prob], writes=[R_ps[bank], R_hacc[tc]])
                        else:
                            ti = (i_ // 2) % 2
                            P.op("act", lambda e, tc=tc, bank=bank, e_=e_, ti=ti: e.activation(out=TMPS[ti][:, :], in_=psb(bank), func=AF.Identity, scale=prob[:, tc, e_:e_ + 1]),
                                 reads=[R_prob], writes=[R_ps[bank], R_tmps[ti]])
                            P.op("pool", lambda e, tc=tc, half=half, ti=ti: e.tensor_tensor(out=HACC[:, tc, half * 512:(half + 1) * 512], in0=HACC[:, tc, half * 512:(half + 1) * 512],
                                                                                        in1=TMPS[ti][:, :], op=ALU.add),
                                 reads=[R_tmps[ti]], writes=[R_hacc[tc]])

            P.wait_all("pool", scr_toks)
            build_p(0)
            gather_idx()
            gather()
            for e_ in range(E):
                transpose_p()
                ffn(e_)
                if e_ + 1 < E:
                    gather_idx()
                scatter(e_)
                if e_ + 1 < E:
                    gather()
            P.barrier()
        NWF = view(OFF_D, [128, D], F32)
        OTL = [view(OFF_E + i * 4 * K, [128, D], F32) for i in range(2)]
        R_nwf = R()
        R_otl = [R(), R()]
        P.dma("sp", ds_c, lambda e: e.dma_start(out=NWF[:, :], in_=nw_d[2]), writes=[R_nwf])
        for tc in range(NTC):
            ob = tc % 2
            P.op("act", lambda e, ob=ob, tc=tc: e.activation(out=OTL[ob][:, :], in_=HACC[:, tc, :], func=AF.Square, accum_out=small[:, 8:9]),
                 reads=[R_hacc[tc]], writes=[R_otl[ob], R_small])
            P.op("act", lambda e: e.activation(out=small[:, 9:10], in_=small[:, 8:9], func=AF.Sqrt, bias=epsc[:, :], scale=1.0 / D),
                 reads=[R_c], writes=[R_small])
            P.op("dve", lambda e: e.reciprocal(out=small[:, 10:11], in_=small[:, 9:10]), reads=[], writes=[R_small])
            P.op("dve", lambda e, ob=ob, tc=tc: e.scalar_tensor_tensor(out=OTL[ob][:, :], in0=HACC[:, tc, :], scalar=small[:, 10:11], in1=NWF[:, :], op0=ALU.mult, op1=ALU.mult),
                 reads=[R_hacc[tc], R_small, R_nwf], writes=[R_otl[ob]])
            out_toks.append(P.dma("sp", ds_o[ob], lambda e, tc=tc, ob=ob: e.dma_start(out=out_d[tc * 128:(tc + 1) * 128, :], in_=OTL[ob][:, :]), reads=[R_otl[ob]]))
        P.wait_all("sp", out_toks)

        with nc.Block() as block:
            @block.tensor
            def _(e):
                for f in P.q["pe"]:
                    f(e)

            @block.scalar
            def _(e):
                for f in P.q["act"]:
                    f(e)

            @block.vector
            def _(e):
                for f in P.q["dve"]:
                    f(e)

            @block.gpsimd
            def _(e):
                for f in P.q["pool"]:
                    f(e)

            @block.sync
            def _(e):
                for f in P.q["sp"]:
                    f(e)
    return nc


DEBUG = False
MOE = True


def make_in_maps(inputs):
    c = host_consts()
    f = lambda a: np.ascontiguousarray(np.asarray(a, dtype=np.float32))
    x = f(inputs["x"])
    shared = {
        "w_in": f(inputs["w_in"][0]),
        "conv_wP": f(np.asarray(inputs["conv_w"][0]).T.reshape(12, 128, 5).transpose(1, 0, 2).reshape(128, 60)),
        "abp": f(np.tile(np.concatenate([np.asarray(inputs["a_log_fwd"][0]), np.asarray(inputs["a_log_bwd"][0]),
                                         np.asarray(inputs["dt_bias_fwd"][0]), np.asarray(inputs["dt_bias_bwd"][0])])[None, :], (128, 1))),
        "nw0": f(np.tile(np.asarray(inputs["norm_mix_w"][0])[None, :], (128, 1))),
        "nw1": f(np.tile(np.asarray(inputs["norm_ffn_w"][0])[None, :], (128, 1))),
        "nw2": f(np.tile(np.asarray(inputs["norm_final_w"])[None, :], (128, 1))),
        "hnw": f(np.tile(np.asarray(inputs["head_norm_w"][0])[None, :], (128, 1))),
        "pool_wP": f(np.asarray(inputs["pool_w"][0]).transpose(1, 0, 2).reshape(128, 512)),
        "pool_scT": f(np.asarray(inputs["pool_scale"][0]).reshape(4, 128).T),
        "w_out": f(inputs["w_out"][0]),
        "router_wP": f(np.asarray(inputs["router_w"][0]).reshape(8, 128, 16).transpose(1, 0, 2).reshape(128, 128)),
        "wg": f(inputs["expert_w_gate"][0]),
        "wu": f(inputs["expert_w_up"][0]),
        "wd": f(inputs["expert_w_down"][0]),
        "c_iota": c["iota"],
        "c_tok": c["tok"],
        "c_invc": c["invc"],
    }
    for n in CONST_NAMES:
        shared["c_" + n] = c[n]
    maps = []
    for b in range(8):
        m = dict(shared)
        m["x"] = np.ascontiguousarray(x[b])
        maps.append(m)
    return maps


def kernel(**inputs):
    nc = build_nc()
    in_maps = make_in_maps(inputs)
    res = run_bass_kernel_spmd(nc, in_maps, core_ids=list(range(8)))
    out = np.stack([np.asarray(r["out"], dtype=np.float32) for r in res.results], axis=0)
    return out
```

```python
import numpy as np
from contextlib import ExitStack
import concourse.bass as bass
import concourse.mybir as mybir
from concourse.bass_utils import run_bass_kernel_spmd

F32 = mybir.dt.float32
BF16 = mybir.dt.bfloat16
U32 = mybir.dt.uint32
F32R = mybir.dt.float32r
ALU = mybir.AluOpType
AF = mybir.ActivationFunctionType
AX = mybir.AxisListType

T = 2048
D = 1024
NTC = 16
H = 4
E = 16
CAP = 256
FF = 2048
INC = 2576
EPS = 1e-6
BIG = 30000.0
ENGS = ("pe", "act", "dve", "pool", "sp")


class Tk:
    __slots__ = ("sem", "val", "snap")

    def __init__(s, sem, val, snap):
        s.sem = sem
        s.val = val
        s.snap = snap


class R:
    __slots__ = ("w", "rs")

    def __init__(s):
        s.w = None
        s.rs = {}


class DS:
    def __init__(s, sem):
        s.sem = sem
        s.count = 0


class Prog:
    def __init__(s, psem):
        s.q = {e: [] for e in ENGS}
        s.cnt = {e: 0 for e in ENGS}
        s.vc = {e: {} for e in ENGS}
        s.psem = psem
        s.dma_toks = []

    def _waits(s, eng, reads, writes):
        vc = s.vc[eng]
        need = {}
        toks = []
        for r in reads:
            if r.w is not None:
                toks.append(r.w)
        for r in writes:
            if r.w is not None:
                toks.append(r.w)
            toks.extend(r.rs.values())
        for t in toks:
            if eng == "pe" and t.sem is s.psem["pe"]:
                continue
            k = id(t.sem)
            if vc.get(k, 0) >= t.val:
                continue
            if k not in need or need[k].val < t.val:
                need[k] = t
        return list(need.values())

    def _absorb(s, eng, waits):
        vc = s.vc[eng]
        for t in waits:
            for k, v in t.snap.items():
                if vc.get(k, 0) < v:
                    vc[k] = v
            k = id(t.sem)
            if vc.get(k, 0) < t.val:
                vc[k] = t.val

    def op(s, eng, fn, reads=(), writes=()):
        waits = s._waits(eng, reads, writes)
        s._absorb(eng, waits)
        s.cnt[eng] += 1
        sem = s.psem[eng]
        tok = Tk(sem, s.cnt[eng], dict(s.vc[eng]))
        wl = [(t.sem, t.val) for t in waits]

        def emit(e):
            for sm, v in wl:
                e.wait_ge(sm, v)
            fn(e).then_inc(sem, 1)

        s.q[eng].append(emit)
        for r in reads:
            r.rs[id(sem)] = tok
        for r in writes:
            r.w = tok
            r.rs = {}
        return tok

    def dma(s, eng, ds, fn, reads=(), writes=()):
        waits = s._waits(eng, reads, writes)
        s._absorb(eng, waits)
        ds.count += 16
        tok = Tk(ds.sem, ds.count, dict(s.vc[eng]))
        wl = [(t.sem, t.val) for t in waits]
        sem = ds.sem

        def emit(e):
            for sm, v in wl:
                e.wait_ge(sm, v)
            fn(e).then_inc(sem, 16)

        s.q[eng].append(emit)
        for r in reads:
            r.rs[id(sem)] = tok
        for r in writes:
            r.w = tok
            r.rs = {}
        s.dma_toks.append(tok)
        return tok

    def barrier(s):
        toks = [Tk(s.psem[e], s.cnt[e], dict(s.vc[e])) for e in ENGS if s.cnt[e] > 0 and e != "sp"]
        toks += s.dma_toks
        s.dma_toks = []
        for eng in ENGS:
            vc = s.vc[eng]
            need = {}
            for t in toks:
                k = id(t.sem)
                if vc.get(k, 0) >= t.val:
                    continue
                if k not in need or need[k].val < t.val:
                    need[k] = t
            waits = list(need.values())
            s._absorb(eng, waits)
            wl = [(t.sem, t.val) for t in waits]
            if wl:
                def emit(e, wl=wl):
                    for sm, v in wl:
                        e.wait_ge(sm, v)
                s.q[eng].append(emit)

    def wait_all(s, eng, toks):
        wl = [(t.sem, t.val) for t in toks]

        def emit(e):
            for sm, v in wl:
                e.wait_ge(sm, v)
        s.q[eng].append(emit)


def host_consts():
    p = np.arange(128)[:, None]
    f = np.arange(128)[None, :]
    c = {}
    c["ident"] = (p == f).astype(np.float32)
    c["ut_f"] = (p <= f).astype(np.float32)
    c["ut_b"] = (p >= f).astype(np.float32)
    c["sl_f"] = (p > f).astype(np.float32)
    c["sl_b"] = (p < f).astype(np.float32)
    c["neg_f"] = (-BIG * (f < p)).astype(np.float32)
    c["neg_b"] = (-BIG * (f > p)).astype(np.float32)
    c["str_f"] = (f > p).astype(np.float32)
    c["str_b"] = (f < p).astype(np.float32)
    c["ones"] = np.ones((128, 128), np.float32)
    c["iota"] = np.tile(np.arange(256, dtype=np.float32)[None, :], (128, 1))
    tok = np.zeros((128, 16, 2), np.float32)
    tok[:, :, 0] = np.arange(16, dtype=np.float32)[None, :]
    tok[:, :, 1] = np.arange(128, dtype=np.float32)[:, None]
    c["tok"] = tok.reshape(128, 32)
    invc = np.zeros((128, 4, 16), np.float32)
    for g, w in enumerate((2, 4, 8, 16)):
        lo = w // 2
        hi = w - lo - 1
        for t in range(lo):
            invc[:, g, t] = 1.0 / (t + hi + 1)
        for i in range(hi):
            t = T - hi + i
            invc[:, g, 8 + i] = 1.0 / (T - t + lo)
    c["invc"] = invc.reshape(128, 64)
    return c


CONST_NAMES = ["ident", "ut_f", "ut_b", "sl_f", "sl_b", "neg_f", "neg_b", "str_f", "str_b", "ones"]


def build_nc():
    nc = bass.Bass("TRN2", target_bir_lowering=False)

    def din(name, shape, dt=F32):
        return nc.dram_tensor(name, list(shape), dt, kind="ExternalInput").ap()

    x_d = din("x", [T, D])
    win_d = din("w_in", [D, INC])
    cw_d = din("conv_wP", [128, 60])
    abp_d = din("abp", [128, 16])
    nw_d = [din("nw%d" % i, [128, D]) for i in range(3)]
    hnw_d = din("hnw", [128, 128])
    pw_d = din("pool_wP", [128, 512])
    psc_d = din("pool_scT", [128, 4])
    wout_d = din("w_out", [D, D])
    rw_d = din("router_wP", [128, 128])
    wg_d = din("wg", [E, D, FF])
    wu_d = din("wu", [E, D, FF])
    wd_d = din("wd", [E, FF, D])
    cst_d = {n: din("c_" + n, [128, 128]) for n in CONST_NAMES}
    iota_d = din("c_iota", [128, 256])
    invc_d = din("c_invc", [128, 64])
    out_d = nc.dram_tensor("out", [T, D], F32, kind="ExternalOutput").ap()
    xn2_d = nc.dram_tensor("xn2_scr", [T, D], BF16, kind="Internal").ap()
    tok_d = din("c_tok", [128, 32])

    es = ExitStack()
    with es:
        def sb(name, shape, dt):
            return es.enter_context(nc.sbuf_tensor(name, list(shape), dt))

        def pstile(name):
            return es.enter_context(nc.psum_tensor(name, [128, 512], F32))

        psem = {e: es.enter_context(nc.semaphore("ps_" + e)) for e in ENGS}
        P = Prog(psem)
        out_toks = []

        def newds(name):
            return DS(es.enter_context(nc.semaphore(name)))

        cst = {n: sb("k_" + n, [128, 128], F32) for n in CONST_NAMES}
        identb = sb("identb", [128, 128], BF16)
        onesb = sb("onesb", [128, 128], BF16)
        ltb = sb("ltb", [128, 128], BF16)
        iota = sb("iota", [128, 256], F32)
        tokf = sb("tokf", [128, 32], F32)
        tokb = sb("tokb", [128, 16, 2], BF16)
        invc = sb("invc", [128, 64], F32)
        cw = sb("cw", [128, 12, 5], F32)
        abp = sb("abp_s", [128, 16], F32)
        hnw = sb("hnw_s", [128, 128], F32)
        psc = sb("psc", [128, 4], F32)
        pwb = sb("pwb", [128, 4, 128], BF16)
        rws = sb("rws", [128, 8, 16], F32)
        epsc = sb("epsc", [128, 1], F32)
        onec = sb("onec", [128, 1], F32)
        mhalf = sb("mhalf", [128, 1], F32)
        small = sb("small", [128, 64], F32)
        gsm = sb("gsm", [128, 16, 8], F32)
        bet = sb("bet", [128, 16, 8], F32)
        nbet = sb("nbet", [128, 16, 8], F32)
        exps = sb("exps", [128, 16, 24], F32)
        prob = sb("prob", [128, 16, 16], F32)
        sel = sb("sel", [128, 16, 16], F32)
        selb = sb("selb", [128, 16, 16], BF16)
        posm = sb("posm", [128, 16, 16], F32)
        R_c = R()
        R_g = R()
        R_exps = R()
        R_prob = R()
        R_sel = R()
        R_posm = R()
        R_small = R()

        ARENA_B = 190 * 1024
        arena = sb("arena", [128, ARENA_B // 2], BF16)

        def view(off, shape, dt):
            n = 1
            for s_ in shape[1:]:
                n *= s_
            nb = n * (4 if dt in (F32, U32) else 2)
            assert off % 4 == 0 and off + nb <= ARENA_B, (off, nb)
            v = arena[:, off // 2:(off + nb) // 2]
            if dt in (F32, U32):
                v = v.bitcast(dt)
            if len(shape) == 2:
                return v
            if len(shape) == 3:
                return v.rearrange("p (a b) -> p a b", b=shape[2])
            if len(shape) == 4:
                return v.rearrange("p (a b c) -> p a b c", b=shape[2], c=shape[3])
            raise ValueError

        K = 1024
        OFF_A, OFF_B, OFF_C, OFF_D, OFF_E, OFF_F = 0, 64 * K, 96 * K, 128 * K, 144 * K, 160 * K

        ps = [pstile("ps%d" % i) for i in range(8)]
        R_ps = [R() for _ in range(8)]

        def psb(i, dt=F32):
            return ps[i][:, :] if dt == F32 else ps[i][:, :].bitcast(BF16)

        ds_c = newds("ds_c")
        ds_cp = newds("ds_cp")
        ds_x = [newds("ds_x0"), newds("ds_x1")]
        ds_x4 = ds_x + [newds("ds_x2"), newds("ds_x3")]
        ds_w = [newds("ds_w%d" % i) for i in range(12)]
        ds_o = [newds("ds_o0"), newds("ds_o1")]
        ds_s = [newds("ds_s0"), newds("ds_s1")]
        ds_g = [newds("ds_g0"), newds("ds_g1")]

        for n in CONST_NAMES:
            P.dma("sp", ds_c, lambda e, n=n: e.dma_start(out=cst[n][:, :], in_=cst_d[n]), writes=[R_c])
        P.dma("sp", ds_c, lambda e: e.dma_start(out=iota[:, :], in_=iota_d), writes=[R_c])
        P.dma("sp", ds_c, lambda e: e.dma_start(out=tokf[:, :], in_=tok_d), writes=[R_c])
        P.dma("sp", ds_c, lambda e: e.dma_start(out=invc[:, :], in_=invc_d), writes=[R_c])
        P.dma("sp", ds_c, lambda e: e.dma_start(out=cw[:, :, :].rearrange("p c j -> p (c j)"), in_=cw_d), writes=[R_c])
        P.dma("sp", ds_c, lambda e: e.dma_start(out=abp[:, :], in_=abp_d), writes=[R_c])
        P.dma("sp", ds_c, lambda e: e.dma_start(out=hnw[:, :], in_=hnw_d), writes=[R_c])
        P.dma("sp", ds_c, lambda e: e.dma_start(out=psc[:, :], in_=psc_d), writes=[R_c])
        P.dma("sp", ds_c, lambda e: e.dma_start(out=rws[:, :, :].rearrange("p c e -> p (c e)"), in_=rw_d), writes=[R_c])
        P.dma("pool", ds_cp, lambda e: e.dma_start(out=pwb[:, :, :].rearrange("p g d -> p (g d)"), in_=pw_d), writes=[R_c])
        P.op("pool", lambda e: e.tensor_copy(out=identb[:, :], in_=cst["ident"][:, :]), reads=[R_c], writes=[R_c])
        P.op("pool", lambda e: e.tensor_copy(out=onesb[:, :], in_=cst["ones"][:, :]), reads=[R_c], writes=[R_c])
        P.op("pool", lambda e: e.tensor_copy(out=ltb[:, :], in_=cst["sl_b"][:, :]), reads=[R_c], writes=[R_c])
        P.op("pool", lambda e: e.tensor_copy(out=tokb[:, :, :].rearrange("p a b -> p (a b)"), in_=tokf[:, :]), reads=[R_c], writes=[R_c])
        P.op("pool", lambda e: e.memset(epsc[:, :], EPS), writes=[R_c])
        P.op("pool", lambda e: e.memset(onec[:, :], 1.0), writes=[R_c])
        P.op("pool", lambda e: e.memset(mhalf[:, :], -0.5), writes=[R_c])
        P.op("act", lambda e: e.activation(out=small[:, 0:8], in_=abp[:, 0:8], func=AF.Exp), reads=[R_c], writes=[R_c])
        P.op("dve", lambda e: e.tensor_scalar(out=small[:, 0:8], in0=small[:, 0:8], scalar1=-1.0, scalar2=None, op0=ALU.mult), reads=[R_c], writes=[R_c])
        P.barrier()
        RC = [R_c]

        ident = cst["ident"]

        XNT = view(OFF_A, [128, 8, T], BF16)
        WINP = [view(OFF_A + 32 * K + i * 8704, [128, 8, 528], BF16) for i in range(2)]
        RAW = view(OFF_A + 32 * K + 17408, [128, 2056], F32)
        RAWS = [RAW, view(OFF_D, [128, 2056], F32)]
        NWBC1 = view(OFF_A + 32 * K + 17408 + 8224, [128, D], F32)
        SB2 = view(OFF_A + 32 * K + 17408, [128, T + 32], F32)
        QT = view(OFF_B, [128, H, T], BF16)
        KT = view(OFF_B + 16 * K, [128, H, T], BF16)
        KTOK = view(OFF_C, [128, NTC, H, 128], BF16)
        VTOK = view(OFF_C + 16 * K, [128, NTC, H, 128], BF16)
        ZS = view(OFF_D, [128, NTC, 512], BF16)
        OPT = view(OFF_E, [128, 4, T], BF16)
        SQ = view(OFF_E, [128, T], BF16)
        VTMP = view(OFF_E, [128, T], BF16)
        RSQ = view(OFF_E + 4 * K, [128, T], F32)
        XT = [view(OFF_F + i * 4 * K, [128, D], F32) for i in range(2)]
        XT4 = XT + [view(OFF_F + 25 * K, [128, D], F32), view(OFF_D + 11 * K, [128, D], F32)]
        XN = view(OFF_F + 8 * K, [128, D], BF16)
        CONVT = view(OFF_F + 10 * K, [128, T], F32)
        CONVS = [CONVT, view(OFF_F + 21 * K, [128, T], F32)]
        UB = view(OFF_F + 10 * K, [128, T + 32], F32)
        ZF = view(OFF_F + 10 * K, [128, 512], F32)
        ABS = view(OFF_F + 19 * K, [128, 16, 16], F32)
        TM1 = view(OFF_F + 20 * K, [128, 16, 8], F32)
        PB3 = view(OFF_F + 21 * K, [128, T + 32], F32)
        DIFB = view(OFF_F, [128, T], BF16)
        R_xnt = [R() for _ in range(NTC)]
        R_xt = [R(), R()]
        R_xt4 = R_xt + [R(), R()]
        R_xn = R()
        R_winp = [R(), R()]
        R_raw = R()
        R_acc = R()
        R_raws = [R_raw, R()]
        R_accs = [R_acc, R()]
        R_nw = R()
        R_qt = [R() for _ in range(H)]
        R_kt = [R() for _ in range(H)]
        R_ktok = R()
        R_vtok = R()
        R_zs = [R() for _ in range(NTC)]
        R_opt = [R() for _ in range(4)]
        R_abs = R()
        R_pb3 = R()
        R_difb = R()

        P.dma("sp", ds_c, lambda e: e.dma_start(out=NWBC1[:, :], in_=nw_d[0]), writes=[R_nw])
        P.op("pool", lambda e: e.memset(RAW[:, :], 0.0), writes=[R_raw])
        P.op("pool", lambda e: e.memset(RAWS[1][:, :], 0.0), writes=[R_raws[1]])

        XNS = [XN, view(OFF_D + 9 * K, [128, D], BF16)]
        R_xns = [R_xn, R()]
        R_sm1 = [R(), R()]
        JUNK = view(OFF_F + 21 * K, [128, D], F32)
        R_junk = R()
        def front_1a(tc):
            b = tc % 2
            sc = 8 + 3 * b
            xb = tc % 4
            P.dma("sp", ds_x4[xb], lambda e: e.dma_start(out=XT4[xb][:, :], in_=x_d[tc * 128:(tc + 1) * 128, :]), writes=[R_xt4[xb]])
            P.op("act", lambda e: e.activation(out=JUNK[:, :], in_=XT4[xb][:, :], func=AF.Square, accum_out=small[:, sc:sc + 1]),
                 reads=[R_xt4[xb]], writes=[R_junk, R_sm1[b]])
            P.op("act", lambda e: e.activation(out=small[:, sc + 1:sc + 2], in_=small[:, sc:sc + 1], func=AF.Sqrt, bias=epsc[:, :], scale=1.0 / D),
                 reads=[R_c], writes=[R_sm1[b]])
            P.op("dve", lambda e: e.reciprocal(out=small[:, sc + 2:sc + 3], in_=small[:, sc + 1:sc + 2]), reads=[], writes=[R_sm1[b]])
            P.op("dve", lambda e: e.scalar_tensor_tensor(out=XNS[b][:, :], in0=XT4[xb][:, :], scalar=small[:, sc + 2:sc + 3], in1=NWBC1[:, :],
                                                         op0=ALU.mult, op1=ALU.mult),
                 reads=[R_xt4[xb], R_sm1[b], R_nw], writes=[R_xns[b]])

        def back_1a(tc):
            b = tc % 2
            pb = tc % 2
            for dc in range(8):
                P.op("pe", lambda e, dc=dc: e.transpose(psb(pb, BF16)[:, dc * 128:(dc + 1) * 128], XNS[b][:, dc * 128:(dc + 1) * 128], identb[:, :]),
                     reads=[R_xns[b], R_c], writes=[R_ps[pb]])
            P.op("act", lambda e: e.activation(out=XNT[:, :, tc * 128:(tc + 1) * 128],
                                               in_=psb(pb, BF16)[:, 0:1024].rearrange("p (a b) -> p a b", b=128), func=AF.Copy),
                 reads=[], writes=[R_ps[pb], R_xnt[tc]])

        front_1a(0)
        for tc in range(NTC):
            if tc + 1 < NTC:
                front_1a(tc + 1)
            back_1a(tc)

        wpi = [0]

        def load_winp(col0, ncol):
            i = wpi[0] % 2
            wpi[0] += 1
            P.dma("pool", ds_w[i], lambda e, i=i: e.dma_start(out=WINP[i][:, :, 0:ncol],
                                                              in_=win_d.rearrange("(c p) n -> p c n", p=128)[:, :, col0:col0 + ncol]),
                  writes=[R_winp[i]])
            return i

        def proj_chunk(wi, lcol, evac):
            for tb in range(4):
                pb = 2 + tb % 2
                for dc in range(8):
                    P.op("pe", lambda e, dc=dc, tb=tb, pb=pb: e.matmul(psb(pb), WINP[wi][:, dc, lcol:lcol + 128], XNT[:, dc, tb * 512:(tb + 1) * 512],
                                                                      start=(dc == 0), stop=(dc == 7)),
                         reads=[R_winp[wi]] + R_xnt[tb * 4:(tb + 1) * 4], writes=[R_ps[pb]])
                evac(tb, pb)

        def mk_evac_raw(RAWB, R_rawb):
            def evac_raw(tb, pb):
                P.op("act", lambda e, tb=tb, pb=pb: e.activation(out=RAWB[:, 2 + tb * 512: 2 + (tb + 1) * 512], in_=psb(pb), func=AF.Copy),
                     reads=[], writes=[R_ps[pb], R_rawb])
            return evac_raw

        RSQS = [RSQ, RSQ]
        R_rsqs = [R_opt[1], R_opt[1]]
        chunks = [(grp, kind, hh) for grp, kind in enumerate(("q", "k", "v")) for hh in range(H)]
        wis = {}

        def stage_a(ci):
            grp, kind, hh = chunks[ci]
            if hh == 0:
                wis[grp] = load_winp(grp * 512, 512)
            wi = wis[grp]
            cc = grp * 4 + hh
            RAWB, R_rawb = RAWS[cc % 2], R_raws[cc % 2]
            CONVB, R_accb = CONVS[cc % 2], R_accs[cc % 2]
            proj_chunk(wi, hh * 128, mk_evac_raw(RAWB, R_rawb))

        def stage_c(ci):
            grp, kind, hh = chunks[ci]
            cc = grp * 4 + hh
            RAWB, R_rawb = RAWS[cc % 2], R_raws[cc % 2]
            CONVB, R_accb = CONVS[cc % 2], R_accs[cc % 2]
            P.op("dve", lambda e: e.tensor_scalar(out=CONVB[:, :], in0=RAWB[:, 0:T], scalar1=cw[:, cc, 0:1], scalar2=None, op0=ALU.mult),
                 reads=[R_rawb, R_c], writes=[R_accb])
            for j in range(1, 5):
                P.op("dve", lambda e, j=j: e.scalar_tensor_tensor(out=CONVB[:, :], in0=RAWB[:, j:j + T], scalar=cw[:, cc, j:j + 1], in1=CONVB[:, :],
                                                                  op0=ALU.mult, op1=ALU.add),
                     reads=[R_rawb, R_c], writes=[R_accb])
            if kind == "v":
                P.op("act", lambda e: e.activation(out=VTMP[:, :], in_=CONVB[:, :], func=AF.Silu), reads=[R_accb], writes=[R_opt[0]])
                for half in range(2):
                    pb = 4 + half
                    for t8 in range(8):
                        tc = half * 8 + t8
                        P.op("pe", lambda e, tc=tc, t8=t8, pb=pb: e.transpose(psb(pb, BF16)[:, t8 * 128:(t8 + 1) * 128], VTMP[:, tc * 128:(tc + 1) * 128], identb[:, :]),
                             reads=[R_opt[0], R_c], writes=[R_ps[pb]])
                    P.op("act", lambda e, half=half, pb=pb: e.activation(out=VTOK[:, half * 8:(half + 1) * 8, hh, :],
                                                                        in_=psb(pb, BF16)[:, 0:1024].rearrange("p (a b) -> p a b", b=128), func=AF.Copy),
                         reads=[], writes=[R_ps[pb], R_vtok])
            else:
                P.op("act", lambda e: e.activation(out=CONVB[:, :], in_=CONVB[:, :], func=AF.Silu), reads=[], writes=[R_accb])
                P.op("pool", lambda e: e.tensor_tensor(out=SQ[:, :], in0=CONVB[:, :], in1=CONVB[:, :], op=ALU.mult), reads=[R_accb], writes=[R_opt[0]])

        def stage_a2(ci):
            grp, kind, hh = chunks[ci]
            cc = grp * 4 + hh
            RSQB, R_rsqb = RSQS[cc % 2], R_rsqs[cc % 2]
            if kind != "v":
                for tb in range(4):
                    pb = 4 + tb % 2
                    P.op("pe", lambda e, tb=tb, pb=pb: e.matmul(psb(pb), onesb[:, :], SQ[:, tb * 512:(tb + 1) * 512], start=True, stop=True),
                         reads=[R_opt[0], R_c], writes=[R_ps[pb]])
                    P.op("act", lambda e, tb=tb, pb=pb: e.activation(out=RSQB[:, tb * 512:(tb + 1) * 512], in_=psb(pb), func=AF.Ln, bias=epsc[:, :], scale=1.0),
                         reads=[R_c], writes=[R_ps[pb], R_rsqb])

        def stage_b(ci):
            grp, kind, hh = chunks[ci]
            if kind == "v":
                return
            cc = grp * 4 + hh
            CONVB, R_accb = CONVS[cc % 2], R_accs[cc % 2]
            RSQB, R_rsqb = RSQS[cc % 2], R_rsqs[cc % 2]
            P.op("act", lambda e: e.activation(out=RSQB[:, :], in_=RSQB[:, :], func=AF.Exp, scale=-0.5), reads=[], writes=[R_rsqb])
            dst = QT if kind == "q" else KT
            rdst = R_qt[hh] if kind == "q" else R_kt[hh]
            scl = 128.0 ** -0.5 if kind == "q" else 1.0
            P.op("dve", lambda e: e.scalar_tensor_tensor(out=dst[:, hh, :], in0=CONVB[:, :], scalar=scl, in1=RSQB[:, :], op0=ALU.mult, op1=ALU.mult),
                 reads=[R_accb, R_rsqb], writes=[rdst])
            if kind == "k":
                for half in range(2):
                    pb = 6 + half
                    for t8 in range(8):
                        tc = half * 8 + t8
                        P.op("pe", lambda e, tc=tc, t8=t8, pb=pb: e.transpose(psb(pb, BF16)[:, t8 * 128:(t8 + 1) * 128], KT[:, hh, tc * 128:(tc + 1) * 128], identb[:, :]),
                             reads=[R_kt[hh], R_c], writes=[R_ps[pb]])
                    P.op("act", lambda e, half=half, pb=pb: e.activation(out=KTOK[:, half * 8:(half + 1) * 8, hh, :],
                                                                        in_=psb(pb, BF16)[:, 0:1024].rearrange("p (a b) -> p a b", b=128), func=AF.Copy),
                         reads=[], writes=[R_ps[pb], R_ktok])

        NCH = len(chunks)
        stage_a(0)
        for ci in range(NCH + 1):
            if ci + 1 < NCH:
                stage_a(ci + 1)
            if ci < NCH:
                stage_c(ci)
            if ci >= 1:
                stage_b(ci - 1)
            if ci < NCH:
                stage_a2(ci)
        P.barrier()

        wi = load_winp(1536, 528)
        for tc in range(NTC):
            pb = 2 + tc % 2
            for dc in range(8):
                P.op("pe", lambda e, dc=dc, tc=tc, pb=pb, wi=wi: e.matmul(psb(pb), XNT[:, dc, tc * 128:(tc + 1) * 128], WINP[wi][:, dc, 0:512], start=(dc == 0), stop=(dc == 7)),
                     reads=[R_winp[wi], R_xnt[tc]], writes=[R_ps[pb]])
            P.op("act", lambda e, pb=pb: e.activation(out=ZF[:, :], in_=psb(pb), func=AF.Silu), reads=[], writes=[R_ps[pb], R_acc])
            P.op("dve", lambda e, tc=tc: e.tensor_tensor(out=ZS[:, tc, :].rearrange("p (h d) -> p h d", d=128), in0=ZF[:, :].rearrange("p (h d) -> p h d", d=128),
                                                         in1=hnw[:, :].unsqueeze(1).to_broadcast([128, 4, 128]), op=ALU.mult),
                 reads=[R_acc, R_c], writes=[R_zs[tc]])
        ABV = psb(4)[:, 0:256].rearrange("p (a b) -> p a b", b=16)
        for tc in range(NTC):
            for dc in range(8):
                P.op("pe", lambda e, dc=dc, tc=tc, wi=wi: e.matmul(ABV[:, tc, :], XNT[:, dc, tc * 128:(tc + 1) * 128], WINP[wi][:, dc, 512:528], start=(dc == 0), stop=(dc == 7)),
                     reads=[R_winp[wi], R_xnt[tc]], writes=[R_ps[4]])
        P.op("act", lambda e: e.activation(out=ABS[:, :, :], in_=ABV, func=AF.Copy), reads=[], writes=[R_ps[4], R_abs])
        ABS5 = ABS[:, :, :].rearrange("p a (d k h) -> p a d k h", d=2, k=2)
        for d_ in range(2):
            P.op("dve", lambda e, d_=d_: e.tensor_tensor(out=TM1[:, :, d_ * 4:(d_ + 1) * 4], in0=ABS5[:, :, d_, 0, :],
                                                         in1=abp[:, 8 + d_ * 4: 12 + d_ * 4].unsqueeze(1).to_broadcast([128, 16, 4]), op=ALU.add),
                 reads=[R_abs, R_c], writes=[R_g])
            P.op("act", lambda e, d_=d_: e.activation(out=bet[:, :, d_ * 4:(d_ + 1) * 4], in_=ABS5[:, :, d_, 1, :], func=AF.Sigmoid),
                 reads=[R_abs], writes=[R_g])
        P.op("act", lambda e: e.activation(out=TM1[:, :, :], in_=TM1[:, :, :], func=AF.Exp), reads=[], writes=[R_g])
        P.op("act", lambda e: e.activation(out=TM1[:, :, :], in_=TM1[:, :, :], func=AF.Ln, bias=onec[:, :], scale=1.0), reads=[R_c], writes=[R_g])
        P.op("dve", lambda e: e.tensor_tensor(out=gsm[:, :, :], in0=TM1[:, :, :], in1=small[:, 0:8].unsqueeze(1).to_broadcast([128, 16, 8]), op=ALU.mult),
             reads=[R_c], writes=[R_g])
        P.op("dve", lambda e: e.tensor_scalar(out=nbet[:, :, :], in0=bet[:, :, :], scalar1=-1.0, scalar2=None, op0=ALU.mult), reads=[], writes=[R_g])

        wi = load_winp(2064, 512)
        TW = T + 8
        P.op("pool", lambda e: e.memset(UB[:, :], 0.0), reads=[], writes=[R_acc])
        P.op("pool", lambda e: e.memset(SB2[:, :], 0.0), reads=[], writes=[R_raw, R_nw])
        P.op("pool", lambda e: e.memset(PB3[:, :], 0.0), reads=[], writes=[R_pb3])

        def evac_u(tb, pb):
            P.op("act", lambda e, tb=tb, pb=pb: e.activation(out=UB[:, 16 + tb * 512: 16 + (tb + 1) * 512], in_=psb(pb), func=AF.Copy),
                 reads=[], writes=[R_ps[pb], R_acc])

        for g, w in enumerate((2, 4, 8, 16)):
            lo = w // 2
            hi = w - lo - 1
            proj_chunk(wi, g * 128, evac_u)
            s_prev, r_prev = UB, R_acc
            sh = 1
            for lv in range({2: 1, 4: 2, 8: 3, 16: 4}[w]):
                dstb, rdst = (SB2, R_raw) if lv % 2 == 0 else (PB3, R_pb3)
                P.op("dve", lambda e, s_prev=s_prev, dstb=dstb, sh=sh: e.tensor_tensor(out=dstb[:, 16:16 + TW], in0=s_prev[:, 16:16 + TW], in1=s_prev[:, 16 - sh:16 - sh + TW], op=ALU.add),
                     reads=[r_prev], writes=[rdst])
                s_prev, r_prev = dstb, rdst
                sh *= 2
            P.op("dve", lambda e, s_prev=s_prev, hi=hi, w=w: e.scalar_tensor_tensor(out=DIFB[:, :], in0=s_prev[:, 16 + hi:16 + hi + T], scalar=1.0 / w, in1=UB[:, 16:16 + T],
                                                                                   op0=ALU.mult, op1=ALU.subtract),
                 reads=[r_prev, R_acc], writes=[R_difb])
            P.op("dve", lambda e, s_prev=s_prev, hi=hi, lo=lo, g=g: e.tensor_tensor(out=small[:, 16:16 + lo], in0=s_prev[:, 16 + hi:16 + hi + lo], in1=invc[:, g * 16:g * 16 + lo], op=ALU.mult),
                 reads=[r_prev, R_c], writes=[R_small])
            P.op("dve", lambda e, lo=lo: e.tensor_tensor(out=DIFB[:, 0:lo], in0=small[:, 16:16 + lo], in1=UB[:, 16:16 + lo], op=ALU.subtract),
                 reads=[R_small, R_acc], writes=[R_difb])
            if hi > 0:
                P.op("dve", lambda e, s_prev=s_prev, hi=hi, g=g: e.tensor_tensor(out=small[:, 32:32 + hi], in0=s_prev[:, 16 + T:16 + T + hi], in1=invc[:, g * 16 + 8:g * 16 + 8 + hi], op=ALU.mult),
                     reads=[r_prev, R_c], writes=[R_small])
                P.op("dve", lambda e, hi=hi: e.tensor_tensor(out=DIFB[:, T - hi:T], in0=small[:, 32:32 + hi], in1=UB[:, 16 + T - hi:16 + T], op=ALU.subtract),
                     reads=[R_small, R_acc], writes=[R_difb])
            for tb in range(4):
                pb = 4 + tb % 2
                P.op("pe", lambda e, tb=tb, pb=pb, g=g: e.matmul(psb(pb), pwb[:, g, :], DIFB[:, tb * 512:(tb + 1) * 512], start=True, stop=True),
                     reads=[R_difb, R_c], writes=[R_ps[pb]])
                P.op("act", lambda e, tb=tb, pb=pb, g=g: e.activation(out=OPT[:, g, tb * 512:(tb + 1) * 512], in_=psb(pb), func=AF.Identity, scale=psc[:, g:g + 1]),
                     reads=[R_c], writes=[R_ps[pb], R_opt[g]])
        P.barrier()

        if DEBUG:
            ds_dbg = newds("ds_dbg")
            for name, ap, shape, dt in (("d_qt", QT, [128, H * T], BF16), ("d_kt", KT, [128, H * T], BF16),
                                        ("d_ktok", KTOK, [128, NTC * H * 128], BF16), ("d_vtok", VTOK, [128, NTC * H * 128], BF16),
                                        ("d_zs", ZS, [128, NTC * 512], BF16), ("d_opt", OPT, [128, 4 * T], BF16)):
                dd = nc.dram_tensor(name, shape, dt, kind="ExternalOutput").ap()
                flat = ap
                if len(ap.shape) == 3:
                    flat = ap.rearrange("p a b -> p (a b)")
                elif len(ap.shape) == 4:
                    flat = ap.rearrange("p a b c -> p (a b c)")
                out_toks.append(P.dma("sp", ds_dbg, lambda e, dd=dd, flat=flat: e.dma_start(out=dd, in_=flat)))
            for name, tl in (("d_gsm", gsm), ("d_bet", bet)):
                dd = nc.dram_tensor(name, [128, 128], F32, kind="ExternalOutput").ap()
                out_toks.append(P.dma("sp", ds_dbg, lambda e, dd=dd, tl=tl: e.dma_start(out=dd, in_=tl[:, :, :].rearrange("p a b -> p (a b)"))))

        NEU = F32
        OACC = view(OFF_A, [128, NTC, H, 128], F32)

        def make_ctx(base, limit, banks):
            o2 = [base]

            def tv(shape, dt):
                n = 1
                for s_ in shape[1:]:
                    n *= s_
                nb = n * (4 if dt == F32 else 2)
                off = o2[0]
                o2[0] += nb
                assert o2[0] <= limit, (o2[0], limit)
                return view(off, shape, dt)
            c = {}
            for nm in ("MG", "EM", "EMS", "BYV"):
                c[nm] = tv([128, 4, 128], F32)
            for nm in ("AK", "ATK", "RRK"):
                c[nm] = [tv([128, 4, 128], NEU) for _ in range(2)]
            for nm in ("QKT", "NTB", "EK", "KDEC", "YWT", "VNEW"):
                c[nm] = tv([128, 4, 128], BF16)
            for nm in ("MG", "EM", "EMS", "BYV", "QKT", "NTB", "EK", "KDEC", "YWT", "VNEW"):
                c["R_" + nm] = R()
            for nm in ("AK", "ATK", "RRK"):
                c["R_" + nm] = [R(), R()]
            c["banks"] = banks
            c["end"] = o2[0]
            return c

        ctxs = [make_ctx(OFF_A + 32 * K, OFF_A + 64 * K, (0, 1, 2, 3)), make_ctx(OFF_F, ARENA_B, (4, 5, 6, 7))]
        SST = view(ctxs[0]["end"], [128, 8, 128], F32)
        SBF = view(ctxs[0]["end"] + 4 * K, [128, 8, 128], BF16)
        assert ctxs[0]["end"] + 6 * K <= OFF_A + 64 * K
        R_oacc = [R() for _ in range(NTC)]
        R_s = [R(), R()]
        R_sbf = [R(), R()]

        for n in range(NTC):
            P.op("pool", lambda e, n=n: e.memset(OACC[:, n, :, :], 0.0), writes=[R_oacc[n]])
        P.op("pool", lambda e: e.memset(SST[:, :, :], 0.0), writes=R_s)
        P.op("pool", lambda e: e.memset(SBF[:, :, :], 0.0), writes=R_sbf)

        EV = psb(0)[:, 0:384].rearrange("p (a b) -> p a b", b=24)
        for d_ in range(2):
            sfx = "_f" if d_ == 0 else "_b"
            for j, lh in enumerate((cst["ut" + sfx], cst["sl" + sfx], cst["ones"])):
                c0 = j * 8 + d_ * 4
                P.op("pe", lambda e, d_=d_, lh=lh, c0=c0: e.matmul(EV[:, :, c0:c0 + 4], lh[:, :], gsm[:, :, d_ * 4:(d_ + 1) * 4], start=True, stop=True),
                     reads=[R_c, R_g], writes=[R_ps[0]])
        P.op("act", lambda e: e.activation(out=exps[:, :, :], in_=EV, func=AF.Exp), reads=[], writes=[R_ps[0], R_exps])

        def v4(i):
            return psb(i).rearrange("p (h c) -> p h c", h=4)

        identbc = ident[:, :].unsqueeze(1).to_broadcast([128, 4, 128])

        def group(n, d_, c):
            sfx = "_f" if d_ == 0 else "_b"
            ut, sl, neg, st = cst["ut" + sfx], cst["sl" + sfx], cst["neg" + sfx], cst["str" + sfx]
            ns = slice(n * 128, (n + 1) * 128)
            c4 = d_ * 4
            b0, b1, b2, b3 = c["banks"]
            MG, EM, EMS, BYV, AK, ATK, RRK = c["MG"], c["EM"], c["EMS"], c["BYV"], c["AK"], c["ATK"], c["RRK"]
            QKT, NTB, EK, KDEC, YWT, VNEW = c["QKT"], c["NTB"], c["EK"], c["KDEC"], c["YWT"], c["VNEW"]
            R_mg, R_em, R_ems, R_byv, R_ak, R_atk, R_rrk = c["R_MG"], c["R_EM"], c["R_EMS"], c["R_BYV"], c["R_AK"], c["R_ATK"], c["R_RRK"]
            R_qkt, R_ntb, R_ek, R_kdec, R_ywt, R_vnew = c["R_QKT"], c["R_NTB"], c["R_EK"], c["R_KDEC"], c["R_YWT"], c["R_VNEW"]
            for h in range(4):
                P.op("pool", lambda e, h=h: e.tensor_scalar(out=MG[:, h, :], in0=sl[:, :], scalar1=gsm[:, n, c4 + h:c4 + h + 1], scalar2=1.0, op0=ALU.mult, op1=ALU.mult),
                     reads=[R_c, R_g], writes=[R_mg])
            yield
            DV = v4(b1)
            for h in range(4):
                P.op("pe", lambda e, h=h: e.matmul(DV[:, h, :], MG[:, h, :], ut[:, :], start=True, stop=False), reads=[R_mg, R_c], writes=[R_ps[b1]])
                P.op("pe", lambda e, h=h: e.matmul(DV[:, h, :], ident[:, :], neg[:, :], start=False, stop=True), reads=[R_c], writes=[R_ps[b1]])
            yield
            P.op("act", lambda e: e.activation(out=EM[:, :, :], in_=DV, func=AF.Exp), reads=[], writes=[R_ps[b1], R_em])
            P.op("pool", lambda e: e.tensor_tensor(out=EMS[:, :, :], in0=EM[:, :, :], in1=st[:, :].unsqueeze(1).to_broadcast([128, 4, 128]), op=ALU.mult),
                 reads=[R_em, R_c], writes=[R_ems])
            yield
            for hp in range(2):
                V4 = psb(b0).rearrange("p (h k c) -> p h k c", h=2, k=2)
                for hl in range(2):
                    h = hp * 2 + hl
                    P.op("pe", lambda e, h=h, hl=hl: e.matmul(V4[:, hl, 0, :], KT[:, h, ns], KT[:, h, ns], start=True, stop=True), reads=[R_kt[h]], writes=[R_ps[b0]])
                    P.op("pe", lambda e, h=h, hl=hl: e.matmul(V4[:, hl, 1, :], KT[:, h, ns], QT[:, h, ns], start=True, stop=True), reads=[R_kt[h], R_qt[h]], writes=[R_ps[b0]])
                yield
                for hl in range(2):
                    h = hp * 2 + hl
                    P.op("dve", lambda e, h=h, hl=hl: e.scalar_tensor_tensor(out=AK[0][:, h, :], in0=V4[:, hl, 0, :], scalar=nbet[:, n, c4 + h:c4 + h + 1], in1=EMS[:, h, :],
                                                                             op0=ALU.mult, op1=ALU.mult),
                         reads=[R_g, R_ems], writes=[R_ps[b0], R_ak[0]])
                P.op("dve", lambda e, hp=hp: e.tensor_tensor(out=QKT[:, 2 * hp:2 * hp + 2, :], in0=V4[:, :, 1, :], in1=EM[:, 2 * hp:2 * hp + 2, :], op=ALU.mult),
                     reads=[R_em], writes=[R_ps[b0], R_qkt])
                yield
            TV = psb(b1, BF16)[:, 0:512].rearrange("p (h c) -> p h c", h=4) if NEU == BF16 else v4(b1)
            idn = identb if NEU == BF16 else ident
            for h in range(4):
                P.op("pe", lambda e, h=h: e.transpose(TV[:, h, :], AK[0][:, h, :], idn[:, :]), reads=[R_ak[0], R_c], writes=[R_ps[b1]])
            yield
            P.op("act", lambda e: e.activation(out=ATK[0][:, :, :], in_=TV, func=AF.Copy), reads=[], writes=[R_ps[b1], R_atk[0]])
            P.op("pool", lambda e: e.tensor_tensor(out=RRK[0][:, :, :], in0=AK[0][:, :, :], in1=identbc, op=ALU.add), reads=[R_ak[0], R_c], writes=[R_rrk[0]])
            yield
            cur = 0
            AV, ATV, RV = v4(b2), v4(b3), v4(b1)
            for k in range(1, 7):
                nxt = 1 - cur
                for h in range(4):
                    P.op("pe", lambda e, h=h, cur=cur: e.matmul(ATV[:, h, :], AK[cur][:, h, :], ATK[cur][:, h, :], start=True, stop=True),
                         reads=[R_ak[cur], R_atk[cur]], writes=[R_ps[b3]])
                if k < 6:
                    for h in range(4):
                        P.op("pe", lambda e, h=h, cur=cur: e.matmul(AV[:, h, :], ATK[cur][:, h, :], AK[cur][:, h, :], start=True, stop=True),
                             reads=[R_ak[cur], R_atk[cur]], writes=[R_ps[b2]])
                yield
                P.op("act", lambda e, nxt=nxt: e.activation(out=ATK[nxt][:, :, :], in_=ATV, func=AF.Copy), reads=[], writes=[R_ps[b3], R_atk[nxt]])
                if k < 6:
                    P.op("dve", lambda e, nxt=nxt: e.tensor_copy(out=AK[nxt][:, :, :], in_=AV), reads=[], writes=[R_ps[b2], R_ak[nxt]])
                yield
                for h in range(4):
                    P.op("pe", lambda e, h=h, cur=cur, nxt=nxt: e.matmul(RV[:, h, :], ATK[nxt][:, h, :], RRK[cur][:, h, :], start=True, stop=True),
                         reads=[R_atk[nxt], R_rrk[cur]], writes=[R_ps[b1]])
                yield
                if k < 6:
                    P.op("dve", lambda e, cur=cur, nxt=nxt: e.tensor_tensor(out=RRK[nxt][:, :, :], in0=RV, in1=RRK[cur][:, :, :], op=ALU.add),
                         reads=[R_rrk[cur]], writes=[R_ps[b1], R_rrk[nxt]])
                else:
                    P.op("dve", lambda e, cur=cur: e.tensor_tensor(out=NTB[:, :, :], in0=RV, in1=RRK[cur][:, :, :], op=ALU.add),
                         reads=[R_rrk[cur]], writes=[R_ps[b1], R_ntb])
                cur = nxt
                yield
            for h in range(4):
                P.op("act", lambda e, h=h: e.activation(out=EK[:, h, :], in_=KTOK[:, n, h, :], func=AF.Identity, scale=exps[:, n, c4 + h:c4 + h + 1]),
                     reads=[R_ktok, R_exps], writes=[R_ek])
                P.op("act", lambda e, h=h: e.activation(out=KDEC[:, h, :], in_=KTOK[:, n, h, :], func=AF.Identity, scale=exps[:, n, 8 + c4 + h:8 + c4 + h + 1]),
                     reads=[R_ktok, R_exps], writes=[R_kdec])
            yield
            YVV, YWV = v4(b0), v4(b2)
            for h in range(4):
                P.op("pe", lambda e, h=h: e.matmul(YVV[:, h, :], NTB[:, h, :], VTOK[:, n, h, :], start=True, stop=True), reads=[R_ntb, R_vtok], writes=[R_ps[b0]])
            for h in range(4):
                P.op("pe", lambda e, h=h: e.matmul(YWV[:, h, :], EK[:, h, :], NTB[:, h, :], start=True, stop=True), reads=[R_ntb, R_ek], writes=[R_ps[b2]])
            yield
            for h in range(4):
                P.op("act", lambda e, h=h: e.activation(out=BYV[:, h, :], in_=YVV[:, h, :], func=AF.Identity, scale=bet[:, n, c4 + h:c4 + h + 1]),
                     reads=[R_g], writes=[R_ps[b0], R_byv])
            P.op("act", lambda e: e.activation(out=YWT[:, :, :], in_=YWV, func=AF.Copy), reads=[], writes=[R_ps[b2], R_ywt])
            yield
            P1V, O1V, O2V, SUV = v4(b1), v4(b3), v4(b2), v4(b0)
            for h in range(4):
                P.op("pe", lambda e, h=h: e.matmul(P1V[:, h, :], YWT[:, h, :], SBF[:, c4 + h, :], start=True, stop=True), reads=[R_ywt, R_sbf[d_]], writes=[R_ps[b1]])
            for h in range(4):
                P.op("pe", lambda e, h=h: e.matmul(O1V[:, h, :], QT[:, h, ns], SBF[:, c4 + h, :], start=True, stop=True), reads=[R_qt[h], R_sbf[d_]], writes=[R_ps[b3]])
            yield
            for h in range(4):
                P.op("dve", lambda e, h=h: e.scalar_tensor_tensor(out=VNEW[:, h, :], in0=P1V[:, h, :], scalar=nbet[:, n, c4 + h:c4 + h + 1], in1=BYV[:, h, :],
                                                                  op0=ALU.mult, op1=ALU.add),
                     reads=[R_g, R_byv], writes=[R_ps[b1], R_vnew])
            yield
            for h in range(4):
                P.op("pe", lambda e, h=h: e.matmul(O2V[:, h, :], QKT[:, h, :], VNEW[:, h, :], start=True, stop=True), reads=[R_qkt, R_vnew], writes=[R_ps[b2]])
            for h in range(4):
                P.op("pe", lambda e, h=h: e.matmul(SUV[:, h, :], KDEC[:, h, :], VNEW[:, h, :], start=True, stop=True), reads=[R_kdec, R_vnew], writes=[R_ps[b0]])
            yield
            for h in range(4):
                P.op("dve", lambda e, h=h: e.scalar_tensor_tensor(out=SST[:, c4 + h, :], in0=SST[:, c4 + h, :], scalar=exps[:, n, 16 + c4 + h:16 + c4 + h + 1], in1=SUV[:, h, :],
                                                                  op0=ALU.mult, op1=ALU.add),
                     reads=[R_exps], writes=[R_ps[b0], R_s[d_]])
            P.op("act", lambda e: e.activation(out=SBF[:, c4:c4 + 4, :], in_=SST[:, c4:c4 + 4, :], func=AF.Copy), reads=[R_s[d_]], writes=[R_sbf[d_]])
            yield
            for h in range(4):
                P.op("dve", lambda e, h=h: e.scalar_tensor_tensor(out=OACC[:, n, h, :], in0=O1V[:, h, :], scalar=exps[:, n, c4 + h:c4 + h + 1], in1=OACC[:, n, h, :],
                                                                  op0=ALU.mult, op1=ALU.add),
                     reads=[R_exps], writes=[R_ps[b3], R_oacc[n]])
            P.op("dve", lambda e: e.tensor_tensor(out=OACC[:, n, :, :], in0=O2V, in1=OACC[:, n, :, :], op=ALU.add), reads=[], writes=[R_ps[b2], R_oacc[n]])
            yield

        for i in range(NTC):
            gens = [group(i, 0, ctxs[0]), group(NTC - 1 - i, 1, ctxs[1])]
            while gens:
                for g_ in list(gens):
                    try:
                        next(g_)
                    except StopIteration:
                        gens.remove(g_)
        P.barrier()

        OT = view(OFF_B, [128, 4, T], BF16)
        TMPO = view(OFF_A + 32 * K, [128, 4, 128], F32)
        OGB = view(OFF_A + 34 * K, [128, 512], BF16)
        R_ot = [R() for _ in range(NTC)]
        R_tmpo = R()
        R_ogb = R()
        for n in range(NTC):
            P.op("dve", lambda e, n=n: e.tensor_tensor(out=TMPO[:, :, :], in0=OACC[:, n, :, :], in1=OACC[:, n, :, :], op=ALU.mult), reads=[R_oacc[n]], writes=[R_tmpo])
            P.op("dve", lambda e: e.tensor_reduce(out=small[:, 40:44], in_=TMPO[:, :, :], axis=AX.X, op=ALU.add), reads=[R_tmpo], writes=[R_small])
            P.op("act", lambda e: e.activation(out=small[:, 44:48], in_=small[:, 40:44], func=AF.Sqrt, bias=epsc[:, :], scale=1.0 / 128), reads=[R_c], writes=[R_small])
            P.op("dve", lambda e: e.reciprocal(out=small[:, 48:52], in_=small[:, 44:48]), reads=[], writes=[R_small])
            P.op("dve", lambda e, n=n: e.tensor_tensor(out=TMPO[:, :, :], in0=OACC[:, n, :, :], in1=small[:, 48:52].unsqueeze(2).to_broadcast([128, 4, 128]), op=ALU.mult),
                 reads=[R_oacc[n], R_small], writes=[R_tmpo])
            P.op("dve", lambda e, n=n: e.tensor_tensor(out=OGB[:, :], in0=TMPO[:, :, :].rearrange("p h d -> p (h d)"), in1=ZS[:, n, :], op=ALU.mult),
                 reads=[R_tmpo, R_zs[n]], writes=[R_ogb])
            pb = n % 2
            for h in range(4):
                P.op("pe", lambda e, h=h, pb=pb: e.transpose(psb(pb, BF16)[:, h * 128:(h + 1) * 128], OGB[:, h * 128:(h + 1) * 128], identb[:, :]),
                     reads=[R_ogb, R_c], writes=[R_ps[pb]])
            P.op("act", lambda e, n=n, pb=pb: e.activation(out=OT[:, :, n * 128:(n + 1) * 128], in_=psb(pb, BF16)[:, 0:512].rearrange("p (a b) -> p a b", b=128), func=AF.Copy),
                 reads=[], writes=[R_ps[pb], R_ot[n]])
        P.barrier()
        if DEBUG:
            dd = nc.dram_tensor("d_ot", [128, 4 * T], BF16, kind="ExternalOutput").ap()
            out_toks.append(P.dma("sp", ds_dbg, lambda e, dd=dd: e.dma_start(out=dd, in_=OT[:, :, :].rearrange("p a b -> p (a b)"))))

        WOUT = view(OFF_B + 16 * K, [128, 8, D], BF16)
        HACC = view(OFF_A, [128, NTC, D], F32)
        XN2 = view(OFF_C, [128, NTC, D], BF16)
        NWBC2 = view(OFF_D, [128, D], F32)
        XN2F = view(OFF_D + 4 * K, [128, D], F32)
        XN2T = view(OFF_D + 8 * K, [128, 8, 128], F32)
        R_wout = R()
        R_hacc = [R() for _ in range(NTC)]
        R_xn2 = [R() for _ in range(NTC)]
        R_nw2 = R()
        R_xn2f = R()
        R_xn2t = R()
        P.dma("pool", ds_w[2], lambda e: e.dma_start(out=WOUT[:, :, :], in_=wout_d.rearrange("(c p) n -> p c n", p=128)), writes=[R_wout])
        P.dma("sp", ds_c, lambda e: e.dma_start(out=NWBC2[:, :], in_=nw_d[1]), writes=[R_nw2])
        XN2Fs = [XN2F, view(OFF_F + 8 * K, [128, D], F32)]
        XN2Ts = [XN2T, view(OFF_F + 12 * K, [128, 8, 128], F32)]
        R_xn2fs = [R_xn2f, R()]
        R_xn2ts = [R_xn2t, R()]

        scr_toks = []
        R_sma = [R(), R()]
        R_smb = R()
        def s3_a(tc):
            b = tc % 2
            ts_ = slice(tc * 128, (tc + 1) * 128)
            XF, R_xf = XN2Fs[b], R_xn2fs[b]
            XTt, R_xt2 = XN2Ts[b], R_xn2ts[b]
            P.dma("sp", ds_x[b], lambda e: e.dma_start(out=XT[b][:, :], in_=x_d[tc * 128:(tc + 1) * 128, :]), writes=[R_xt[b]])
            for half in range(2):
                pb = 2 * (tc % 2) + half
                for cc in range(8):
                    lh = OT[:, cc, ts_] if cc < 4 else OPT[:, cc - 4, ts_]
                    rr = R_ot[tc] if cc < 4 else R_opt[cc - 4]
                    P.op("pe", lambda e, cc=cc, half=half, pb=pb, lh=lh: e.matmul(psb(pb), lh, WOUT[:, cc, half * 512:(half + 1) * 512], start=(cc == 0), stop=(cc == 7)),
                         reads=[rr, R_wout], writes=[R_ps[pb]])
                P.op("dve", lambda e, half=half, pb=pb: e.tensor_tensor(out=HACC[:, tc, half * 512:(half + 1) * 512], in0=psb(pb), in1=XT[b][:, half * 512:(half + 1) * 512], op=ALU.add),
                     reads=[R_xt[b]], writes=[R_ps[pb], R_hacc[tc]])

        def s3_n(tc):
            if not MOE:
                return
            b = tc % 2
            XF, R_xf = XN2Fs[b], R_xn2fs[b]
            XTt, R_xt2 = XN2Ts[b], R_xn2ts[b]
            sc = 56 + 3 * b
            P.op("act", lambda e: e.activation(out=XF[:, :], in_=HACC[:, tc, :], func=AF.Square, accum_out=small[:, sc:sc + 1]),
                 reads=[R_hacc[tc]], writes=[R_xf, R_sma[b]])
            P.op("act", lambda e: e.activation(out=small[:, sc + 1:sc + 2], in_=small[:, sc:sc + 1], func=AF.Sqrt, bias=epsc[:, :], scale=1.0 / D), reads=[R_c], writes=[R_sma[b]])
            P.op("dve", lambda e: e.reciprocal(out=small[:, sc + 2:sc + 3], in_=small[:, sc + 1:sc + 2]), reads=[], writes=[R_sma[b]])
            P.op("dve", lambda e: e.scalar_tensor_tensor(out=XF[:, :], in0=HACC[:, tc, :], scalar=small[:, sc + 2:sc + 3], in1=NWBC2[:, :], op0=ALU.mult, op1=ALU.mult),
                 reads=[R_hacc[tc], R_sma[b], R_nw2], writes=[R_xf])
            P.op("pool", lambda e: e.tensor_copy(out=XN2[:, tc, :], in_=XF[:, :]), reads=[R_xf], writes=[R_xn2[tc]])
            scr_toks.append(P.dma("sp", ds_s[b], lambda e: e.dma_start(out=xn2_d[tc * 128:(tc + 1) * 128, :], in_=XN2[:, tc, :]), reads=[R_xn2[tc]]))
            for dc in range(8):
                pb = 4 + dc // 4
                P.op("pe", lambda e, dc=dc, pb=pb: e.transpose(psb(pb)[:, (dc % 4) * 128:(dc % 4 + 1) * 128], XF[:, dc * 128:(dc + 1) * 128], ident[:, :]),
                     reads=[R_xf, R_c], writes=[R_ps[pb]])
            for hb in range(2):
                P.op("act", lambda e, hb=hb: e.activation(out=XTt[:, hb * 4:(hb + 1) * 4, :], in_=psb(4 + hb).rearrange("p (a b) -> p a b", b=128), func=AF.Copy),
                     reads=[], writes=[R_ps[4 + hb], R_xt2])

        def s3_b(tc):
            if not MOE:
                return
            b = tc % 2
            XTt, R_xt2 = XN2Ts[b], R_xn2ts[b]
            LG = psb(6 + b)[:, 0:16]
            for dc in range(8):
                P.op("pe", lambda e, dc=dc: e.matmul(LG, XTt[:, dc, :], rws[:, dc, :], start=(dc == 0), stop=(dc == 7)), reads=[R_xt2, R_c], writes=[R_ps[6 + b]])
            P.op("dve", lambda e: e.tensor_reduce(out=small[:, 52:53], in_=LG, axis=AX.X, op=ALU.max), reads=[], writes=[R_ps[6 + b], R_smb])
            P.op("dve", lambda e: e.tensor_scalar(out=small[:, 53:54], in0=small[:, 52:53], scalar1=-1.0, scalar2=None, op0=ALU.mult), reads=[], writes=[R_smb])
            P.op("act", lambda e: e.activation(out=prob[:, tc, :], in_=LG, func=AF.Exp, bias=small[:, 53:54], scale=1.0, accum_out=small[:, 54:55]),
                 reads=[R_smb], writes=[R_ps[6 + b], R_prob, R_smb])
            P.op("dve", lambda e: e.reciprocal(out=small[:, 55:56], in_=small[:, 54:55]), reads=[], writes=[R_smb])
            P.op("dve", lambda e: e.tensor_scalar(out=prob[:, tc, :], in0=prob[:, tc, :], scalar1=small[:, 55:56], scalar2=None, op0=ALU.mult), reads=[R_smb], writes=[R_prob])

        s3_a(0)
        for tc in range(NTC + 1):
            if tc + 1 < NTC:
                s3_a(tc + 1)
            if tc < NTC:
                s3_n(tc)
            if tc >= 1:
                s3_b(tc - 1)
        P.barrier()
        if MOE:
            PF = view(OFF_B, [128, T], F32)
            WORK = view(OFF_B + 8 * K, [128, T], F32)
            SELT = view(OFF_B + 16 * K, [128, T], F32)
            M8 = view(OFF_B + 24 * K, [128, 8], F32)
            TH = view(OFF_B + 24 * K + 64, [128, 1], F32)
            R_pf, R_work, R_selt, R_m8, R_th = R(), R(), R(), R(), R()
            slot_off = [OFF_F, OFF_F + 12 * K, OFF_E + 4 * K, OFF_B + 20 * K]
            NSLOT = len(slot_off)
            WGs = [view(o_, [128, 8, 256], BF16) for o_ in slot_off]
            WUs = [view(o_ + 4 * K, [128, 8, 256], BF16) for o_ in slot_off]
            WDs = [view(o_ + 8 * K, [128, 2, D], BF16) for o_ in slot_off]
            R_wg = [R() for _ in range(NSLOT)]
            R_wu = [R() for _ in range(NSLOT)]
            R_wd = [R() for _ in range(NSLOT)]

            def load_piece(pi):
                e_, pc = pi // 8, pi % 8
                sl_ = pi % NSLOT
                f0 = pc * 256
                P.dma("pool", ds_w[3 * sl_], lambda e, e_=e_, sl_=sl_, f0=f0: e.dma_start(out=WGs[sl_][:, :, :], in_=wg_d[e_].rearrange("(c p) f -> p c f", p=128)[:, :, f0:f0 + 256]),
                      writes=[R_wg[sl_]])
                P.dma("pool", ds_w[3 * sl_ + 1], lambda e, e_=e_, sl_=sl_, f0=f0: e.dma_start(out=WUs[sl_][:, :, :], in_=wu_d[e_].rearrange("(c p) f -> p c f", p=128)[:, :, f0:f0 + 256]),
                      writes=[R_wu[sl_]])
                P.dma("pool", ds_w[3 * sl_ + 2], lambda e, e_=e_, sl_=sl_, f0=f0: e.dma_start(out=WDs[sl_][:, :, :], in_=wd_d[e_, f0:f0 + 256, :].rearrange("(c p) d -> p c d", p=128)),
                      writes=[R_wd[sl_]])

            for pi in range(3):
                load_piece(pi)

            for tc in range(NTC):
                b = tc // 4
                P.op("pe", lambda e, tc=tc, b=b: e.transpose(psb(b)[0:16, (tc % 4) * 128:(tc % 4 + 1) * 128], prob[:, tc, :], ident[:, :]),
                     reads=[R_prob, R_c], writes=[R_ps[b]])
            for b in range(4):
                P.op("act", lambda e, b=b: e.activation(out=PF[0:16, b * 512:(b + 1) * 512], in_=psb(b)[0:16, :], func=AF.Copy), reads=[], writes=[R_ps[b], R_pf])
                P.op("dve", lambda e, b=b: e.tensor_copy(out=WORK[0:16, b * 512:(b + 1) * 512], in_=psb(b)[0:16, :]), reads=[], writes=[R_ps[b], R_work])
            for rnd in range(CAP // 8):
                P.op("dve", lambda e: e.max(out=M8[0:16, :], in_=WORK[0:16, :]), reads=[R_work], writes=[R_m8])
                if rnd < CAP // 8 - 1:
                    P.op("dve", lambda e: e.match_replace(out=WORK[0:16, :], in_to_replace=M8[0:16, :], in_values=WORK[0:16, :], imm_value=-1.0),
                         reads=[R_m8], writes=[R_work])
            P.op("dve", lambda e: e.tensor_reduce(out=TH[0:16, :], in_=M8[0:16, :], axis=AX.X, op=ALU.min), reads=[R_m8], writes=[R_th])
            P.op("dve", lambda e: e.tensor_scalar(out=SELT[0:16, :], in0=PF[0:16, :], scalar1=TH[0:16, 0:1], scalar2=None, op0=ALU.is_ge),
                 reads=[R_pf, R_th], writes=[R_selt])
            SV = psb(4)[:, 0:256].rearrange("p (a b) -> p a b", b=16)
            for tc in range(NTC):
                P.op("pe", lambda e, tc=tc: e.transpose(SV[:, tc, :], SELT[0:16, tc * 128:(tc + 1) * 128], ident[0:16, 0:16]),
                     reads=[R_selt, R_c], writes=[R_ps[4]])
            P.op("act", lambda e: e.activation(out=sel[:, :, :], in_=SV, func=AF.Copy), reads=[], writes=[R_ps[4], R_sel])
            P.op("dve", lambda e: e.tensor_copy(out=selb[:, :, :], in_=SV), reads=[], writes=[R_ps[4], R_sel])
            PV = psb(5)[:, 0:256].rearrange("p (a b) -> p a b", b=16)
            for tc in range(NTC):
                mms = [(onesb, t2) for t2 in range(tc)] + [(ltb, tc)]
                for i_, (lh, t2) in enumerate(mms):
                    P.op("pe", lambda e, tc=tc, lh=lh, t2=t2, i_=i_, nmm=len(mms): e.matmul(PV[:, tc, :], lh[:, :], selb[:, t2, :], start=(i_ == 0), stop=(i_ == nmm - 1)),
                         reads=[R_sel, R_c], writes=[R_ps[5]])
            P.op("dve", lambda e: e.scalar_tensor_tensor(out=posm[:, :, :], in0=PV, scalar=1.0, in1=sel[:, :, :], op0=ALU.add, op1=ALU.mult),
                 reads=[R_sel], writes=[R_ps[5], R_posm])
            P.op("dve", lambda e: e.tensor_scalar(out=posm[:, :, :], in0=posm[:, :, :], scalar1=-1.0, scalar2=None, op0=ALU.add), reads=[], writes=[R_posm])
            P.barrier()
            if DEBUG:
                for name, tl in (("d_prob", prob), ("d_sel", sel), ("d_posm", posm)):
                    dd = nc.dram_tensor(name, [128, 256], F32, kind="ExternalOutput").ap()
                    out_toks.append(P.dma("sp", ds_dbg, lambda e, dd=dd, tl=tl: e.dma_start(out=dd, in_=tl[:, :, :].rearrange("p a b -> p (a b)"))))

            load_piece(3)
            PE_ = view(OFF_B, [128, NTC, 256], BF16)
            PTE = view(OFF_B + 8 * K, [128, 2, T], BF16)
            XG = view(OFF_D, [128, 8, 256], BF16)
            HTT = view(OFF_D + 4 * K, [128, 16, 256], BF16)
            SGs = [view(OFF_D + 12 * K + i * K, [128, 256], F32) for i in range(2)]
            TMPS = [view(OFF_B + 16 * K + i * 2 * K, [128, 512], F32) for i in range(2)]
            R_tmps = [R(), R()]
            YG = view(OFF_E, [128, 2, D], BF16)
            R_pe, R_pte, R_xg, R_yg = R(), R(), R(), R()
            XGT = view(OFF_C, [128, 2, D], BF16)
            IDXF = view(OFF_C + 4 * K, [128, 2], F32)
            IDXU = view(OFF_C + 4 * K + 64, [128, 2], U32)
            IDX4 = view(OFF_C + 4 * K + 128, [128, 4], F32)
            R_xgt, R_idx = [R(), R()], R()
            R_scr = R()
            R_scr.w = scr_toks[-1] if False else None
            R_sgs = [R(), R()]
            R_htt = [R() for _ in range(16)]
            next_piece = [NSLOT]

            def build_p1(e_, tc):
                P.op("dve", lambda e, tc=tc, e_=e_: e.tensor_scalar(out=PE_[:, tc, :], in0=iota[:, :], scalar1=posm[:, tc, e_:e_ + 1], scalar2=None, op0=ALU.is_equal),
                     reads=[R_posm, R_c], writes=[R_pe])

            def build_p(e_):
                for tc in range(NTC):
                    build_p1(e_, tc)

            def gather_idx():
                IV = psb(7)[:, 0:4]
                for jc in range(2):
                    for tc in range(NTC):
                        P.op("pe", lambda e, jc=jc, tc=tc: e.matmul(IV[:, jc * 2:jc * 2 + 2], PE_[:, tc, jc * 128:(jc + 1) * 128], tokb[:, tc, :], start=(tc == 0), stop=(tc == NTC - 1)),
                             reads=[R_pe, R_c], writes=[R_ps[7]])
                P.op("dve", lambda e: e.tensor_copy(out=IDX4[:, :], in_=IV), reads=[], writes=[R_ps[7], R_idx])
                IV3 = IDX4[:, :].rearrange("p (j k) -> p j k", k=2)
                P.op("dve", lambda e: e.scalar_tensor_tensor(out=IDXF[:, :], in0=IV3[:, :, 0], scalar=128.0, in1=IV3[:, :, 1], op0=ALU.mult, op1=ALU.add),
                     reads=[], writes=[R_idx])
                P.op("dve", lambda e: e.tensor_copy(out=IDXU[:, :], in_=IDXF[:, :]), reads=[], writes=[R_idx])
                for jc in range(2):
                    P.dma("pool", ds_g[jc], lambda e, jc=jc: e.indirect_dma_start(out=XGT[:, jc, :], out_offset=None, in_=xn2_d[:, :],
                                                                                   in_offset=bass.IndirectOffsetOnAxis(ap=IDXU[:, jc:jc + 1], axis=0)),
                          reads=[R_idx], writes=[R_xgt[jc]])

            def gather():
                for jc in range(2):
                    bank = 6 + jc
                    for dc in range(8):
                        P.op("pe", lambda e, jc=jc, dc=dc, bank=bank: e.transpose(psb(bank, BF16)[:, dc * 128:(dc + 1) * 128], XGT[:, jc, dc * 128:(dc + 1) * 128], identb[:, :]),
                             reads=[R_xgt[jc], R_c], writes=[R_ps[bank]])
                    P.op("act", lambda e, jc=jc, bank=bank: e.activation(out=XG[:, :, jc * 128:(jc + 1) * 128], in_=psb(bank, BF16)[:, 0:1024].rearrange("p (a b) -> p a b", b=128), func=AF.Copy),
                         reads=[], writes=[R_ps[bank], R_xg])

            def transpose_p():
                for jc in range(2):
                    for th in range(2):
                        bank = 4 + (jc * 2 + th) % 2
                        for t8 in range(8):
                            tc = th * 8 + t8
                            P.op("pe", lambda e, jc=jc, tc=tc, t8=t8, bank=bank: e.transpose(psb(bank, BF16)[:, t8 * 128:(t8 + 1) * 128], PE_[:, tc, jc * 128:(jc + 1) * 128], identb[:, :]),
                                 reads=[R_pe, R_c], writes=[R_ps[bank]])
                        P.op("act", lambda e, jc=jc, th=th, bank=bank: e.activation(out=PTE[:, jc, th * 1024:(th + 1) * 1024], in_=psb(bank, BF16)[:, 0:1024], func=AF.Copy),
                             reads=[], writes=[R_ps[bank], R_pte])

            def down(e_, fc):
                sl_ = (e_ * 8 + fc // 2) % NSLOT
                sub = fc % 2
                for jc in range(2):
                    for half in range(2):
                        yb = jc * 2 + half
                        P.op("pe", lambda e, fc=fc, jc=jc, half=half, yb=yb, sl_=sl_, sub=sub: e.matmul(psb(yb), HTT[:, fc, jc * 128:(jc + 1) * 128], WDs[sl_][:, sub, half * 512:(half + 1) * 512],
                                                                                                    start=(fc == 0), stop=(fc == 15)),
                             reads=[R_htt[fc], R_wd[sl_]], writes=[R_ps[yb]])
                if sub == 1:
                    if next_piece[0] < E * 8:
                        load_piece(next_piece[0])
                        next_piece[0] += 1

            def ffn(e_):
                for fc in range(16):
                    sl_ = (e_ * 8 + fc // 2) % NSLOT
                    sub = fc % 2
                    hb = 4 + fc % 2
                    for wi_, (W, RW) in enumerate(((WGs, R_wg), (WUs, R_wu))):
                        for dc in range(8):
                            P.op("pe", lambda e, wi_=wi_, W=W, dc=dc, hb=hb, sl_=sl_, sub=sub: e.matmul(psb(hb)[:, wi_ * 256:(wi_ + 1) * 256], W[sl_][:, dc, sub * 128:(sub + 1) * 128], XG[:, dc, :],
                                                                                                    start=(dc == 0), stop=(dc == 7)),
                                 reads=[RW[sl_], R_xg], writes=[R_ps[hb]])
                    sgi = fc % 2
                    P.op("act", lambda e, hb=hb, sgi=sgi: e.activation(out=SGs[sgi][:, :], in_=psb(hb)[:, 0:256], func=AF.Silu), reads=[], writes=[R_ps[hb], R_sgs[sgi]])
                    P.op("dve", lambda e, hb=hb, fc=fc, sgi=sgi: e.tensor_tensor(out=HTT[:, fc, :], in0=SGs[sgi][:, :], in1=psb(hb)[:, 256:512], op=ALU.mult),
                         reads=[R_sgs[sgi]], writes=[R_ps[hb], R_htt[fc]])
                    if e_ + 1 < E:
                        if fc < 8:
                            build_p1(e_ + 1, 2 * fc)
                            build_p1(e_ + 1, 2 * fc + 1)
                        if fc == 9:
                            gather_idx()
                    if fc >= 2:
                        down(e_, fc - 2)
                down(e_, 14)
                down(e_, 15)
                for jc in range(2):
                    for half in range(2):
                        yb = jc * 2 + half
                        P.op("act", lambda e, jc=jc, half=half, yb=yb: e.activation(out=YG[:, jc, half * 512:(half + 1) * 512], in_=psb(yb), func=AF.Copy),
                             reads=[], writes=[R_ps[yb], R_yg])

            def scatter(e_):
                sbanks = (6, 7, 0, 1, 2, 3)
                for tc in range(NTC):
                    for half in range(2):
                        i_ = tc * 2 + half
                        bank = sbanks[i_ % len(sbanks)]
                        for jc in range(2):
                            P.op("pe", lambda e, tc=tc, half=half, jc=jc, bank=bank: e.matmul(psb(bank), PTE[:, jc, tc * 128:(tc + 1) * 128], YG[:, jc, half * 512:(half + 1) * 512],
                                                                                            start=(jc == 0), stop=(jc == 1)),
                                 reads=[R_pte, R_yg], writes=[R_ps[bank]])
                        if i_ % 2 == 0:
                            P.op("dve", lambda e, tc=tc, half=half, bank=bank, e_=e_: e.scalar_tensor_tensor(out=HACC[:, tc, half * 512:(half + 1) * 512], in0=psb(bank), scalar=prob[:, tc, e_:e_ + 1],
                                                                                                         in1=HACC[:, tc, half * 512:(half + 1) * 512], op0=ALU.mult, op1=ALU.add),
                                 reads=[R_prob], writes=[R_ps[bank], R_hacc[tc]])
                        else:
                            ti = (i_ // 2) % 2
                            P.op("act", lambda e, tc=tc, bank=bank, e_=e_, ti=ti: e.activation(out=TMPS[ti][:, :], in_=psb(bank), func=AF.Identity, scale=prob[:, tc, e_:e_ + 1]),
                                 reads=[R_prob], writes=[R_ps[bank], R_tmps[ti]])
                            P.op("pool", lambda e, tc=tc, half=half, ti=ti: e.tensor_tensor(out=HACC[:, tc, half * 512:(half + 1) * 512], in0=HACC[:, tc, half * 512:(half + 1) * 512],
                                                                                        in1=TMPS[ti][:, :], op=ALU.add),
                                 reads=[R_tmps[ti]], writes=[R_hacc[tc]])

            P.wait_all("pool", scr_toks)
            build_p(0)
            gather_idx()
            gather()
            for e_ in range(E):
                transpose_p()
                ffn(e_)
                scatter(e_)
                if e_ + 1 < E:
                    gather()
            P.barrier()
        NWF = view(OFF_D, [128, D], F32)
        OTL = [view(OFF_E + i * 4 * K, [128, D], F32) for i in range(2)]
        R_nwf = R()
        R_otl = [R(), R()]
        P.dma("sp", ds_c, lambda e: e.dma_start(out=NWF[:, :], in_=nw_d[2]), writes=[R_nwf])
        for tc in range(NTC):
            ob = tc % 2
            P.op("act", lambda e, ob=ob, tc=tc: e.activation(out=OTL[ob][:, :], in_=HACC[:, tc, :], func=AF.Square, accum_out=small[:, 8:9]),
                 reads=[R_hacc[tc]], writes=[R_otl[ob], R_small])
            P.op("act", lambda e: e.activation(out=small[:, 9:10], in_=small[:, 8:9], func=AF.Sqrt, bias=epsc[:, :], scale=1.0 / D),
                 reads=[R_c], writes=[R_small])
            P.op("dve", lambda e: e.reciprocal(out=small[:, 10:11], in_=small[:, 9:10]), reads=[], writes=[R_small])
            P.op("dve", lambda e, ob=ob, tc=tc: e.scalar_tensor_tensor(out=OTL[ob][:, :], in0=HACC[:, tc, :], scalar=small[:, 10:11], in1=NWF[:, :], op0=ALU.mult, op1=ALU.mult),
                 reads=[R_hacc[tc], R_small, R_nwf], writes=[R_otl[ob]])
            out_toks.append(P.dma("sp", ds_o[ob], lambda e, tc=tc, ob=ob: e.dma_start(out=out_d[tc * 128:(tc + 1) * 128, :], in_=OTL[ob][:, :]), reads=[R_otl[ob]]))
        P.wait_all("sp", out_toks)

        with nc.Block() as block:
            @block.tensor
            def _(e):
                for f in P.q["pe"]:
                    f(e)

            @block.scalar
            def _(e):
                for f in P.q["act"]:
                    f(e)

            @block.vector
            def _(e):
                for f in P.q["dve"]:
                    f(e)

            @block.gpsimd
            def _(e):
                for f in P.q["pool"]:
                    f(e)

            @block.sync
            def _(e):
                for f in P.q["sp"]:
                    f(e)
    return nc


DEBUG = False
MOE = True


def make_in_maps(inputs):
    c = host_consts()
    f = lambda a: np.ascontiguousarray(np.asarray(a, dtype=np.float32))
    x = f(inputs["x"])
    shared = {
        "w_in": f(inputs["w_in"][0]),
        "conv_wP": f(np.asarray(inputs["conv_w"][0]).T.reshape(12, 128, 5).transpose(1, 0, 2).reshape(128, 60)),
        "abp": f(np.tile(np.concatenate([np.asarray(inputs["a_log_fwd"][0]), np.asarray(inputs["a_log_bwd"][0]),
                                         np.asarray(inputs["dt_bias_fwd"][0]), np.asarray(inputs["dt_bias_bwd"][0])])[None, :], (128, 1))),
        "nw0": f(np.tile(np.asarray(inputs["norm_mix_w"][0])[None, :], (128, 1))),
        "nw1": f(np.tile(np.asarray(inputs["norm_ffn_w"][0])[None, :], (128, 1))),
        "nw2": f(np.tile(np.asarray(inputs["norm_final_w"])[None, :], (128, 1))),
        "hnw": f(np.tile(np.asarray(inputs["head_norm_w"][0])[None, :], (128, 1))),
        "pool_wP": f(np.asarray(inputs["pool_w"][0]).transpose(1, 0, 2).reshape(128, 512)),
        "pool_scT": f(np.asarray(inputs["pool_scale"][0]).reshape(4, 128).T),
        "w_out": f(inputs["w_out"][0]),
        "router_wP": f(np.asarray(inputs["router_w"][0]).reshape(8, 128, 16).transpose(1, 0, 2).reshape(128, 128)),
        "wg": f(inputs["expert_w_gate"][0]),
        "wu": f(inputs["expert_w_up"][0]),
        "wd": f(inputs["expert_w_down"][0]),
        "c_iota": c["iota"],
        "c_tok": c["tok"],
        "c_invc": c["invc"],
    }
    for n in CONST_NAMES:
        shared["c_" + n] = c[n]
    maps = []
    for b in range(8):
        m = dict(shared)
        m["x"] = np.ascontiguousarray(x[b])
        maps.append(m)
    return maps


def kernel(**inputs):
    nc = build_nc()
    in_maps = make_in_maps(inputs)
    res = run_bass_kernel_spmd(nc, in_maps, core_ids=list(range(8)))
    out = np.stack([np.asarray(r["out"], dtype=np.float32) for r in res.results], axis=0)
    return out
```

```python
import numpy as np
from contextlib import ExitStack
import concourse.bass as bass
import concourse.mybir as mybir
from concourse.bass_utils import run_bass_kernel_spmd

F32 = mybir.dt.float32
BF16 = mybir.dt.bfloat16
U32 = mybir.dt.uint32
F32R = mybir.dt.float32r
ALU = mybir.AluOpType
AF = mybir.ActivationFunctionType
AX = mybir.AxisListType

T = 2048
D = 1024
NTC = 16
H = 4
E = 16
CAP = 256
FF = 2048
INC = 2576
EPS = 1e-6
BIG = 30000.0
ENGS = ("pe", "act", "dve", "pool", "sp")


class Tk:
    __slots__ = ("sem", "val", "snap")

    def __init__(s, sem, val, snap):
        s.sem = sem
        s.val = val
        s.snap = snap


class R:
    __slots__ = ("w", "rs")

    def __init__(s):
        s.w = None
        s.rs = {}


class DS:
    def __init__(s, sem):
        s.sem = sem
        s.count = 0


class Prog:
    def __init__(s, psem):
        s.q = {e: [] for e in ENGS}
        s.cnt = {e: 0 for e in ENGS}
        s.vc = {e: {} for e in ENGS}
        s.psem = psem
        s.dma_toks = []

    def _waits(s, eng, reads, writes):
        vc = s.vc[eng]
        need = {}
        toks = []
        for r in reads:
            if r.w is not None:
                toks.append(r.w)
        for r in writes:
            if r.w is not None:
                toks.append(r.w)
            toks.extend(r.rs.values())
        for t in toks:
            if eng == "pe" and t.sem is s.psem["pe"]:
                continue
            k = id(t.sem)
            if vc.get(k, 0) >= t.val:
                continue
            if k not in need or need[k].val < t.val:
                need[k] = t
        return list(need.values())

    def _absorb(s, eng, waits):
        vc = s.vc[eng]
        for t in waits:
            for k, v in t.snap.items():
                if vc.get(k, 0) < v:
                    vc[k] = v
            k = id(t.sem)
            if vc.get(k, 0) < t.val:
                vc[k] = t.val

    def op(s, eng, fn, reads=(), writes=()):
        waits = s._waits(eng, reads, writes)
        s._absorb(eng, waits)
        s.cnt[eng] += 1
        sem = s.psem[eng]
        tok = Tk(sem, s.cnt[eng], dict(s.vc[eng]))
        wl = [(t.sem, t.val) for t in waits]

        def emit(e):
            for sm, v in wl:
                e.wait_ge(sm, v)
            fn(e).then_inc(sem, 1)

        s.q[eng].append(emit)
        for r in reads:
            r.rs[id(sem)] = tok
        for r in writes:
            r.w = tok
            r.rs = {}
        return tok

    def dma(s, eng, ds, fn, reads=(), writes=()):
        waits = s._waits(eng, reads, writes)
        s._absorb(eng, waits)
        ds.count += 16
        tok = Tk(ds.sem, ds.count, dict(s.vc[eng]))
        wl = [(t.sem, t.val) for t in waits]
        sem = ds.sem

        def emit(e):
            for sm, v in wl:
                e.wait_ge(sm, v)
            fn(e).then_inc(sem, 16)

        s.q[eng].append(emit)
        for r in reads:
            r.rs[id(sem)] = tok
        for r in writes:
            r.w = tok
            r.rs = {}
        s.dma_toks.append(tok)
        return tok

    def barrier(s):
        toks = [Tk(s.psem[e], s.cnt[e], dict(s.vc[e])) for e in ENGS if s.cnt[e] > 0 and e != "sp"]
        toks += s.dma_toks
        s.dma_toks = []
        for eng in ENGS:
            vc = s.vc[eng]
            need = {}
            for t in toks:
                k = id(t.sem)
                if vc.get(k, 0) >= t.val:
                    continue
                if k not in need or need[k].val < t.val:
                    need[k] = t
            waits = list(need.values())
            s._absorb(eng, waits)
            wl = [(t.sem, t.val) for t in waits]
            if wl:
                def emit(e, wl=wl):
                    for sm, v in wl:
                        e.wait_ge(sm, v)
                s.q[eng].append(emit)

    def wait_all(s, eng, toks):
        wl = [(t.sem, t.val) for t in toks]

        def emit(e):
            for sm, v in wl:
                e.wait_ge(sm, v)
        s.q[eng].append(emit)


def host_consts():
    p = np.arange(128)[:, None]
    f = np.arange(128)[None, :]
    c = {}
    c["ident"] = (p == f).astype(np.float32)
    c["ut_f"] = (p <= f).astype(np.float32)
    c["ut_b"] = (p >= f).astype(np.float32)
    c["sl_f"] = (p > f).astype(np.float32)
    c["sl_b"] = (p < f).astype(np.float32)
    c["neg_f"] = (-BIG * (f < p)).astype(np.float32)
    c["neg_b"] = (-BIG * (f > p)).astype(np.float32)
    c["str_f"] = (f > p).astype(np.float32)
    c["str_b"] = (f < p).astype(np.float32)
    c["ones"] = np.ones((128, 128), np.float32)
    c["iota"] = np.tile(np.arange(256, dtype=np.float32)[None, :], (128, 1))
    tok = np.zeros((128, 16, 2), np.float32)
    tok[:, :, 0] = np.arange(16, dtype=np.float32)[None, :]
    tok[:, :, 1] = np.arange(128, dtype=np.float32)[:, None]
    c["tok"] = tok.reshape(128, 32)
    invc = np.zeros((128, 4, 16), np.float32)
    for g, w in enumerate((2, 4, 8, 16)):
        lo = w // 2
        hi = w - lo - 1
        for t in range(lo):
            invc[:, g, t] = 1.0 / (t + hi + 1)
        for i in range(hi):
            t = T - hi + i
            invc[:, g, 8 + i] = 1.0 / (T - t + lo)
    c["invc"] = invc.reshape(128, 64)
    return c


CONST_NAMES = ["ident", "ut_f", "ut_b", "sl_f", "sl_b", "neg_f", "neg_b", "str_f", "str_b", "ones"]


def build_nc():
    nc = bass.Bass("TRN2", target_bir_lowering=False)

    def din(name, shape, dt=F32):
        return nc.dram_tensor(name, list(shape), dt, kind="ExternalInput").ap()

    x_d = din("x", [T, D])
    win_d = din("w_in", [D, INC])
    cw_d = din("conv_wP", [128, 60])
    abp_d = din("abp", [128, 16])
    nw_d = [din("nw%d" % i, [128, D]) for i in range(3)]
    hnw_d = din("hnw", [128, 128])
    pw_d = din("pool_wP", [128, 512])
    psc_d = din("pool_scT", [128, 4])
    wout_d = din("w_out", [D, D])
    rw_d = din("router_wP", [128, 128])
    wg_d = din("wg", [E, D, FF])
    wu_d = din("wu", [E, D, FF])
    wd_d = din("wd", [E, FF, D])
    cst_d = {n: din("c_" + n, [128, 128]) for n in CONST_NAMES}
    iota_d = din("c_iota", [128, 256])
    invc_d = din("c_invc", [128, 64])
    out_d = nc.dram_tensor("out", [T, D], F32, kind="ExternalOutput").ap()
    xn2_d = nc.dram_tensor("xn2_scr", [T, D], BF16, kind="Internal").ap()
    NPC = 8
    wgb_d = nc.dram_tensor("wg_bf", [NPC, D, FF], BF16, kind="Internal").ap()
    wub_d = nc.dram_tensor("wu_bf", [NPC, D, FF], BF16, kind="Internal").ap()
    wdb_d = nc.dram_tensor("wd_bf", [NPC, FF, D], BF16, kind="Internal").ap()
    tok_d = din("c_tok", [128, 32])

    es = ExitStack()
    with es:
        def sb(name, shape, dt):
            return es.enter_context(nc.sbuf_tensor(name, list(shape), dt))

        def pstile(name):
            return es.enter_context(nc.psum_tensor(name, [128, 512], F32))

        psem = {e: es.enter_context(nc.semaphore("ps_" + e)) for e in ENGS}
        P = Prog(psem)
        out_toks = []

        def newds(name):
            return DS(es.enter_context(nc.semaphore(name)))

        cst = {n: sb("k_" + n, [128, 128], F32) for n in CONST_NAMES}
        identb = sb("identb", [128, 128], BF16)
        onesb = sb("onesb", [128, 128], BF16)
        ltb = sb("ltb", [128, 128], BF16)
        iota = sb("iota", [128, 256], F32)
        tokf = sb("tokf", [128, 32], F32)
        tokb = sb("tokb", [128, 16, 2], BF16)
        invc = sb("invc", [128, 64], F32)
        cw = sb("cw", [128, 12, 5], F32)
        abp = sb("abp_s", [128, 16], F32)
        hnw = sb("hnw_s", [128, 128], F32)
        psc = sb("psc", [128, 4], F32)
        pwb = sb("pwb", [128, 4, 128], BF16)
        rws = sb("rws", [128, 8, 16], F32)
        epsc = sb("epsc", [128, 1], F32)
        onec = sb("onec", [128, 1], F32)
        mhalf = sb("mhalf", [128, 1], F32)
        small = sb("small", [128, 64], F32)
        gsm = sb("gsm", [128, 16, 8], F32)
        bet = sb("bet", [128, 16, 8], F32)
        nbet = sb("nbet", [128, 16, 8], F32)
        exps = sb("exps", [128, 16, 24], F32)
        prob = sb("prob", [128, 16, 16], F32)
        sel = sb("sel", [128, 16, 16], F32)
        selb = sb("selb", [128, 16, 16], BF16)
        posm = sb("posm", [128, 16, 16], F32)
        R_c = R()
        R_g = R()
        R_exps = R()
        R_prob = R()
        R_sel = R()
        R_posm = R()
        R_small = R()

        ARENA_B = 190 * 1024
        arena = sb("arena", [128, ARENA_B // 2], BF16)

        def view(off, shape, dt):
            n = 1
            for s_ in shape[1:]:
                n *= s_
            nb = n * (4 if dt in (F32, U32) else 2)
            assert off % 4 == 0 and off + nb <= ARENA_B, (off, nb)
            v = arena[:, off // 2:(off + nb) // 2]
            if dt in (F32, U32):
                v = v.bitcast(dt)
            if len(shape) == 2:
                return v
            if len(shape) == 3:
                return v.rearrange("p (a b) -> p a b", b=shape[2])
            if len(shape) == 4:
                return v.rearrange("p (a b c) -> p a b c", b=shape[2], c=shape[3])
            raise ValueError

        K = 1024
        OFF_A, OFF_B, OFF_C, OFF_D, OFF_E, OFF_F = 0, 64 * K, 96 * K, 128 * K, 144 * K, 160 * K

        ps = [pstile("ps%d" % i) for i in range(8)]
        R_ps = [R() for _ in range(8)]

        def psb(i, dt=F32):
            return ps[i][:, :] if dt == F32 else ps[i][:, :].bitcast(BF16)

        ds_c = newds("ds_c")
        ds_cp = newds("ds_cp")
        ds_x = [newds("ds_x0"), newds("ds_x1")]
        ds_x4 = ds_x + [newds("ds_x2"), newds("ds_x3")]
        ds_w = [newds("ds_w%d" % i) for i in range(12)]
        ds_o = [newds("ds_o0"), newds("ds_o1")]
        ds_s = [newds("ds_s0"), newds("ds_s1")]
        ds_pc = [newds("ds_pc%d" % i) for i in range(NPC)]
        pc_jobs = []
        for i_ in range(NPC):
            for r0 in range(0, D, 128):
                pc_jobs.append((i_, wgb_d[i_, r0:r0 + 128, :], wg_d[E - NPC + i_, r0:r0 + 128, :]))
                pc_jobs.append((i_, wub_d[i_, r0:r0 + 128, :], wu_d[E - NPC + i_, r0:r0 + 128, :]))
            for r0 in range(0, FF, 256):
                pc_jobs.append((i_, wdb_d[i_, r0:r0 + 256, :].rearrange("(a p) d -> p a d", p=128), wd_d[E - NPC + i_, r0:r0 + 256, :].rearrange("(a p) d -> p a d", p=128)))
        pc_next = [0]
        pc_toks = [[] for _ in range(NPC)]

        def precast(n):
            for _ in range(n):
                if pc_next[0] >= len(pc_jobs):
                    return
                i_, dst, src = pc_jobs[pc_next[0]]
                pc_next[0] += 1
                pc_toks[i_].append(P.dma("pool", ds_pc[i_], lambda e, dst=dst, src=src: e.dma_start(out=dst, in_=src)))
        ds_g = [newds("ds_g0"), newds("ds_g1")]

        for n in CONST_NAMES:
            P.dma("sp", ds_c, lambda e, n=n: e.dma_start(out=cst[n][:, :], in_=cst_d[n]), writes=[R_c])
        P.dma("sp", ds_c, lambda e: e.dma_start(out=iota[:, :], in_=iota_d), writes=[R_c])
        P.dma("sp", ds_c, lambda e: e.dma_start(out=tokf[:, :], in_=tok_d), writes=[R_c])
        P.dma("sp", ds_c, lambda e: e.dma_start(out=invc[:, :], in_=invc_d), writes=[R_c])
        P.dma("sp", ds_c, lambda e: e.dma_start(out=cw[:, :, :].rearrange("p c j -> p (c j)"), in_=cw_d), writes=[R_c])
        P.dma("sp", ds_c, lambda e: e.dma_start(out=abp[:, :], in_=abp_d), writes=[R_c])
        P.dma("sp", ds_c, lambda e: e.dma_start(out=hnw[:, :], in_=hnw_d), writes=[R_c])
        P.dma("sp", ds_c, lambda e: e.dma_start(out=psc[:, :], in_=psc_d), writes=[R_c])
        P.dma("sp", ds_c, lambda e: e.dma_start(out=rws[:, :, :].rearrange("p c e -> p (c e)"), in_=rw_d), writes=[R_c])
        P.dma("pool", ds_cp, lambda e: e.dma_start(out=pwb[:, :, :].rearrange("p g d -> p (g d)"), in_=pw_d), writes=[R_c])
        P.op("pool", lambda e: e.tensor_copy(out=identb[:, :], in_=cst["ident"][:, :]), reads=[R_c], writes=[R_c])
        P.op("pool", lambda e: e.tensor_copy(out=onesb[:, :], in_=cst["ones"][:, :]), reads=[R_c], writes=[R_c])
        P.op("pool", lambda e: e.tensor_copy(out=ltb[:, :], in_=cst["sl_b"][:, :]), reads=[R_c], writes=[R_c])
        P.op("pool", lambda e: e.tensor_copy(out=tokb[:, :, :].rearrange("p a b -> p (a b)"), in_=tokf[:, :]), reads=[R_c], writes=[R_c])
        P.op("pool", lambda e: e.memset(epsc[:, :], EPS), writes=[R_c])
        P.op("pool", lambda e: e.memset(onec[:, :], 1.0), writes=[R_c])
        P.op("pool", lambda e: e.memset(mhalf[:, :], -0.5), writes=[R_c])
        P.op("act", lambda e: e.activation(out=small[:, 0:8], in_=abp[:, 0:8], func=AF.Exp), reads=[R_c], writes=[R_c])
        P.op("dve", lambda e: e.tensor_scalar(out=small[:, 0:8], in0=small[:, 0:8], scalar1=-1.0, scalar2=None, op0=ALU.mult), reads=[R_c], writes=[R_c])
        P.barrier()
        RC = [R_c]

        ident = cst["ident"]

        XNT = view(OFF_A, [128, 8, T], BF16)
        WINP = [view(OFF_A + 32 * K + i * 8704, [128, 8, 528], BF16) for i in range(2)]
        RAW = view(OFF_A + 32 * K + 17408, [128, 2056], F32)
        RAWS = [RAW, view(OFF_D, [128, 2056], F32)]
        NWBC1 = view(OFF_A + 32 * K + 17408 + 8224, [128, D], F32)
        SB2 = view(OFF_A + 32 * K + 17408, [128, T + 32], F32)
        QT = view(OFF_B, [128, H, T], BF16)
        KT = view(OFF_B + 16 * K, [128, H, T], BF16)
        KTOK = view(OFF_C, [128, NTC, H, 128], BF16)
        VTOK = view(OFF_C + 16 * K, [128, NTC, H, 128], BF16)
        ZS = view(OFF_D, [128, NTC, 512], BF16)
        OPT = view(OFF_E, [128, 4, T], BF16)
        SQ = view(OFF_E, [128, T], BF16)
        VTMP = view(OFF_E, [128, T], BF16)
        RSQ = view(OFF_E + 4 * K, [128, T], F32)
        XT = [view(OFF_F + i * 4 * K, [128, D], F32) for i in range(2)]
        XT4 = XT + [view(OFF_F + 25 * K, [128, D], F32), view(OFF_D + 11 * K, [128, D], F32)]
        XN = view(OFF_F + 8 * K, [128, D], BF16)
        CONVT = view(OFF_F + 10 * K, [128, T], F32)
        CONVS = [CONVT, view(OFF_F + 21 * K, [128, T], F32)]
        UB = view(OFF_F + 10 * K, [128, T + 32], F32)
        ZF = view(OFF_F + 10 * K, [128, 512], F32)
        ABS = view(OFF_F + 19 * K, [128, 16, 16], F32)
        TM1 = view(OFF_F + 20 * K, [128, 16, 8], F32)
        PB3 = view(OFF_F + 21 * K, [128, T + 32], F32)
        DIFB = view(OFF_F, [128, T], BF16)
        R_xnt = [R() for _ in range(NTC)]
        R_xt = [R(), R()]
        R_xt4 = R_xt + [R(), R()]
        R_xn = R()
        R_winp = [R(), R()]
        R_raw = R()
        R_acc = R()
        R_raws = [R_raw, R()]
        R_accs = [R_acc, R()]
        R_nw = R()
        R_qt = [R() for _ in range(H)]
        R_kt = [R() for _ in range(H)]
        R_ktok = R()
        R_vtok = R()
        R_zs = [R() for _ in range(NTC)]
        R_opt = [R() for _ in range(4)]
        R_abs = R()
        R_pb3 = R()
        R_difb = R()

        P.dma("sp", ds_c, lambda e: e.dma_start(out=NWBC1[:, :], in_=nw_d[0]), writes=[R_nw])
        P.op("pool", lambda e: e.memset(RAW[:, :], 0.0), writes=[R_raw])
        P.op("pool", lambda e: e.memset(RAWS[1][:, :], 0.0), writes=[R_raws[1]])

        XNS = [XN, view(OFF_D + 9 * K, [128, D], BF16)]
        R_xns = [R_xn, R()]
        R_sm1 = [R(), R()]
        JUNK = view(OFF_F + 21 * K, [128, D], F32)
        R_junk = R()
        def front_1a(tc):
            b = tc % 2
            sc = 8 + 3 * b
            xb = tc % 4
            P.dma("sp", ds_x4[xb], lambda e: e.dma_start(out=XT4[xb][:, :], in_=x_d[tc * 128:(tc + 1) * 128, :]), writes=[R_xt4[xb]])
            P.op("act", lambda e: e.activation(out=JUNK[:, :], in_=XT4[xb][:, :], func=AF.Square, accum_out=small[:, sc:sc + 1]),
                 reads=[R_xt4[xb]], writes=[R_junk, R_sm1[b]])
            P.op("act", lambda e: e.activation(out=small[:, sc + 1:sc + 2], in_=small[:, sc:sc + 1], func=AF.Sqrt, bias=epsc[:, :], scale=1.0 / D),
                 reads=[R_c], writes=[R_sm1[b]])
            P.op("dve", lambda e: e.reciprocal(out=small[:, sc + 2:sc + 3], in_=small[:, sc + 1:sc + 2]), reads=[], writes=[R_sm1[b]])
            P.op("dve", lambda e: e.scalar_tensor_tensor(out=XNS[b][:, :], in0=XT4[xb][:, :], scalar=small[:, sc + 2:sc + 3], in1=NWBC1[:, :],
                                                         op0=ALU.mult, op1=ALU.mult),
                 reads=[R_xt4[xb], R_sm1[b], R_nw], writes=[R_xns[b]])

        def back_1a(tc):
            b = tc % 2
            pb = tc % 2
            for dc in range(8):
                P.op("pe", lambda e, dc=dc: e.transpose(psb(pb, BF16)[:, dc * 128:(dc + 1) * 128], XNS[b][:, dc * 128:(dc + 1) * 128], identb[:, :]),
                     reads=[R_xns[b], R_c], writes=[R_ps[pb]])
            P.op("act", lambda e: e.activation(out=XNT[:, :, tc * 128:(tc + 1) * 128],
                                               in_=psb(pb, BF16)[:, 0:1024].rearrange("p (a b) -> p a b", b=128), func=AF.Copy),
                 reads=[], writes=[R_ps[pb], R_xnt[tc]])

        front_1a(0)
        for tc in range(NTC):
            if tc + 1 < NTC:
                front_1a(tc + 1)
            back_1a(tc)

        wpi = [0]

        def load_winp(col0, ncol):
            i = wpi[0] % 2
            wpi[0] += 1
            P.dma("pool", ds_w[i], lambda e, i=i: e.dma_start(out=WINP[i][:, :, 0:ncol],
                                                              in_=win_d.rearrange("(c p) n -> p c n", p=128)[:, :, col0:col0 + ncol]),
                  writes=[R_winp[i]])
            return i

        def proj_chunk(wi, lcol, evac):
            for tb in range(4):
                pb = 2 + tb % 2
                for dc in range(8):
                    P.op("pe", lambda e, dc=dc, tb=tb, pb=pb: e.matmul(psb(pb), WINP[wi][:, dc, lcol:lcol + 128], XNT[:, dc, tb * 512:(tb + 1) * 512],
                                                                      start=(dc == 0), stop=(dc == 7)),
                         reads=[R_winp[wi]] + R_xnt[tb * 4:(tb + 1) * 4], writes=[R_ps[pb]])
                evac(tb, pb)

        def mk_evac_raw(RAWB, R_rawb):
            def evac_raw(tb, pb):
                P.op("act", lambda e, tb=tb, pb=pb: e.activation(out=RAWB[:, 2 + tb * 512: 2 + (tb + 1) * 512], in_=psb(pb), func=AF.Copy),
                     reads=[], writes=[R_ps[pb], R_rawb])
            return evac_raw

        RSQS = [RSQ, RSQ]
        R_rsqs = [R_opt[1], R_opt[1]]
        chunks = [(grp, kind, hh) for grp, kind in enumerate(("q", "k", "v")) for hh in range(H)]
        wis = {}

        def stage_a(ci):
            grp, kind, hh = chunks[ci]
            if hh == 0:
                wis[grp] = load_winp(grp * 512, 512)
            wi = wis[grp]
            cc = grp * 4 + hh
            RAWB, R_rawb = RAWS[cc % 2], R_raws[cc % 2]
            CONVB, R_accb = CONVS[cc % 2], R_accs[cc % 2]
            proj_chunk(wi, hh * 128, mk_evac_raw(RAWB, R_rawb))

        def stage_c(ci):
            grp, kind, hh = chunks[ci]
            cc = grp * 4 + hh
            RAWB, R_rawb = RAWS[cc % 2], R_raws[cc % 2]
            CONVB, R_accb = CONVS[cc % 2], R_accs[cc % 2]
            P.op("dve", lambda e: e.tensor_scalar(out=CONVB[:, :], in0=RAWB[:, 0:T], scalar1=cw[:, cc, 0:1], scalar2=None, op0=ALU.mult),
                 reads=[R_rawb, R_c], writes=[R_accb])
            for j in range(1, 5):
                P.op("dve", lambda e, j=j: e.scalar_tensor_tensor(out=CONVB[:, :], in0=RAWB[:, j:j + T], scalar=cw[:, cc, j:j + 1], in1=CONVB[:, :],
                                                                  op0=ALU.mult, op1=ALU.add),
                     reads=[R_rawb, R_c], writes=[R_accb])
            if kind == "v":
                P.op("act", lambda e: e.activation(out=VTMP[:, :], in_=CONVB[:, :], func=AF.Silu), reads=[R_accb], writes=[R_opt[0]])
                for half in range(2):
                    pb = 4 + half
                    for t8 in range(8):
                        tc = half * 8 + t8
                        P.op("pe", lambda e, tc=tc, t8=t8, pb=pb: e.transpose(psb(pb, BF16)[:, t8 * 128:(t8 + 1) * 128], VTMP[:, tc * 128:(tc + 1) * 128], identb[:, :]),
                             reads=[R_opt[0], R_c], writes=[R_ps[pb]])
                    P.op("act", lambda e, half=half, pb=pb: e.activation(out=VTOK[:, half * 8:(half + 1) * 8, hh, :],
                                                                        in_=psb(pb, BF16)[:, 0:1024].rearrange("p (a b) -> p a b", b=128), func=AF.Copy),
                         reads=[], writes=[R_ps[pb], R_vtok])
            else:
                P.op("act", lambda e: e.activation(out=CONVB[:, :], in_=CONVB[:, :], func=AF.Silu), reads=[], writes=[R_accb])
                P.op("pool", lambda e: e.tensor_tensor(out=SQ[:, :], in0=CONVB[:, :], in1=CONVB[:, :], op=ALU.mult), reads=[R_accb], writes=[R_opt[0]])

        def stage_a2(ci):
            grp, kind, hh = chunks[ci]
            cc = grp * 4 + hh
            RSQB, R_rsqb = RSQS[cc % 2], R_rsqs[cc % 2]
            if kind != "v":
                for tb in range(4):
                    pb = 4 + tb % 2
                    P.op("pe", lambda e, tb=tb, pb=pb: e.matmul(psb(pb), onesb[:, :], SQ[:, tb * 512:(tb + 1) * 512], start=True, stop=True),
                         reads=[R_opt[0], R_c], writes=[R_ps[pb]])
                    P.op("act", lambda e, tb=tb, pb=pb: e.activation(out=RSQB[:, tb * 512:(tb + 1) * 512], in_=psb(pb), func=AF.Ln, bias=epsc[:, :], scale=1.0),
                         reads=[R_c], writes=[R_ps[pb], R_rsqb])

        def stage_b(ci):
            grp, kind, hh = chunks[ci]
            if kind == "v":
                return
            cc = grp * 4 + hh
            CONVB, R_accb = CONVS[cc % 2], R_accs[cc % 2]
            RSQB, R_rsqb = RSQS[cc % 2], R_rsqs[cc % 2]
            P.op("act", lambda e: e.activation(out=RSQB[:, :], in_=RSQB[:, :], func=AF.Exp, scale=-0.5), reads=[], writes=[R_rsqb])
            dst = QT if kind == "q" else KT
            rdst = R_qt[hh] if kind == "q" else R_kt[hh]
            scl = 128.0 ** -0.5 if kind == "q" else 1.0
            P.op("dve", lambda e: e.scalar_tensor_tensor(out=dst[:, hh, :], in0=CONVB[:, :], scalar=scl, in1=RSQB[:, :], op0=ALU.mult, op1=ALU.mult),
                 reads=[R_accb, R_rsqb], writes=[rdst])
            if kind == "k":
                for half in range(2):
                    pb = 6 + half
                    for t8 in range(8):
                        tc = half * 8 + t8
                        P.op("pe", lambda e, tc=tc, t8=t8, pb=pb: e.transpose(psb(pb, BF16)[:, t8 * 128:(t8 + 1) * 128], KT[:, hh, tc * 128:(tc + 1) * 128], identb[:, :]),
                             reads=[R_kt[hh], R_c], writes=[R_ps[pb]])
                    P.op("act", lambda e, half=half, pb=pb: e.activation(out=KTOK[:, half * 8:(half + 1) * 8, hh, :],
                                                                        in_=psb(pb, BF16)[:, 0:1024].rearrange("p (a b) -> p a b", b=128), func=AF.Copy),
                         reads=[], writes=[R_ps[pb], R_ktok])

        NCH = len(chunks)
        stage_a(0)
        for ci in range(NCH + 1):
            if ci + 1 < NCH:
                stage_a(ci + 1)
            if ci < NCH:
                stage_c(ci)
            if ci >= 1:
                stage_b(ci - 1)
            if ci < NCH:
                stage_a2(ci)
        P.barrier()

        wi = load_winp(1536, 528)
        for tc in range(NTC):
            pb = 2 + tc % 2
            for dc in range(8):
                P.op("pe", lambda e, dc=dc, tc=tc, pb=pb, wi=wi: e.matmul(psb(pb), XNT[:, dc, tc * 128:(tc + 1) * 128], WINP[wi][:, dc, 0:512], start=(dc == 0), stop=(dc == 7)),
                     reads=[R_winp[wi], R_xnt[tc]], writes=[R_ps[pb]])
            P.op("act", lambda e, pb=pb: e.activation(out=ZF[:, :], in_=psb(pb), func=AF.Silu), reads=[], writes=[R_ps[pb], R_acc])
            P.op("dve", lambda e, tc=tc: e.tensor_tensor(out=ZS[:, tc, :].rearrange("p (h d) -> p h d", d=128), in0=ZF[:, :].rearrange("p (h d) -> p h d", d=128),
                                                         in1=hnw[:, :].unsqueeze(1).to_broadcast([128, 4, 128]), op=ALU.mult),
                 reads=[R_acc, R_c], writes=[R_zs[tc]])
        ABV = psb(4)[:, 0:256].rearrange("p (a b) -> p a b", b=16)
        for tc in range(NTC):
            for dc in range(8):
                P.op("pe", lambda e, dc=dc, tc=tc, wi=wi: e.matmul(ABV[:, tc, :], XNT[:, dc, tc * 128:(tc + 1) * 128], WINP[wi][:, dc, 512:528], start=(dc == 0), stop=(dc == 7)),
                     reads=[R_winp[wi], R_xnt[tc]], writes=[R_ps[4]])
        P.op("act", lambda e: e.activation(out=ABS[:, :, :], in_=ABV, func=AF.Copy), reads=[], writes=[R_ps[4], R_abs])
        ABS5 = ABS[:, :, :].rearrange("p a (d k h) -> p a d k h", d=2, k=2)
        for d_ in range(2):
            P.op("dve", lambda e, d_=d_: e.tensor_tensor(out=TM1[:, :, d_ * 4:(d_ + 1) * 4], in0=ABS5[:, :, d_, 0, :],
                                                         in1=abp[:, 8 + d_ * 4: 12 + d_ * 4].unsqueeze(1).to_broadcast([128, 16, 4]), op=ALU.add),
                 reads=[R_abs, R_c], writes=[R_g])
            P.op("act", lambda e, d_=d_: e.activation(out=bet[:, :, d_ * 4:(d_ + 1) * 4], in_=ABS5[:, :, d_, 1, :], func=AF.Sigmoid),
                 reads=[R_abs], writes=[R_g])
        P.op("act", lambda e: e.activation(out=TM1[:, :, :], in_=TM1[:, :, :], func=AF.Exp), reads=[], writes=[R_g])
        P.op("act", lambda e: e.activation(out=TM1[:, :, :], in_=TM1[:, :, :], func=AF.Ln, bias=onec[:, :], scale=1.0), reads=[R_c], writes=[R_g])
        P.op("dve", lambda e: e.tensor_tensor(out=gsm[:, :, :], in0=TM1[:, :, :], in1=small[:, 0:8].unsqueeze(1).to_broadcast([128, 16, 8]), op=ALU.mult),
             reads=[R_c], writes=[R_g])
        P.op("dve", lambda e: e.tensor_scalar(out=nbet[:, :, :], in0=bet[:, :, :], scalar1=-1.0, scalar2=None, op0=ALU.mult), reads=[], writes=[R_g])

        wi = load_winp(2064, 512)
        TW = T + 8
        P.op("pool", lambda e: e.memset(UB[:, :], 0.0), reads=[], writes=[R_acc])
        P.op("pool", lambda e: e.memset(SB2[:, :], 0.0), reads=[], writes=[R_raw, R_nw])
        P.op("pool", lambda e: e.memset(PB3[:, :], 0.0), reads=[], writes=[R_pb3])

        def evac_u(tb, pb):
            P.op("act", lambda e, tb=tb, pb=pb: e.activation(out=UB[:, 16 + tb * 512: 16 + (tb + 1) * 512], in_=psb(pb), func=AF.Copy),
                 reads=[], writes=[R_ps[pb], R_acc])

        for g, w in enumerate((2, 4, 8, 16)):
            lo = w // 2
            hi = w - lo - 1
            proj_chunk(wi, g * 128, evac_u)
            s_prev, r_prev = UB, R_acc
            sh = 1
            for lv in range({2: 1, 4: 2, 8: 3, 16: 4}[w]):
                dstb, rdst = (SB2, R_raw) if lv % 2 == 0 else (PB3, R_pb3)
                P.op("dve", lambda e, s_prev=s_prev, dstb=dstb, sh=sh: e.tensor_tensor(out=dstb[:, 16:16 + TW], in0=s_prev[:, 16:16 + TW], in1=s_prev[:, 16 - sh:16 - sh + TW], op=ALU.add),
                     reads=[r_prev], writes=[rdst])
                s_prev, r_prev = dstb, rdst
                sh *= 2
            P.op("dve", lambda e, s_prev=s_prev, hi=hi, w=w: e.scalar_tensor_tensor(out=DIFB[:, :], in0=s_prev[:, 16 + hi:16 + hi + T], scalar=1.0 / w, in1=UB[:, 16:16 + T],
                                                                                   op0=ALU.mult, op1=ALU.subtract),
                 reads=[r_prev, R_acc], writes=[R_difb])
            P.op("dve", lambda e, s_prev=s_prev, hi=hi, lo=lo, g=g: e.tensor_tensor(out=small[:, 16:16 + lo], in0=s_prev[:, 16 + hi:16 + hi + lo], in1=invc[:, g * 16:g * 16 + lo], op=ALU.mult),
                 reads=[r_prev, R_c], writes=[R_small])
            P.op("dve", lambda e, lo=lo: e.tensor_tensor(out=DIFB[:, 0:lo], in0=small[:, 16:16 + lo], in1=UB[:, 16:16 + lo], op=ALU.subtract),
                 reads=[R_small, R_acc], writes=[R_difb])
            if hi > 0:
                P.op("dve", lambda e, s_prev=s_prev, hi=hi, g=g: e.tensor_tensor(out=small[:, 32:32 + hi], in0=s_prev[:, 16 + T:16 + T + hi], in1=invc[:, g * 16 + 8:g * 16 + 8 + hi], op=ALU.mult),
                     reads=[r_prev, R_c], writes=[R_small])
                P.op("dve", lambda e, hi=hi: e.tensor_tensor(out=DIFB[:, T - hi:T], in0=small[:, 32:32 + hi], in1=UB[:, 16 + T - hi:16 + T], op=ALU.subtract),
                     reads=[R_small, R_acc], writes=[R_difb])
            for tb in range(4):
                pb = 4 + tb % 2
                P.op("pe", lambda e, tb=tb, pb=pb, g=g: e.matmul(psb(pb), pwb[:, g, :], DIFB[:, tb * 512:(tb + 1) * 512], start=True, stop=True),
                     reads=[R_difb, R_c], writes=[R_ps[pb]])
                P.op("act", lambda e, tb=tb, pb=pb, g=g: e.activation(out=OPT[:, g, tb * 512:(tb + 1) * 512], in_=psb(pb), func=AF.Identity, scale=psc[:, g:g + 1]),
                     reads=[R_c], writes=[R_ps[pb], R_opt[g]])
        P.barrier()

        if DEBUG:
            ds_dbg = newds("ds_dbg")
            for name, ap, shape, dt in (("d_qt", QT, [128, H * T], BF16), ("d_kt", KT, [128, H * T], BF16),
                                        ("d_ktok", KTOK, [128, NTC * H * 128], BF16), ("d_vtok", VTOK, [128, NTC * H * 128], BF16),
                                        ("d_zs", ZS, [128, NTC * 512], BF16), ("d_opt", OPT, [128, 4 * T], BF16)):
                dd = nc.dram_tensor(name, shape, dt, kind="ExternalOutput").ap()
                flat = ap
                if len(ap.shape) == 3:
                    flat = ap.rearrange("p a b -> p (a b)")
                elif len(ap.shape) == 4:
                    flat = ap.rearrange("p a b c -> p (a b c)")
                out_toks.append(P.dma("sp", ds_dbg, lambda e, dd=dd, flat=flat: e.dma_start(out=dd, in_=flat)))
            for name, tl in (("d_gsm", gsm), ("d_bet", bet)):
                dd = nc.dram_tensor(name, [128, 128], F32, kind="ExternalOutput").ap()
                out_toks.append(P.dma("sp", ds_dbg, lambda e, dd=dd, tl=tl: e.dma_start(out=dd, in_=tl[:, :, :].rearrange("p a b -> p (a b)"))))

        NEU = F32
        OACC = view(OFF_A, [128, NTC, H, 128], F32)

        def make_ctx(base, limit, banks):
            o2 = [base]

            def tv(shape, dt):
                n = 1
                for s_ in shape[1:]:
                    n *= s_
                nb = n * (4 if dt == F32 else 2)
                off = o2[0]
                o2[0] += nb
                assert o2[0] <= limit, (o2[0], limit)
                return view(off, shape, dt)
            c = {}
            for nm in ("MG", "EM", "EMS", "BYV"):
                c[nm] = tv([128, 4, 128], F32)
            for nm in ("AK", "ATK", "RRK"):
                c[nm] = [tv([128, 4, 128], NEU) for _ in range(2)]
            for nm in ("QKT", "NTB", "EK", "KDEC", "YWT", "VNEW"):
                c[nm] = tv([128, 4, 128], BF16)
            for nm in ("MG", "EM", "EMS", "BYV", "QKT", "NTB", "EK", "KDEC", "YWT", "VNEW"):
                c["R_" + nm] = R()
            for nm in ("AK", "ATK", "RRK"):
                c["R_" + nm] = [R(), R()]
            c["banks"] = banks
            c["end"] = o2[0]
            return c

        ctxs = [make_ctx(OFF_A + 32 * K, OFF_A + 64 * K, (0, 1, 2, 3)), make_ctx(OFF_F, ARENA_B, (4, 5, 6, 7))]
        SST = view(ctxs[0]["end"], [128, 8, 128], F32)
        SBF = view(ctxs[0]["end"] + 4 * K, [128, 8, 128], BF16)
        assert ctxs[0]["end"] + 6 * K <= OFF_A + 64 * K
        R_oacc = [R() for _ in range(NTC)]
        R_s = [R(), R()]
        R_sbf = [R(), R()]

        for n in range(NTC):
            P.op("pool", lambda e, n=n: e.memset(OACC[:, n, :, :], 0.0), writes=[R_oacc[n]])
        P.op("pool", lambda e: e.memset(SST[:, :, :], 0.0), writes=R_s)
        P.op("pool", lambda e: e.memset(SBF[:, :, :], 0.0), writes=R_sbf)

        EV = psb(0)[:, 0:384].rearrange("p (a b) -> p a b", b=24)
        for d_ in range(2):
            sfx = "_f" if d_ == 0 else "_b"
            for j, lh in enumerate((cst["ut" + sfx], cst["sl" + sfx], cst["ones"])):
                c0 = j * 8 + d_ * 4
                P.op("pe", lambda e, d_=d_, lh=lh, c0=c0: e.matmul(EV[:, :, c0:c0 + 4], lh[:, :], gsm[:, :, d_ * 4:(d_ + 1) * 4], start=True, stop=True),
                     reads=[R_c, R_g], writes=[R_ps[0]])
        P.op("act", lambda e: e.activation(out=exps[:, :, :], in_=EV, func=AF.Exp), reads=[], writes=[R_ps[0], R_exps])

        def v4(i):
            return psb(i).rearrange("p (h c) -> p h c", h=4)

        identbc = ident[:, :].unsqueeze(1).to_broadcast([128, 4, 128])

        def group(n, d_, c):
            sfx = "_f" if d_ == 0 else "_b"
            ut, sl, neg, st = cst["ut" + sfx], cst["sl" + sfx], cst["neg" + sfx], cst["str" + sfx]
            ns = slice(n * 128, (n + 1) * 128)
            c4 = d_ * 4
            b0, b1, b2, b3 = c["banks"]
            MG, EM, EMS, BYV, AK, ATK, RRK = c["MG"], c["EM"], c["EMS"], c["BYV"], c["AK"], c["ATK"], c["RRK"]
            QKT, NTB, EK, KDEC, YWT, VNEW = c["QKT"], c["NTB"], c["EK"], c["KDEC"], c["YWT"], c["VNEW"]
            R_mg, R_em, R_ems, R_byv, R_ak, R_atk, R_rrk = c["R_MG"], c["R_EM"], c["R_EMS"], c["R_BYV"], c["R_AK"], c["R_ATK"], c["R_RRK"]
            R_qkt, R_ntb, R_ek, R_kdec, R_ywt, R_vnew = c["R_QKT"], c["R_NTB"], c["R_EK"], c["R_KDEC"], c["R_YWT"], c["R_VNEW"]
            for h in range(4):
                P.op("pool", lambda e, h=h: e.tensor_scalar(out=MG[:, h, :], in0=sl[:, :], scalar1=gsm[:, n, c4 + h:c4 + h + 1], scalar2=1.0, op0=ALU.mult, op1=ALU.mult),
                     reads=[R_c, R_g], writes=[R_mg])
            yield
            DV = v4(b1)
            for h in range(4):
                P.op("pe", lambda e, h=h: e.matmul(DV[:, h, :], MG[:, h, :], ut[:, :], start=True, stop=False), reads=[R_mg, R_c], writes=[R_ps[b1]])
                P.op("pe", lambda e, h=h: e.matmul(DV[:, h, :], ident[:, :], neg[:, :], start=False, stop=True), reads=[R_c], writes=[R_ps[b1]])
            yield
            P.op("act", lambda e: e.activation(out=EM[:, :, :], in_=DV, func=AF.Exp), reads=[], writes=[R_ps[b1], R_em])
            P.op("pool", lambda e: e.tensor_tensor(out=EMS[:, :, :], in0=EM[:, :, :], in1=st[:, :].unsqueeze(1).to_broadcast([128, 4, 128]), op=ALU.mult),
                 reads=[R_em, R_c], writes=[R_ems])
            yield
            for hp in range(2):
                V4 = psb(b0).rearrange("p (h k c) -> p h k c", h=2, k=2)
                for hl in range(2):
                    h = hp * 2 + hl
                    P.op("pe", lambda e, h=h, hl=hl: e.matmul(V4[:, hl, 0, :], KT[:, h, ns], KT[:, h, ns], start=True, stop=True), reads=[R_kt[h]], writes=[R_ps[b0]])
                    P.op("pe", lambda e, h=h, hl=hl: e.matmul(V4[:, hl, 1, :], KT[:, h, ns], QT[:, h, ns], start=True, stop=True), reads=[R_kt[h], R_qt[h]], writes=[R_ps[b0]])
                yield
                for hl in range(2):
                    h = hp * 2 + hl
                    P.op("dve", lambda e, h=h, hl=hl: e.scalar_tensor_tensor(out=AK[0][:, h, :], in0=V4[:, hl, 0, :], scalar=nbet[:, n, c4 + h:c4 + h + 1], in1=EMS[:, h, :],
                                                                             op0=ALU.mult, op1=ALU.mult),
                         reads=[R_g, R_ems], writes=[R_ps[b0], R_ak[0]])
                P.op("dve", lambda e, hp=hp: e.tensor_tensor(out=QKT[:, 2 * hp:2 * hp + 2, :], in0=V4[:, :, 1, :], in1=EM[:, 2 * hp:2 * hp + 2, :], op=ALU.mult),
                     reads=[R_em], writes=[R_ps[b0], R_qkt])
                yield
            TV = psb(b1, BF16)[:, 0:512].rearrange("p (h c) -> p h c", h=4) if NEU == BF16 else v4(b1)
            idn = identb if NEU == BF16 else ident
            for h in range(4):
                P.op("pe", lambda e, h=h: e.transpose(TV[:, h, :], AK[0][:, h, :], idn[:, :]), reads=[R_ak[0], R_c], writes=[R_ps[b1]])
            yield
            P.op("act", lambda e: e.activation(out=ATK[0][:, :, :], in_=TV, func=AF.Copy), reads=[], writes=[R_ps[b1], R_atk[0]])
            P.op("pool", lambda e: e.tensor_tensor(out=RRK[0][:, :, :], in0=AK[0][:, :, :], in1=identbc, op=ALU.add), reads=[R_ak[0], R_c], writes=[R_rrk[0]])
            yield
            cur = 0
            AV, ATV, RV = v4(b2), v4(b3), v4(b1)
            for k in range(1, 7):
                nxt = 1 - cur
                for h in range(4):
                    P.op("pe", lambda e, h=h, cur=cur: e.matmul(ATV[:, h, :], AK[cur][:, h, :], ATK[cur][:, h, :], start=True, stop=True),
                         reads=[R_ak[cur], R_atk[cur]], writes=[R_ps[b3]])
                if k < 6:
                    for h in range(4):
                        P.op("pe", lambda e, h=h, cur=cur: e.matmul(AV[:, h, :], ATK[cur][:, h, :], AK[cur][:, h, :], start=True, stop=True),
                             reads=[R_ak[cur], R_atk[cur]], writes=[R_ps[b2]])
                yield
                P.op("act", lambda e, nxt=nxt: e.activation(out=ATK[nxt][:, :, :], in_=ATV, func=AF.Copy), reads=[], writes=[R_ps[b3], R_atk[nxt]])
                if k < 6:
                    P.op("dve", lambda e, nxt=nxt: e.tensor_copy(out=AK[nxt][:, :, :], in_=AV), reads=[], writes=[R_ps[b2], R_ak[nxt]])
                yield
                for h in range(4):
                    P.op("pe", lambda e, h=h, cur=cur, nxt=nxt: e.matmul(RV[:, h, :], ATK[nxt][:, h, :], RRK[cur][:, h, :], start=True, stop=True),
                         reads=[R_atk[nxt], R_rrk[cur]], writes=[R_ps[b1]])
                yield
                if k < 6:
                    P.op("dve", lambda e, cur=cur, nxt=nxt: e.tensor_tensor(out=RRK[nxt][:, :, :], in0=RV, in1=RRK[cur][:, :, :], op=ALU.add),
                         reads=[R_rrk[cur]], writes=[R_ps[b1], R_rrk[nxt]])
                else:
                    P.op("dve", lambda e, cur=cur: e.tensor_tensor(out=NTB[:, :, :], in0=RV, in1=RRK[cur][:, :, :], op=ALU.add),
                         reads=[R_rrk[cur]], writes=[R_ps[b1], R_ntb])
                cur = nxt
                yield
            for h in range(4):
                P.op("act", lambda e, h=h: e.activation(out=EK[:, h, :], in_=KTOK[:, n, h, :], func=AF.Identity, scale=exps[:, n, c4 + h:c4 + h + 1]),
                     reads=[R_ktok, R_exps], writes=[R_ek])
                P.op("act", lambda e, h=h: e.activation(out=KDEC[:, h, :], in_=KTOK[:, n, h, :], func=AF.Identity, scale=exps[:, n, 8 + c4 + h:8 + c4 + h + 1]),
                     reads=[R_ktok, R_exps], writes=[R_kdec])
            yield
            YVV, YWV = v4(b0), v4(b2)
            for h in range(4):
                P.op("pe", lambda e, h=h: e.matmul(YVV[:, h, :], NTB[:, h, :], VTOK[:, n, h, :], start=True, stop=True), reads=[R_ntb, R_vtok], writes=[R_ps[b0]])
            for h in range(4):
                P.op("pe", lambda e, h=h: e.matmul(YWV[:, h, :], EK[:, h, :], NTB[:, h, :], start=True, stop=True), reads=[R_ntb, R_ek], writes=[R_ps[b2]])
            yield
            for h in range(4):
                P.op("act", lambda e, h=h: e.activation(out=BYV[:, h, :], in_=YVV[:, h, :], func=AF.Identity, scale=bet[:, n, c4 + h:c4 + h + 1]),
                     reads=[R_g], writes=[R_ps[b0], R_byv])
            P.op("act", lambda e: e.activation(out=YWT[:, :, :], in_=YWV, func=AF.Copy), reads=[], writes=[R_ps[b2], R_ywt])
            yield
            P1V, O1V, O2V, SUV = v4(b1), v4(b3), v4(b2), v4(b0)
            for h in range(4):
                P.op("pe", lambda e, h=h: e.matmul(P1V[:, h, :], YWT[:, h, :], SBF[:, c4 + h, :], start=True, stop=True), reads=[R_ywt, R_sbf[d_]], writes=[R_ps[b1]])
            for h in range(4):
                P.op("pe", lambda e, h=h: e.matmul(O1V[:, h, :], QT[:, h, ns], SBF[:, c4 + h, :], start=True, stop=True), reads=[R_qt[h], R_sbf[d_]], writes=[R_ps[b3]])
            yield
            for h in range(4):
                P.op("dve", lambda e, h=h: e.scalar_tensor_tensor(out=VNEW[:, h, :], in0=P1V[:, h, :], scalar=nbet[:, n, c4 + h:c4 + h + 1], in1=BYV[:, h, :],
                                                                  op0=ALU.mult, op1=ALU.add),
                     reads=[R_g, R_byv], writes=[R_ps[b1], R_vnew])
            yield
            for h in range(4):
                P.op("pe", lambda e, h=h: e.matmul(O2V[:, h, :], QKT[:, h, :], VNEW[:, h, :], start=True, stop=True), reads=[R_qkt, R_vnew], writes=[R_ps[b2]])
            for h in range(4):
                P.op("pe", lambda e, h=h: e.matmul(SUV[:, h, :], KDEC[:, h, :], VNEW[:, h, :], start=True, stop=True), reads=[R_kdec, R_vnew], writes=[R_ps[b0]])
            yield
            for h in range(4):
                P.op("dve", lambda e, h=h: e.scalar_tensor_tensor(out=SST[:, c4 + h, :], in0=SST[:, c4 + h, :], scalar=exps[:, n, 16 + c4 + h:16 + c4 + h + 1], in1=SUV[:, h, :],
                                                                  op0=ALU.mult, op1=ALU.add),
                     reads=[R_exps], writes=[R_ps[b0], R_s[d_]])
            P.op("act", lambda e: e.activation(out=SBF[:, c4:c4 + 4, :], in_=SST[:, c4:c4 + 4, :], func=AF.Copy), reads=[R_s[d_]], writes=[R_sbf[d_]])
            yield
            for h in range(4):
                P.op("dve", lambda e, h=h: e.scalar_tensor_tensor(out=OACC[:, n, h, :], in0=O1V[:, h, :], scalar=exps[:, n, c4 + h:c4 + h + 1], in1=OACC[:, n, h, :],
                                                                  op0=ALU.mult, op1=ALU.add),
                     reads=[R_exps], writes=[R_ps[b3], R_oacc[n]])
            P.op("dve", lambda e: e.tensor_tensor(out=OACC[:, n, :, :], in0=O2V, in1=OACC[:, n, :, :], op=ALU.add), reads=[], writes=[R_ps[b2], R_oacc[n]])
            yield

        for i in range(NTC):
            precast(12)
            gens = [group(i, 0, ctxs[0]), group(NTC - 1 - i, 1, ctxs[1])]
            while gens:
                for g_ in list(gens):
                    try:
                        next(g_)
                    except StopIteration:
                        gens.remove(g_)
        P.barrier()

        OT = view(OFF_B, [128, 4, T], BF16)
        TMPO = view(OFF_A + 32 * K, [128, 4, 128], F32)
        OGB = view(OFF_A + 34 * K, [128, 512], BF16)
        R_ot = [R() for _ in range(NTC)]
        R_tmpo = R()
        R_ogb = R()
        for n in range(NTC):
            P.op("dve", lambda e, n=n: e.tensor_tensor(out=TMPO[:, :, :], in0=OACC[:, n, :, :], in1=OACC[:, n, :, :], op=ALU.mult), reads=[R_oacc[n]], writes=[R_tmpo])
            P.op("dve", lambda e: e.tensor_reduce(out=small[:, 40:44], in_=TMPO[:, :, :], axis=AX.X, op=ALU.add), reads=[R_tmpo], writes=[R_small])
            P.op("act", lambda e: e.activation(out=small[:, 44:48], in_=small[:, 40:44], func=AF.Sqrt, bias=epsc[:, :], scale=1.0 / 128), reads=[R_c], writes=[R_small])
            P.op("dve", lambda e: e.reciprocal(out=small[:, 48:52], in_=small[:, 44:48]), reads=[], writes=[R_small])
            P.op("dve", lambda e, n=n: e.tensor_tensor(out=TMPO[:, :, :], in0=OACC[:, n, :, :], in1=small[:, 48:52].unsqueeze(2).to_broadcast([128, 4, 128]), op=ALU.mult),
                 reads=[R_oacc[n], R_small], writes=[R_tmpo])
            P.op("dve", lambda e, n=n: e.tensor_tensor(out=OGB[:, :], in0=TMPO[:, :, :].rearrange("p h d -> p (h d)"), in1=ZS[:, n, :], op=ALU.mult),
                 reads=[R_tmpo, R_zs[n]], writes=[R_ogb])
            pb = n % 2
            for h in range(4):
                P.op("pe", lambda e, h=h, pb=pb: e.transpose(psb(pb, BF16)[:, h * 128:(h + 1) * 128], OGB[:, h * 128:(h + 1) * 128], identb[:, :]),
                     reads=[R_ogb, R_c], writes=[R_ps[pb]])
            P.op("act", lambda e, n=n, pb=pb: e.activation(out=OT[:, :, n * 128:(n + 1) * 128], in_=psb(pb, BF16)[:, 0:512].rearrange("p (a b) -> p a b", b=128), func=AF.Copy),
                 reads=[], writes=[R_ps[pb], R_ot[n]])
        P.barrier()
        if DEBUG:
            dd = nc.dram_tensor("d_ot", [128, 4 * T], BF16, kind="ExternalOutput").ap()
            out_toks.append(P.dma("sp", ds_dbg, lambda e, dd=dd: e.dma_start(out=dd, in_=OT[:, :, :].rearrange("p a b -> p (a b)"))))

        WOUT = view(OFF_B + 16 * K, [128, 8, D], BF16)
        HACC = view(OFF_A, [128, NTC, D], F32)
        XN2 = view(OFF_C, [128, NTC, D], BF16)
        NWBC2 = view(OFF_D, [128, D], F32)
        XN2F = view(OFF_D + 4 * K, [128, D], F32)
        XN2T = view(OFF_D + 8 * K, [128, 8, 128], F32)
        R_wout = R()
        R_hacc = [R() for _ in range(NTC)]
        R_xn2 = [R() for _ in range(NTC)]
        R_nw2 = R()
        R_xn2f = R()
        R_xn2t = R()
        P.dma("pool", ds_w[2], lambda e: e.dma_start(out=WOUT[:, :, :], in_=wout_d.rearrange("(c p) n -> p c n", p=128)), writes=[R_wout])
        P.dma("sp", ds_c, lambda e: e.dma_start(out=NWBC2[:, :], in_=nw_d[1]), writes=[R_nw2])
        XN2Fs = [XN2F, view(OFF_F + 8 * K, [128, D], F32)]
        XN2Ts = [XN2T, view(OFF_F + 12 * K, [128, 8, 128], F32)]
        R_xn2fs = [R_xn2f, R()]
        R_xn2ts = [R_xn2t, R()]

        scr_toks = []
        R_sma = [R(), R()]
        R_smb = R()
        def s3_a(tc):
            b = tc % 2
            ts_ = slice(tc * 128, (tc + 1) * 128)
            XF, R_xf = XN2Fs[b], R_xn2fs[b]
            XTt, R_xt2 = XN2Ts[b], R_xn2ts[b]
            P.dma("sp", ds_x[b], lambda e: e.dma_start(out=XT[b][:, :], in_=x_d[tc * 128:(tc + 1) * 128, :]), writes=[R_xt[b]])
            for half in range(2):
                pb = 2 * (tc % 2) + half
                for cc in range(8):
                    lh = OT[:, cc, ts_] if cc < 4 else OPT[:, cc - 4, ts_]
                    rr = R_ot[tc] if cc < 4 else R_opt[cc - 4]
                    P.op("pe", lambda e, cc=cc, half=half, pb=pb, lh=lh: e.matmul(psb(pb), lh, WOUT[:, cc, half * 512:(half + 1) * 512], start=(cc == 0), stop=(cc == 7)),
                         reads=[rr, R_wout], writes=[R_ps[pb]])
                P.op("dve", lambda e, half=half, pb=pb: e.tensor_tensor(out=HACC[:, tc, half * 512:(half + 1) * 512], in0=psb(pb), in1=XT[b][:, half * 512:(half + 1) * 512], op=ALU.add),
                     reads=[R_xt[b]], writes=[R_ps[pb], R_hacc[tc]])

        def s3_n(tc):
            if not MOE:
                return
            b = tc % 2
            XF, R_xf = XN2Fs[b], R_xn2fs[b]
            XTt, R_xt2 = XN2Ts[b], R_xn2ts[b]
            sc = 56 + 3 * b
            P.op("act", lambda e: e.activation(out=XF[:, :], in_=HACC[:, tc, :], func=AF.Square, accum_out=small[:, sc:sc + 1]),
                 reads=[R_hacc[tc]], writes=[R_xf, R_sma[b]])
            P.op("act", lambda e: e.activation(out=small[:, sc + 1:sc + 2], in_=small[:, sc:sc + 1], func=AF.Sqrt, bias=epsc[:, :], scale=1.0 / D), reads=[R_c], writes=[R_sma[b]])
            P.op("dve", lambda e: e.reciprocal(out=small[:, sc + 2:sc + 3], in_=small[:, sc + 1:sc + 2]), reads=[], writes=[R_sma[b]])
            P.op("dve", lambda e: e.scalar_tensor_tensor(out=XF[:, :], in0=HACC[:, tc, :], scalar=small[:, sc + 2:sc + 3], in1=NWBC2[:, :], op0=ALU.mult, op1=ALU.mult),
                 reads=[R_hacc[tc], R_sma[b], R_nw2], writes=[R_xf])
            P.op("pool", lambda e: e.tensor_copy(out=XN2[:, tc, :], in_=XF[:, :]), reads=[R_xf], writes=[R_xn2[tc]])
            scr_toks.append(P.dma("sp", ds_s[b], lambda e: e.dma_start(out=xn2_d[tc * 128:(tc + 1) * 128, :], in_=XN2[:, tc, :]), reads=[R_xn2[tc]]))
            for dc in range(8):
                pb = 4 + dc // 4
                P.op("pe", lambda e, dc=dc, pb=pb: e.transpose(psb(pb)[:, (dc % 4) * 128:(dc % 4 + 1) * 128], XF[:, dc * 128:(dc + 1) * 128], ident[:, :]),
                     reads=[R_xf, R_c], writes=[R_ps[pb]])
            for hb in range(2):
                P.op("act", lambda e, hb=hb: e.activation(out=XTt[:, hb * 4:(hb + 1) * 4, :], in_=psb(4 + hb).rearrange("p (a b) -> p a b", b=128), func=AF.Copy),
                     reads=[], writes=[R_ps[4 + hb], R_xt2])

        def s3_b(tc):
            if not MOE:
                return
            b = tc % 2
            XTt, R_xt2 = XN2Ts[b], R_xn2ts[b]
            LG = psb(6 + b)[:, 0:16]
            for dc in range(8):
                P.op("pe", lambda e, dc=dc: e.matmul(LG, XTt[:, dc, :], rws[:, dc, :], start=(dc == 0), stop=(dc == 7)), reads=[R_xt2, R_c], writes=[R_ps[6 + b]])
            P.op("dve", lambda e: e.tensor_reduce(out=small[:, 52:53], in_=LG, axis=AX.X, op=ALU.max), reads=[], writes=[R_ps[6 + b], R_smb])
            P.op("dve", lambda e: e.tensor_scalar(out=small[:, 53:54], in0=small[:, 52:53], scalar1=-1.0, scalar2=None, op0=ALU.mult), reads=[], writes=[R_smb])
            P.op("act", lambda e: e.activation(out=prob[:, tc, :], in_=LG, func=AF.Exp, bias=small[:, 53:54], scale=1.0, accum_out=small[:, 54:55]),
                 reads=[R_smb], writes=[R_ps[6 + b], R_prob, R_smb])
            P.op("dve", lambda e: e.reciprocal(out=small[:, 55:56], in_=small[:, 54:55]), reads=[], writes=[R_smb])
            P.op("dve", lambda e: e.tensor_scalar(out=prob[:, tc, :], in0=prob[:, tc, :], scalar1=small[:, 55:56], scalar2=None, op0=ALU.mult), reads=[R_smb], writes=[R_prob])

        s3_a(0)
        for tc in range(NTC + 1):
            if tc + 1 < NTC:
                s3_a(tc + 1)
            if tc < NTC:
                s3_n(tc)
            if tc >= 1:
                s3_b(tc - 1)
        P.barrier()
        if MOE:
            PF = view(OFF_B, [128, T], F32)
            WORK = view(OFF_B + 8 * K, [128, T], F32)
            SELT = view(OFF_B + 16 * K, [128, T], F32)
            M8 = view(OFF_B + 24 * K, [128, 8], F32)
            TH = view(OFF_B + 24 * K + 64, [128, 1], F32)
            R_pf, R_work, R_selt, R_m8, R_th = R(), R(), R(), R(), R()
            slot_off = [OFF_F, OFF_F + 12 * K, OFF_E + 4 * K, OFF_B + 20 * K]
            NSLOT = len(slot_off)
            WGs = [view(o_, [128, 8, 256], BF16) for o_ in slot_off]
            WUs = [view(o_ + 4 * K, [128, 8, 256], BF16) for o_ in slot_off]
            WDs = [view(o_ + 8 * K, [128, 2, D], BF16) for o_ in slot_off]
            R_wg = [R() for _ in range(NSLOT)]
            R_wu = [R() for _ in range(NSLOT)]
            R_wd = [R() for _ in range(NSLOT)]

            def load_piece(pi):
                e_, pc = pi // 8, pi % 8
                sl_ = pi % NSLOT
                f0 = pc * 256
                if e_ >= E - NPC:
                    i_ = e_ - (E - NPC)
                    if pc == 0:
                        precast(10 ** 6)
                        P.wait_all("pool", pc_toks[i_])
                    P.dma("pool", ds_w[3 * sl_], lambda e, i_=i_, sl_=sl_, f0=f0: e.dma_start(out=WGs[sl_][:, :, :], in_=wgb_d[i_].rearrange("(c p) f -> p c f", p=128)[:, :, f0:f0 + 256]),
                          writes=[R_wg[sl_]])
                    P.dma("pool", ds_w[3 * sl_ + 1], lambda e, i_=i_, sl_=sl_, f0=f0: e.dma_start(out=WUs[sl_][:, :, :], in_=wub_d[i_].rearrange("(c p) f -> p c f", p=128)[:, :, f0:f0 + 256]),
                          writes=[R_wu[sl_]])
                    P.dma("pool", ds_w[3 * sl_ + 2], lambda e, i_=i_, sl_=sl_, f0=f0: e.dma_start(out=WDs[sl_][:, :, :], in_=wdb_d[i_, f0:f0 + 256, :].rearrange("(c p) d -> p c d", p=128)),
                          writes=[R_wd[sl_]])
                    return
                P.dma("pool", ds_w[3 * sl_], lambda e, e_=e_, sl_=sl_, f0=f0: e.dma_start(out=WGs[sl_][:, :, :], in_=wg_d[e_].rearrange("(c p) f -> p c f", p=128)[:, :, f0:f0 + 256]),
                      writes=[R_wg[sl_]])
                P.dma("pool", ds_w[3 * sl_ + 1], lambda e, e_=e_, sl_=sl_, f0=f0: e.dma_start(out=WUs[sl_][:, :, :], in_=wu_d[e_].rearrange("(c p) f -> p c f", p=128)[:, :, f0:f0 + 256]),
                      writes=[R_wu[sl_]])
                P.dma("pool", ds_w[3 * sl_ + 2], lambda e, e_=e_, sl_=sl_, f0=f0: e.dma_start(out=WDs[sl_][:, :, :], in_=wd_d[e_, f0:f0 + 256, :].rearrange("(c p) d -> p c d", p=128)),
                      writes=[R_wd[sl_]])

            for pi in range(3):
                load_piece(pi)

            for tc in range(NTC):
                b = tc // 4
                P.op("pe", lambda e, tc=tc, b=b: e.transpose(psb(b)[0:16, (tc % 4) * 128:(tc % 4 + 1) * 128], prob[:, tc, :], ident[:, :]),
                     reads=[R_prob, R_c], writes=[R_ps[b]])
            for b in range(4):
                P.op("act", lambda e, b=b: e.activation(out=PF[0:16, b * 512:(b + 1) * 512], in_=psb(b)[0:16, :], func=AF.Copy), reads=[], writes=[R_ps[b], R_pf])
                P.op("dve", lambda e, b=b: e.tensor_copy(out=WORK[0:16, b * 512:(b + 1) * 512], in_=psb(b)[0:16, :]), reads=[], writes=[R_ps[b], R_work])
            for rnd in range(CAP // 8):
                P.op("dve", lambda e: e.max(out=M8[0:16, :], in_=WORK[0:16, :]), reads=[R_work], writes=[R_m8])
                if rnd < CAP // 8 - 1:
                    P.op("dve", lambda e: e.match_replace(out=WORK[0:16, :], in_to_replace=M8[0:16, :], in_values=WORK[0:16, :], imm_value=-1.0),
                         reads=[R_m8], writes=[R_work])
            P.op("dve", lambda e: e.tensor_reduce(out=TH[0:16, :], in_=M8[0:16, :], axis=AX.X, op=ALU.min), reads=[R_m8], writes=[R_th])
            P.op("dve", lambda e: e.tensor_scalar(out=SELT[0:16, :], in0=PF[0:16, :], scalar1=TH[0:16, 0:1], scalar2=None, op0=ALU.is_ge),
                 reads=[R_pf, R_th], writes=[R_selt])
            SV = psb(4)[:, 0:256].rearrange("p (a b) -> p a b", b=16)
            for tc in range(NTC):
                P.op("pe", lambda e, tc=tc: e.transpose(SV[:, tc, :], SELT[0:16, tc * 128:(tc + 1) * 128], ident[0:16, 0:16]),
                     reads=[R_selt, R_c], writes=[R_ps[4]])
            P.op("act", lambda e: e.activation(out=sel[:, :, :], in_=SV, func=AF.Copy), reads=[], writes=[R_ps[4], R_sel])
            P.op("dve", lambda e: e.tensor_copy(out=selb[:, :, :], in_=SV), reads=[], writes=[R_ps[4], R_sel])
            PV = psb(5)[:, 0:256].rearrange("p (a b) -> p a b", b=16)
            for tc in range(NTC):
                mms = [(onesb, t2) for t2 in range(tc)] + [(ltb, tc)]
                for i_, (lh, t2) in enumerate(mms):
                    P.op("pe", lambda e, tc=tc, lh=lh, t2=t2, i_=i_, nmm=len(mms): e.matmul(PV[:, tc, :], lh[:, :], selb[:, t2, :], start=(i_ == 0), stop=(i_ == nmm - 1)),
                         reads=[R_sel, R_c], writes=[R_ps[5]])
            P.op("dve", lambda e: e.scalar_tensor_tensor(out=posm[:, :, :], in0=PV, scalar=1.0, in1=sel[:, :, :], op0=ALU.add, op1=ALU.mult),
                 reads=[R_sel], writes=[R_ps[5], R_posm])
            P.op("dve", lambda e: e.tensor_scalar(out=posm[:, :, :], in0=posm[:, :, :], scalar1=-1.0, scalar2=None, op0=ALU.add), reads=[], writes=[R_posm])
            P.barrier()
            if DEBUG:
                for name, tl in (("d_prob", prob), ("d_sel", sel), ("d_posm", posm)):
                    dd = nc.dram_tensor(name, [128, 256], F32, kind="ExternalOutput").ap()
                    out_toks.append(P.dma("sp", ds_dbg, lambda e, dd=dd, tl=tl: e.dma_start(out=dd, in_=tl[:, :, :].rearrange("p a b -> p (a b)"))))

            load_piece(3)
            PE_ = view(OFF_B, [128, NTC, 256], BF16)
            PTE = view(OFF_B + 8 * K, [128, 2, T], BF16)
            XG = view(OFF_D, [128, 8, 256], BF16)
            HTT = view(OFF_D + 4 * K, [128, 16, 256], BF16)
            SGs = [view(OFF_D + 12 * K + i * K, [128, 256], F32) for i in range(2)]
            TMPS = [view(OFF_B + 16 * K + i * 2 * K, [128, 512], F32) for i in range(2)]
            R_tmps = [R(), R()]
            YG = view(OFF_E, [128, 2, D], BF16)
            R_pe, R_pte, R_xg, R_yg = R(), R(), R(), R()
            XGT = view(OFF_C, [128, 2, D], BF16)
            IDXF = view(OFF_C + 4 * K, [128, 2], F32)
            IDXU = view(OFF_C + 4 * K + 64, [128, 2], U32)
            IDX4 = view(OFF_C + 4 * K + 128, [128, 4], F32)
            R_xgt, R_idx = [R(), R()], R()
            R_scr = R()
            R_scr.w = scr_toks[-1] if False else None
            R_sgs = [R(), R()]
            R_htt = [R() for _ in range(16)]
            next_piece = [NSLOT]

            def build_p1(e_, tc):
                P.op("dve", lambda e, tc=tc, e_=e_: e.tensor_scalar(out=PE_[:, tc, :], in0=iota[:, :], scalar1=posm[:, tc, e_:e_ + 1], scalar2=None, op0=ALU.is_equal),
                     reads=[R_posm, R_c], writes=[R_pe])

            def build_p(e_):
                for tc in range(NTC):
                    build_p1(e_, tc)

            def gather_idx():
                IV = psb(7)[:, 0:4]
                for jc in range(2):
                    for tc in range(NTC):
                        P.op("pe", lambda e, jc=jc, tc=tc: e.matmul(IV[:, jc * 2:jc * 2 + 2], PE_[:, tc, jc * 128:(jc + 1) * 128], tokb[:, tc, :], start=(tc == 0), stop=(tc == NTC - 1)),
                             reads=[R_pe, R_c], writes=[R_ps[7]])
                P.op("dve", lambda e: e.tensor_copy(out=IDX4[:, :], in_=IV), reads=[], writes=[R_ps[7], R_idx])
                IV3 = IDX4[:, :].rearrange("p (j k) -> p j k", k=2)
                P.op("dve", lambda e: e.scalar_tensor_tensor(out=IDXF[:, :], in0=IV3[:, :, 0], scalar=128.0, in1=IV3[:, :, 1], op0=ALU.mult, op1=ALU.add),
                     reads=[], writes=[R_idx])
                P.op("dve", lambda e: e.tensor_copy(out=IDXU[:, :], in_=IDXF[:, :]), reads=[], writes=[R_idx])
                for jc in range(2):
                    P.dma("pool", ds_g[jc], lambda e, jc=jc: e.indirect_dma_start(out=XGT[:, jc, :], out_offset=None, in_=xn2_d[:, :],
                                                                                   in_offset=bass.IndirectOffsetOnAxis(ap=IDXU[:, jc:jc + 1], axis=0)),
                          reads=[R_idx], writes=[R_xgt[jc]])

            def gather():
                for jc in range(2):
                    bank = 6 + jc
                    for dc in range(8):
                        P.op("pe", lambda e, jc=jc, dc=dc, bank=bank: e.transpose(psb(bank, BF16)[:, dc * 128:(dc + 1) * 128], XGT[:, jc, dc * 128:(dc + 1) * 128], identb[:, :]),
                             reads=[R_xgt[jc], R_c], writes=[R_ps[bank]])
                    P.op("act", lambda e, jc=jc, bank=bank: e.activation(out=XG[:, :, jc * 128:(jc + 1) * 128], in_=psb(bank, BF16)[:, 0:1024].rearrange("p (a b) -> p a b", b=128), func=AF.Copy),
                         reads=[], writes=[R_ps[bank], R_xg])

            def transpose_p():
                for jc in range(2):
                    for th in range(2):
                        bank = 4 + (jc * 2 + th) % 2
                        for t8 in range(8):
                            tc = th * 8 + t8
                            P.op("pe", lambda e, jc=jc, tc=tc, t8=t8, bank=bank: e.transpose(psb(bank, BF16)[:, t8 * 128:(t8 + 1) * 128], PE_[:, tc, jc * 128:(jc + 1) * 128], identb[:, :]),
                                 reads=[R_pe, R_c], writes=[R_ps[bank]])
                        P.op("act", lambda e, jc=jc, th=th, bank=bank: e.activation(out=PTE[:, jc, th * 1024:(th + 1) * 1024], in_=psb(bank, BF16)[:, 0:1024], func=AF.Copy),
                             reads=[], writes=[R_ps[bank], R_pte])

            def down(e_, fc):
                sl_ = (e_ * 8 + fc // 2) % NSLOT
                sub = fc % 2
                for jc in range(2):
                    for half in range(2):
                        yb = jc * 2 + half
                        P.op("pe", lambda e, fc=fc, jc=jc, half=half, yb=yb, sl_=sl_, sub=sub: e.matmul(psb(yb), HTT[:, fc, jc * 128:(jc + 1) * 128], WDs[sl_][:, sub, half * 512:(half + 1) * 512],
                                                                                                    start=(fc == 0), stop=(fc == 15)),
                             reads=[R_htt[fc], R_wd[sl_]], writes=[R_ps[yb]])
                if sub == 1:
                    if next_piece[0] < E * 8:
                        load_piece(next_piece[0])
                        next_piece[0] += 1

            def ffn(e_):
                for fc in range(16):
                    sl_ = (e_ * 8 + fc // 2) % NSLOT
                    sub = fc % 2
                    hb = 4 + fc % 2
                    for wi_, (W, RW) in enumerate(((WGs, R_wg), (WUs, R_wu))):
                        for dc in range(8):
                            P.op("pe", lambda e, wi_=wi_, W=W, dc=dc, hb=hb, sl_=sl_, sub=sub: e.matmul(psb(hb)[:, wi_ * 256:(wi_ + 1) * 256], W[sl_][:, dc, sub * 128:(sub + 1) * 128], XG[:, dc, :],
                                                                                                    start=(dc == 0), stop=(dc == 7)),
                                 reads=[RW[sl_], R_xg], writes=[R_ps[hb]])
                    sgi = fc % 2
                    P.op("act", lambda e, hb=hb, sgi=sgi: e.activation(out=SGs[sgi][:, :], in_=psb(hb)[:, 0:256], func=AF.Silu), reads=[], writes=[R_ps[hb], R_sgs[sgi]])
                    P.op("dve", lambda e, hb=hb, fc=fc, sgi=sgi: e.tensor_tensor(out=HTT[:, fc, :], in0=SGs[sgi][:, :], in1=psb(hb)[:, 256:512], op=ALU.mult),
                         reads=[R_sgs[sgi]], writes=[R_ps[hb], R_htt[fc]])
                    if e_ + 1 < E:
                        if fc < 8:
                            build_p1(e_ + 1, 2 * fc)
                            build_p1(e_ + 1, 2 * fc + 1)
                        if fc == 9:
                            gather_idx()
                    if fc >= 2:
                        down(e_, fc - 2)
                down(e_, 14)
                down(e_, 15)
                for jc in range(2):
                    for half in range(2):
                        yb = jc * 2 + half
                        P.op("act", lambda e, jc=jc, half=half, yb=yb: e.activation(out=YG[:, jc, half * 512:(half + 1) * 512], in_=psb(yb), func=AF.Copy),
                             reads=[], writes=[R_ps[yb], R_yg])

            def scatter(e_):
                sbanks = (6, 7, 0, 1, 2, 3)
                for tc in range(NTC):
                    for half in range(2):
                        i_ = tc * 2 + half
                        bank = sbanks[i_ % len(sbanks)]
                        for jc in range(2):
                            P.op("pe", lambda e, tc=tc, half=half, jc=jc, bank=bank: e.matmul(psb(bank), PTE[:, jc, tc * 128:(tc + 1) * 128], YG[:, jc, half * 512:(half + 1) * 512],
                                                                                            start=(jc == 0), stop=(jc == 1)),
                                 reads=[R_pte, R_yg], writes=[R_ps[bank]])
                        if i_ % 2 == 0:
                            P.op("dve", lambda e, tc=tc, half=half, bank=bank, e_=e_: e.scalar_tensor_tensor(out=HACC[:, tc, half * 512:(half + 1) * 512], in0=psb(bank), scalar=prob[:, tc, e_:e_ + 1],
                                                                                                         in1=HACC[:, tc, half * 512:(half + 1) * 512], op0=ALU.mult, op1=ALU.add),
                                 reads=[R_prob], writes=[R_ps[bank], R_hacc[tc]])
                        else:
                            ti = (i_ // 2) % 2
                            P.op("act", lambda e, tc=tc, bank=bank, e_=e_, ti=ti: e.activation(out=TMPS[ti][:, :], in_=psb(bank), func=AF.Identity, scale=prob[:, tc, e_:e_ + 1]),
                                 reads=[R_prob], writes=[R_ps[bank], R_tmps[ti]])
                            P.op("pool", lambda e, tc=tc, half=half, ti=ti: e.tensor_tensor(out=HACC[:, tc, half * 512:(half + 1) * 512], in0=HACC[:, tc, half * 512:(half + 1) * 512],
                                                                                        in1=TMPS[ti][:, :], op=ALU.add),
                                 reads=[R_tmps[ti]], writes=[R_hacc[tc]])

            P.wait_all("pool", scr_toks)
            build_p(0)
            gather_idx()
            gather()
            for e_ in range(E):
                transpose_p()
                ffn(e_)
                scatter(e_)
                if e_ + 1 < E:
                    gather()
            P.barrier()
        NWF = view(OFF_D, [128, D], F32)
        OTL = [view(OFF_E + i * 4 * K, [128, D], F32) for i in range(2)]
        R_nwf = R()
        R_otl = [R(), R()]
        P.dma("sp", ds_c, lambda e: e.dma_start(out=NWF[:, :], in_=nw_d[2]), writes=[R_nwf])
        for tc in range(NTC):
            ob = tc % 2
            P.op("act", lambda e, ob=ob, tc=tc: e.activation(out=OTL[ob][:, :], in_=HACC[:, tc, :], func=AF.Square, accum_out=small[:, 8:9]),
                 reads=[R_hacc[tc]], writes=[R_otl[ob], R_small])
            P.op("act", lambda e: e.activation(out=small[:, 9:10], in_=small[:, 8:9], func=AF.Sqrt, bias=epsc[:, :], scale=1.0 / D),
                 reads=[R_c], writes=[R_small])
            P.op("dve", lambda e: e.reciprocal(out=small[:, 10:11], in_=small[:, 9:10]), reads=[], writes=[R_small])
            P.op("dve", lambda e, ob=ob, tc=tc: e.scalar_tensor_tensor(out=OTL[ob][:, :], in0=HACC[:, tc, :], scalar=small[:, 10:11], in1=NWF[:, :], op0=ALU.mult, op1=ALU.mult),
                 reads=[R_hacc[tc], R_small, R_nwf], writes=[R_otl[ob]])
            out_toks.append(P.dma("sp", ds_o[ob], lambda e, tc=tc, ob=ob: e.dma_start(out=out_d[tc * 128:(tc + 1) * 128, :], in_=OTL[ob][:, :]), reads=[R_otl[ob]]))
        P.wait_all("sp", out_toks)

        with nc.Block() as block:
            @block.tensor
            def _(e):
                for f in P.q["pe"]:
                    f(e)

            @block.scalar
            def _(e):
                for f in P.q["act"]:
                    f(e)

            @block.vector
            def _(e):
                for f in P.q["dve"]:
                    f(e)

            @block.gpsimd
            def _(e):
                for f in P.q["pool"]:
                    f(e)

            @block.sync
            def _(e):
                for f in P.q["sp"]:
                    f(e)
    return nc


DEBUG = False
MOE = True


def make_in_maps(inputs):
    c = host_consts()
    f = lambda a: np.ascontiguousarray(np.asarray(a, dtype=np.float32))
    x = f(inputs["x"])
    shared = {
        "w_in": f(inputs["w_in"][0]),
        "conv_wP": f(np.asarray(inputs["conv_w"][0]).T.reshape(12, 128, 5).transpose(1, 0, 2).reshape(128, 60)),
        "abp": f(np.tile(np.concatenate([np.asarray(inputs["a_log_fwd"][0]), np.asarray(inputs["a_log_bwd"][0]),
                                         np.asarray(inputs["dt_bias_fwd"][0]), np.asarray(inputs["dt_bias_bwd"][0])])[None, :], (128, 1))),
        "nw0": f(np.tile(np.asarray(inputs["norm_mix_w"][0])[None, :], (128, 1))),
        "nw1": f(np.tile(np.asarray(inputs["norm_ffn_w"][0])[None, :], (128, 1))),
        "nw2": f(np.tile(np.asarray(inputs["norm_final_w"])[None, :], (128, 1))),
        "hnw": f(np.tile(np.asarray(inputs["head_norm_w"][0])[None, :], (128, 1))),
        "pool_wP": f(np.asarray(inputs["pool_w"][0]).transpose(1, 0, 2).reshape(128, 512)),
        "pool_scT": f(np.asarray(inputs["pool_scale"][0]).reshape(4, 128).T),
        "w_out": f(inputs["w_out"][0]),
        "router_wP": f(np.asarray(inputs["router_w"][0]).reshape(8, 128, 16).transpose(1, 0, 2).reshape(128, 128)),
        "wg": f(inputs["expert_w_gate"][0]),
        "wu": f(inputs["expert_w_up"][0]),
        "wd": f(inputs["expert_w_down"][0]),
        "c_iota": c["iota"],
        "c_tok": c["tok"],
        "c_invc": c["invc"],
    }
    for n in CONST_NAMES:
        shared["c_" + n] = c[n]
    maps = []
    for b in range(8):
        m = dict(shared)
        m["x"] = np.ascontiguousarray(x[b])
        maps.append(m)
    return maps


def kernel(**inputs):
    nc = build_nc()
    in_maps = make_in_maps(inputs)
    res = run_bass_kernel_spmd(nc, in_maps, core_ids=list(range(8)))
    out = np.stack([np.asarray(r["out"], dtype=np.float32) for r in res.results], axis=0)
    return out
```

```python
import numpy as np
from contextlib import ExitStack
import concourse.bass as bass
import concourse.mybir as mybir
from concourse.bass_utils import run_bass_kernel_spmd

F32 = mybir.dt.float32
BF16 = mybir.dt.bfloat16
U32 = mybir.dt.uint32
F32R = mybir.dt.float32r
ALU = mybir.AluOpType
AF = mybir.ActivationFunctionType
AX = mybir.AxisListType

T = 2048
D = 1024
NTC = 16
H = 4
E = 16
CAP = 256
FF = 2048
INC = 2576
EPS = 1e-6
BIG = 30000.0
ENGS = ("pe", "act", "dve", "pool", "sp")


class Tk:
    __slots__ = ("sem", "val", "snap")

    def __init__(s, sem, val, snap):
        s.sem = sem
        s.val = val
        s.snap = snap


class R:
    __slots__ = ("w", "rs")

    def __init__(s):
        s.w = None
        s.rs = {}


class DS:
    def __init__(s, sem):
        s.sem = sem
        s.count = 0


class Prog:
    def __init__(s, psem):
        s.q = {e: [] for e in ENGS}
        s.cnt = {e: 0 for e in ENGS}
        s.vc = {e: {} for e in ENGS}
        s.psem = psem
        s.dma_toks = []

    def _waits(s, eng, reads, writes):
        vc = s.vc[eng]
        need = {}
        toks = []
        for r in reads:
            if r.w is not None:
                toks.append(r.w)
        for r in writes:
            if r.w is not None:
                toks.append(r.w)
            toks.extend(r.rs.values())
        for t in toks:
            if eng == "pe" and t.sem is s.psem["pe"]:
                continue
            k = id(t.sem)
            if vc.get(k, 0) >= t.val:
                continue
            if k not in need or need[k].val < t.val:
                need[k] = t
        return list(need.values())

    def _absorb(s, eng, waits):
        vc = s.vc[eng]
        for t in waits:
            for k, v in t.snap.items():
                if vc.get(k, 0) < v:
                    vc[k] = v
            k = id(t.sem)
            if vc.get(k, 0) < t.val:
                vc[k] = t.val

    def op(s, eng, fn, reads=(), writes=()):
        waits = s._waits(eng, reads, writes)
        s._absorb(eng, waits)
        s.cnt[eng] += 1
        sem = s.psem[eng]
        tok = Tk(sem, s.cnt[eng], dict(s.vc[eng]))
        wl = [(t.sem, t.val) for t in waits]

        def emit(e):
            for sm, v in wl:
                e.wait_ge(sm, v)
            fn(e).then_inc(sem, 1)

        s.q[eng].append(emit)
        for r in reads:
            r.rs[id(sem)] = tok
        for r in writes:
            r.w = tok
            r.rs = {}
        return tok

    def dma(s, eng, ds, fn, reads=(), writes=()):
        waits = s._waits(eng, reads, writes)
        s._absorb(eng, waits)
        ds.count += 16
        tok = Tk(ds.sem, ds.count, dict(s.vc[eng]))
        wl = [(t.sem, t.val) for t in waits]
        sem = ds.sem

        def emit(e):
            for sm, v in wl:
                e.wait_ge(sm, v)
            fn(e).then_inc(sem, 16)

        s.q[eng].append(emit)
        for r in reads:
            r.rs[id(sem)] = tok
        for r in writes:
            r.w = tok
            r.rs = {}
        s.dma_toks.append(tok)
        return tok

    def barrier(s):
        toks = [Tk(s.psem[e], s.cnt[e], dict(s.vc[e])) for e in ENGS if s.cnt[e] > 0 and e != "sp"]
        toks += s.dma_toks
        s.dma_toks = []
        for eng in ENGS:
            vc = s.vc[eng]
            need = {}
            for t in toks:
                k = id(t.sem)
                if vc.get(k, 0) >= t.val:
                    continue
                if k not in need or need[k].val < t.val:
                    need[k] = t
            waits = list(need.values())
            s._absorb(eng, waits)
            wl = [(t.sem, t.val) for t in waits]
            if wl:
                def emit(e, wl=wl):
                    for sm, v in wl:
                        e.wait_ge(sm, v)
                s.q[eng].append(emit)

    def wait_all(s, eng, toks):
        wl = [(t.sem, t.val) for t in toks]

        def emit(e):
            for sm, v in wl:
                e.wait_ge(sm, v)
        s.q[eng].append(emit)


def host_consts():
    p = np.arange(128)[:, None]
    f = np.arange(128)[None, :]
    c = {}
    c["ident"] = (p == f).astype(np.float32)
    c["ut_f"] = (p <= f).astype(np.float32)
    c["ut_b"] = (p >= f).astype(np.float32)
    c["sl_f"] = (p > f).astype(np.float32)
    c["sl_b"] = (p < f).astype(np.float32)
    c["neg_f"] = (-BIG * (f < p)).astype(np.float32)
    c["neg_b"] = (-BIG * (f > p)).astype(np.float32)
    c["str_f"] = (f > p).astype(np.float32)
    c["str_b"] = (f < p).astype(np.float32)
    c["ones"] = np.ones((128, 128), np.float32)
    c["iota"] = np.tile(np.arange(256, dtype=np.float32)[None, :], (128, 1))
    tok = np.zeros((128, 16, 2), np.float32)
    tok[:, :, 0] = np.arange(16, dtype=np.float32)[None, :]
    tok[:, :, 1] = np.arange(128, dtype=np.float32)[:, None]
    c["tok"] = tok.reshape(128, 32)
    invc = np.zeros((128, 4, 16), np.float32)
    for g, w in enumerate((2, 4, 8, 16)):
        lo = w // 2
        hi = w - lo - 1
        for t in range(lo):
            invc[:, g, t] = 1.0 / (t + hi + 1)
        for i in range(hi):
            t = T - hi + i
            invc[:, g, 8 + i] = 1.0 / (T - t + lo)
    c["invc"] = invc.reshape(128, 64)
    return c


CONST_NAMES = ["ident", "ut_f", "ut_b", "sl_f", "sl_b", "neg_f", "neg_b", "str_f", "str_b", "ones"]


def build_nc():
    nc = bass.Bass("TRN2", target_bir_lowering=False)

    def din(name, shape, dt=F32):
        return nc.dram_tensor(name, list(shape), dt, kind="ExternalInput").ap()

    x_d = din("x", [T, D])
    win_d = din("w_in", [D, INC])
    cw_d = din("conv_wP", [128, 60])
    abp_d = din("abp", [128, 16])
    nw_d = [din("nw%d" % i, [128, D]) for i in range(3)]
    hnw_d = din("hnw", [128, 128])
    pw_d = din("pool_wP", [128, 512])
    psc_d = din("pool_scT", [128, 4])
    wout_d = din("w_out", [D, D])
    rw_d = din("router_wP", [128, 128])
    wg_d = din("wg", [E, D, FF])
    wu_d = din("wu", [E, D, FF])
    wd_d = din("wd", [E, FF, D])
    cst_d = {n: din("c_" + n, [128, 128]) for n in CONST_NAMES}
    iota_d = din("c_iota", [128, 256])
    invc_d = din("c_invc", [128, 64])
    out_d = nc.dram_tensor("out", [T, D], F32, kind="ExternalOutput").ap()
    xn2_d = nc.dram_tensor("xn2_scr", [T, D], BF16, kind="Internal").ap()
    NPC = 11
    wgb_d = nc.dram_tensor("wg_bf", [NPC, D, FF], BF16, kind="Internal").ap()
    wub_d = nc.dram_tensor("wu_bf", [NPC, D, FF], BF16, kind="Internal").ap()
    wdb_d = nc.dram_tensor("wd_bf", [NPC, FF, D], BF16, kind="Internal").ap()
    tok_d = din("c_tok", [128, 32])

    es = ExitStack()
    with es:
        def sb(name, shape, dt):
            return es.enter_context(nc.sbuf_tensor(name, list(shape), dt))

        def pstile(name):
            return es.enter_context(nc.psum_tensor(name, [128, 512], F32))

        psem = {e: es.enter_context(nc.semaphore("ps_" + e)) for e in ENGS}
        P = Prog(psem)
        out_toks = []

        def newds(name):
            return DS(es.enter_context(nc.semaphore(name)))

        cst = {n: sb("k_" + n, [128, 128], F32) for n in CONST_NAMES}
        identb = sb("identb", [128, 128], BF16)
        onesb = sb("onesb", [128, 128], BF16)
        ltb = sb("ltb", [128, 128], BF16)
        iota = sb("iota", [128, 256], F32)
        tokf = sb("tokf", [128, 32], F32)
        tokb = sb("tokb", [128, 16, 2], BF16)
        invc = sb("invc", [128, 64], F32)
        cw = sb("cw", [128, 12, 5], F32)
        abp = sb("abp_s", [128, 16], F32)
        hnw = sb("hnw_s", [128, 128], F32)
        psc = sb("psc", [128, 4], F32)
        pwb = sb("pwb", [128, 4, 128], BF16)
        rws = sb("rws", [128, 8, 16], F32)
        epsc = sb("epsc", [128, 1], F32)
        onec = sb("onec", [128, 1], F32)
        mhalf = sb("mhalf", [128, 1], F32)
        small = sb("small", [128, 64], F32)
        gsm = sb("gsm", [128, 16, 8], F32)
        bet = sb("bet", [128, 16, 8], F32)
        nbet = sb("nbet", [128, 16, 8], F32)
        exps = sb("exps", [128, 16, 24], F32)
        prob = sb("prob", [128, 16, 16], F32)
        sel = sb("sel", [128, 16, 16], F32)
        selb = sb("selb", [128, 16, 16], BF16)
        posm = sb("posm", [128, 16, 16], F32)
        R_c = R()
        R_g = R()
        R_exps = R()
        R_prob = R()
        R_sel = R()
        R_posm = R()
        R_small = R()

        ARENA_B = 190 * 1024
        arena = sb("arena", [128, ARENA_B // 2], BF16)

        def view(off, shape, dt):
            n = 1
            for s_ in shape[1:]:
                n *= s_
            nb = n * (4 if dt in (F32, U32) else 2)
            assert off % 4 == 0 and off + nb <= ARENA_B, (off, nb)
            v = arena[:, off // 2:(off + nb) // 2]
            if dt in (F32, U32):
                v = v.bitcast(dt)
            if len(shape) == 2:
                return v
            if len(shape) == 3:
                return v.rearrange("p (a b) -> p a b", b=shape[2])
            if len(shape) == 4:
                return v.rearrange("p (a b c) -> p a b c", b=shape[2], c=shape[3])
            raise ValueError

        K = 1024
        OFF_A, OFF_B, OFF_C, OFF_D, OFF_E, OFF_F = 0, 64 * K, 96 * K, 128 * K, 144 * K, 160 * K

        ps = [pstile("ps%d" % i) for i in range(8)]
        R_ps = [R() for _ in range(8)]

        def psb(i, dt=F32):
            return ps[i][:, :] if dt == F32 else ps[i][:, :].bitcast(BF16)

        ds_c = newds("ds_c")
        ds_cp = newds("ds_cp")
        ds_x = [newds("ds_x0"), newds("ds_x1")]
        ds_x4 = ds_x + [newds("ds_x2"), newds("ds_x3")]
        ds_w = [newds("ds_w%d" % i) for i in range(12)]
        ds_o = [newds("ds_o0"), newds("ds_o1")]
        ds_s = [newds("ds_s0"), newds("ds_s1")]
        ds_pc = [newds("ds_pc%d" % i) for i in range(NPC)]
        pc_jobs = []
        for i_ in range(NPC):
            for r0 in range(0, D, 128):
                pc_jobs.append((i_, wgb_d[i_, r0:r0 + 128, :], wg_d[E - NPC + i_, r0:r0 + 128, :]))
                pc_jobs.append((i_, wub_d[i_, r0:r0 + 128, :], wu_d[E - NPC + i_, r0:r0 + 128, :]))
            for r0 in range(0, FF, 256):
                pc_jobs.append((i_, wdb_d[i_, r0:r0 + 256, :].rearrange("(a p) d -> p a d", p=128), wd_d[E - NPC + i_, r0:r0 + 256, :].rearrange("(a p) d -> p a d", p=128)))
        pc_next = [0]
        pc_toks = [[] for _ in range(NPC)]

        def precast(n):
            for _ in range(n):
                if pc_next[0] >= len(pc_jobs):
                    return
                i_, dst, src = pc_jobs[pc_next[0]]
                pc_next[0] += 1
                pc_toks[i_].append(P.dma("pool", ds_pc[i_], lambda e, dst=dst, src=src: e.dma_start(out=dst, in_=src)))
        ds_g = [newds("ds_g0"), newds("ds_g1")]

        for n in CONST_NAMES:
            P.dma("sp", ds_c, lambda e, n=n: e.dma_start(out=cst[n][:, :], in_=cst_d[n]), writes=[R_c])
        P.dma("sp", ds_c, lambda e: e.dma_start(out=iota[:, :], in_=iota_d), writes=[R_c])
        P.dma("sp", ds_c, lambda e: e.dma_start(out=tokf[:, :], in_=tok_d), writes=[R_c])
        P.dma("sp", ds_c, lambda e: e.dma_start(out=invc[:, :], in_=invc_d), writes=[R_c])
        P.dma("sp", ds_c, lambda e: e.dma_start(out=cw[:, :, :].rearrange("p c j -> p (c j)"), in_=cw_d), writes=[R_c])
        P.dma("sp", ds_c, lambda e: e.dma_start(out=abp[:, :], in_=abp_d), writes=[R_c])
        P.dma("sp", ds_c, lambda e: e.dma_start(out=hnw[:, :], in_=hnw_d), writes=[R_c])
        P.dma("sp", ds_c, lambda e: e.dma_start(out=psc[:, :], in_=psc_d), writes=[R_c])
        P.dma("sp", ds_c, lambda e: e.dma_start(out=rws[:, :, :].rearrange("p c e -> p (c e)"), in_=rw_d), writes=[R_c])
        P.dma("pool", ds_cp, lambda e: e.dma_start(out=pwb[:, :, :].rearrange("p g d -> p (g d)"), in_=pw_d), writes=[R_c])
        P.op("pool", lambda e: e.tensor_copy(out=identb[:, :], in_=cst["ident"][:, :]), reads=[R_c], writes=[R_c])
        P.op("pool", lambda e: e.tensor_copy(out=onesb[:, :], in_=cst["ones"][:, :]), reads=[R_c], writes=[R_c])
        P.op("pool", lambda e: e.tensor_copy(out=ltb[:, :], in_=cst["sl_b"][:, :]), reads=[R_c], writes=[R_c])
        P.op("pool", lambda e: e.tensor_copy(out=tokb[:, :, :].rearrange("p a b -> p (a b)"), in_=tokf[:, :]), reads=[R_c], writes=[R_c])
        P.op("pool", lambda e: e.memset(epsc[:, :], EPS), writes=[R_c])
        P.op("pool", lambda e: e.memset(onec[:, :], 1.0), writes=[R_c])
        P.op("pool", lambda e: e.memset(mhalf[:, :], -0.5), writes=[R_c])
        P.op("act", lambda e: e.activation(out=small[:, 0:8], in_=abp[:, 0:8], func=AF.Exp), reads=[R_c], writes=[R_c])
        P.op("dve", lambda e: e.tensor_scalar(out=small[:, 0:8], in0=small[:, 0:8], scalar1=-1.0, scalar2=None, op0=ALU.mult), reads=[R_c], writes=[R_c])
        P.barrier()
        RC = [R_c]

        ident = cst["ident"]

        XNT = view(OFF_A, [128, 8, T], BF16)
        WINP = [view(OFF_A + 32 * K + i * 8704, [128, 8, 528], BF16) for i in range(2)]
        RAW = view(OFF_A + 32 * K + 17408, [128, 2056], F32)
        RAWS = [RAW, view(OFF_D, [128, 2056], F32)]
        NWBC1 = view(OFF_A + 32 * K + 17408 + 8224, [128, D], F32)
        SB2 = view(OFF_A + 32 * K + 17408, [128, T + 32], F32)
        QT = view(OFF_B, [128, H, T], BF16)
        KT = view(OFF_B + 16 * K, [128, H, T], BF16)
        KTOK = view(OFF_C, [128, NTC, H, 128], BF16)
        VTOK = view(OFF_C + 16 * K, [128, NTC, H, 128], BF16)
        ZS = view(OFF_D, [128, NTC, 512], BF16)
        OPT = view(OFF_E, [128, 4, T], BF16)
        SQ = view(OFF_E, [128, T], BF16)
        VTMP = view(OFF_E, [128, T], BF16)
        RSQ = view(OFF_E + 4 * K, [128, T], F32)
        XT = [view(OFF_F + i * 4 * K, [128, D], F32) for i in range(2)]
        XT4 = XT + [view(OFF_F + 25 * K, [128, D], F32), view(OFF_D + 11 * K, [128, D], F32)]
        XN = view(OFF_F + 8 * K, [128, D], BF16)
        CONVT = view(OFF_F + 10 * K, [128, T], F32)
        CONVS = [CONVT, view(OFF_F + 21 * K, [128, T], F32)]
        UB = view(OFF_F + 10 * K, [128, T + 32], F32)
        ZF = view(OFF_F + 10 * K, [128, 512], F32)
        ABS = view(OFF_F + 19 * K, [128, 16, 16], F32)
        TM1 = view(OFF_F + 20 * K, [128, 16, 8], F32)
        PB3 = view(OFF_F + 21 * K, [128, T + 32], F32)
        DIFB = view(OFF_F, [128, T], BF16)
        R_xnt = [R() for _ in range(NTC)]
        R_xt = [R(), R()]
        R_xt4 = R_xt + [R(), R()]
        R_xn = R()
        R_winp = [R(), R()]
        R_raw = R()
        R_acc = R()
        R_raws = [R_raw, R()]
        R_accs = [R_acc, R()]
        R_nw = R()
        R_qt = [R() for _ in range(H)]
        R_kt = [R() for _ in range(H)]
        R_ktok = R()
        R_vtok = R()
        R_zs = [R() for _ in range(NTC)]
        R_opt = [R() for _ in range(4)]
        R_abs = R()
        R_pb3 = R()
        R_difb = R()

        P.dma("sp", ds_c, lambda e: e.dma_start(out=NWBC1[:, :], in_=nw_d[0]), writes=[R_nw])
        P.op("pool", lambda e: e.memset(RAW[:, :], 0.0), writes=[R_raw])
        P.op("pool", lambda e: e.memset(RAWS[1][:, :], 0.0), writes=[R_raws[1]])

        XNS = [XN, view(OFF_D + 9 * K, [128, D], BF16)]
        R_xns = [R_xn, R()]
        R_sm1 = [R(), R()]
        JUNK = view(OFF_F + 21 * K, [128, D], F32)
        R_junk = R()
        def front_1a(tc):
            b = tc % 2
            sc = 8 + 3 * b
            xb = tc % 4
            P.dma("sp", ds_x4[xb], lambda e: e.dma_start(out=XT4[xb][:, :], in_=x_d[tc * 128:(tc + 1) * 128, :]), writes=[R_xt4[xb]])
            P.op("act", lambda e: e.activation(out=JUNK[:, :], in_=XT4[xb][:, :], func=AF.Square, accum_out=small[:, sc:sc + 1]),
                 reads=[R_xt4[xb]], writes=[R_junk, R_sm1[b]])
            P.op("act", lambda e: e.activation(out=small[:, sc + 1:sc + 2], in_=small[:, sc:sc + 1], func=AF.Ln, bias=epsc[:, :], scale=1.0 / D),
                 reads=[R_c], writes=[R_sm1[b]])
            P.op("act", lambda e: e.activation(out=small[:, sc + 2:sc + 3], in_=small[:, sc + 1:sc + 2], func=AF.Exp, scale=-0.5), reads=[], writes=[R_sm1[b]])
            P.op("dve", lambda e: e.scalar_tensor_tensor(out=XNS[b][:, :], in0=XT4[xb][:, :], scalar=small[:, sc + 2:sc + 3], in1=NWBC1[:, :],
                                                         op0=ALU.mult, op1=ALU.mult),
                 reads=[R_xt4[xb], R_sm1[b], R_nw], writes=[R_xns[b]])

        def back_1a(tc):
            b = tc % 2
            pb = tc % 2
            for dc in range(8):
                P.op("pe", lambda e, dc=dc: e.transpose(psb(pb, BF16)[:, dc * 128:(dc + 1) * 128], XNS[b][:, dc * 128:(dc + 1) * 128], identb[:, :]),
                     reads=[R_xns[b], R_c], writes=[R_ps[pb]])
            P.op("act", lambda e: e.activation(out=XNT[:, :, tc * 128:(tc + 1) * 128],
                                               in_=psb(pb, BF16)[:, 0:1024].rearrange("p (a b) -> p a b", b=128), func=AF.Copy),
                 reads=[], writes=[R_ps[pb], R_xnt[tc]])

        front_1a(0)
        for tc in range(NTC):
            if tc + 1 < NTC:
                front_1a(tc + 1)
            back_1a(tc)

        wpi = [0]

        def load_winp(col0, ncol):
            i = wpi[0] % 2
            wpi[0] += 1
            P.dma("pool", ds_w[i], lambda e, i=i: e.dma_start(out=WINP[i][:, :, 0:ncol],
                                                              in_=win_d.rearrange("(c p) n -> p c n", p=128)[:, :, col0:col0 + ncol]),
                  writes=[R_winp[i]])
            return i

        def proj_chunk(wi, lcol, evac):
            for tb in range(4):
                pb = 2 + tb % 2
                for dc in range(8):
                    P.op("pe", lambda e, dc=dc, tb=tb, pb=pb: e.matmul(psb(pb), WINP[wi][:, dc, lcol:lcol + 128], XNT[:, dc, tb * 512:(tb + 1) * 512],
                                                                      start=(dc == 0), stop=(dc == 7)),
                         reads=[R_winp[wi]] + R_xnt[tb * 4:(tb + 1) * 4], writes=[R_ps[pb]])
                evac(tb, pb)

        def mk_evac_raw(RAWB, R_rawb):
            def evac_raw(tb, pb):
                P.op("act", lambda e, tb=tb, pb=pb: e.activation(out=RAWB[:, 2 + tb * 512: 2 + (tb + 1) * 512], in_=psb(pb), func=AF.Copy),
                     reads=[], writes=[R_ps[pb], R_rawb])
            return evac_raw

        RSQS = [RSQ, RSQ]
        R_rsqs = [R_opt[1], R_opt[1]]
        chunks = [(grp, kind, hh) for grp, kind in enumerate(("q", "k", "v")) for hh in range(H)]
        wis = {}

        def stage_a(ci):
            grp, kind, hh = chunks[ci]
            if hh == 0:
                wis[grp] = load_winp(grp * 512, 512)
            wi = wis[grp]
            cc = grp * 4 + hh
            RAWB, R_rawb = RAWS[cc % 2], R_raws[cc % 2]
            CONVB, R_accb = CONVS[cc % 2], R_accs[cc % 2]
            proj_chunk(wi, hh * 128, mk_evac_raw(RAWB, R_rawb))

        def stage_c(ci):
            grp, kind, hh = chunks[ci]
            cc = grp * 4 + hh
            RAWB, R_rawb = RAWS[cc % 2], R_raws[cc % 2]
            CONVB, R_accb = CONVS[cc % 2], R_accs[cc % 2]
            P.op("dve", lambda e: e.tensor_scalar(out=CONVB[:, :], in0=RAWB[:, 0:T], scalar1=cw[:, cc, 0:1], scalar2=None, op0=ALU.mult),
                 reads=[R_rawb, R_c], writes=[R_accb])
            for j in range(1, 5):
                P.op("dve", lambda e, j=j: e.scalar_tensor_tensor(out=CONVB[:, :], in0=RAWB[:, j:j + T], scalar=cw[:, cc, j:j + 1], in1=CONVB[:, :],
                                                                  op0=ALU.mult, op1=ALU.add),
                     reads=[R_rawb, R_c], writes=[R_accb])
            if kind == "v":
                P.op("act", lambda e: e.activation(out=VTMP[:, :], in_=CONVB[:, :], func=AF.Silu), reads=[R_accb], writes=[R_opt[0]])
                for half in range(2):
                    pb = 4 + half
                    for t8 in range(8):
                        tc = half * 8 + t8
                        P.op("pe", lambda e, tc=tc, t8=t8, pb=pb: e.transpose(psb(pb, BF16)[:, t8 * 128:(t8 + 1) * 128], VTMP[:, tc * 128:(tc + 1) * 128], identb[:, :]),
                             reads=[R_opt[0], R_c], writes=[R_ps[pb]])
                    P.op("act", lambda e, half=half, pb=pb: e.activation(out=VTOK[:, half * 8:(half + 1) * 8, hh, :],
                                                                        in_=psb(pb, BF16)[:, 0:1024].rearrange("p (a b) -> p a b", b=128), func=AF.Copy),
                         reads=[], writes=[R_ps[pb], R_vtok])
            else:
                P.op("act", lambda e: e.activation(out=CONVB[:, :], in_=CONVB[:, :], func=AF.Silu), reads=[], writes=[R_accb])
                P.op("pool", lambda e: e.tensor_tensor(out=SQ[:, :], in0=CONVB[:, :], in1=CONVB[:, :], op=ALU.mult), reads=[R_accb], writes=[R_opt[0]])

        def stage_a2(ci):
            grp, kind, hh = chunks[ci]
            cc = grp * 4 + hh
            RSQB, R_rsqb = RSQS[cc % 2], R_rsqs[cc % 2]
            if kind != "v":
                for tb in range(4):
                    pb = 4 + tb % 2
                    P.op("pe", lambda e, tb=tb, pb=pb: e.matmul(psb(pb), onesb[:, :], SQ[:, tb * 512:(tb + 1) * 512], start=True, stop=True),
                         reads=[R_opt[0], R_c], writes=[R_ps[pb]])
                    P.op("act", lambda e, tb=tb, pb=pb: e.activation(out=RSQB[:, tb * 512:(tb + 1) * 512], in_=psb(pb), func=AF.Ln, bias=epsc[:, :], scale=1.0),
                         reads=[R_c], writes=[R_ps[pb], R_rsqb])

        def stage_b(ci):
            grp, kind, hh = chunks[ci]
            if kind == "v":
                return
            cc = grp * 4 + hh
            CONVB, R_accb = CONVS[cc % 2], R_accs[cc % 2]
            RSQB, R_rsqb = RSQS[cc % 2], R_rsqs[cc % 2]
            P.op("act", lambda e: e.activation(out=RSQB[:, :], in_=RSQB[:, :], func=AF.Exp, scale=-0.5), reads=[], writes=[R_rsqb])
            dst = QT if kind == "q" else KT
            rdst = R_qt[hh] if kind == "q" else R_kt[hh]
            scl = 128.0 ** -0.5 if kind == "q" else 1.0
            P.op("dve", lambda e: e.scalar_tensor_tensor(out=dst[:, hh, :], in0=CONVB[:, :], scalar=scl, in1=RSQB[:, :], op0=ALU.mult, op1=ALU.mult),
                 reads=[R_accb, R_rsqb], writes=[rdst])
            if kind == "k":
                for half in range(2):
                    pb = 6 + half
                    for t8 in range(8):
                        tc = half * 8 + t8
                        P.op("pe", lambda e, tc=tc, t8=t8, pb=pb: e.transpose(psb(pb, BF16)[:, t8 * 128:(t8 + 1) * 128], KT[:, hh, tc * 128:(tc + 1) * 128], identb[:, :]),
                             reads=[R_kt[hh], R_c], writes=[R_ps[pb]])
                    P.op("act", lambda e, half=half, pb=pb: e.activation(out=KTOK[:, half * 8:(half + 1) * 8, hh, :],
                                                                        in_=psb(pb, BF16)[:, 0:1024].rearrange("p (a b) -> p a b", b=128), func=AF.Copy),
                         reads=[], writes=[R_ps[pb], R_ktok])

        NCH = len(chunks)
        stage_a(0)
        for ci in range(NCH + 1):
            if ci + 1 < NCH:
                stage_a(ci + 1)
            if ci < NCH:
                stage_c(ci)
            if ci >= 1:
                stage_b(ci - 1)
            if ci < NCH:
                stage_a2(ci)
        P.barrier()

        wi = load_winp(1536, 528)
        for tc in range(NTC):
            pb = 2 + tc % 2
            for dc in range(8):
                P.op("pe", lambda e, dc=dc, tc=tc, pb=pb, wi=wi: e.matmul(psb(pb), XNT[:, dc, tc * 128:(tc + 1) * 128], WINP[wi][:, dc, 0:512], start=(dc == 0), stop=(dc == 7)),
                     reads=[R_winp[wi], R_xnt[tc]], writes=[R_ps[pb]])
            P.op("act", lambda e, pb=pb: e.activation(out=ZF[:, :], in_=psb(pb), func=AF.Silu), reads=[], writes=[R_ps[pb], R_acc])
            P.op("dve", lambda e, tc=tc: e.tensor_tensor(out=ZS[:, tc, :].rearrange("p (h d) -> p h d", d=128), in0=ZF[:, :].rearrange("p (h d) -> p h d", d=128),
                                                         in1=hnw[:, :].unsqueeze(1).to_broadcast([128, 4, 128]), op=ALU.mult),
                 reads=[R_acc, R_c], writes=[R_zs[tc]])
        ABV = psb(4)[:, 0:256].rearrange("p (a b) -> p a b", b=16)
        for tc in range(NTC):
            for dc in range(8):
                P.op("pe", lambda e, dc=dc, tc=tc, wi=wi: e.matmul(ABV[:, tc, :], XNT[:, dc, tc * 128:(tc + 1) * 128], WINP[wi][:, dc, 512:528], start=(dc == 0), stop=(dc == 7)),
                     reads=[R_winp[wi], R_xnt[tc]], writes=[R_ps[4]])
        P.op("act", lambda e: e.activation(out=ABS[:, :, :], in_=ABV, func=AF.Copy), reads=[], writes=[R_ps[4], R_abs])
        ABS5 = ABS[:, :, :].rearrange("p a (d k h) -> p a d k h", d=2, k=2)
        for d_ in range(2):
            P.op("dve", lambda e, d_=d_: e.tensor_tensor(out=TM1[:, :, d_ * 4:(d_ + 1) * 4], in0=ABS5[:, :, d_, 0, :],
                                                         in1=abp[:, 8 + d_ * 4: 12 + d_ * 4].unsqueeze(1).to_broadcast([128, 16, 4]), op=ALU.add),
                 reads=[R_abs, R_c], writes=[R_g])
            P.op("act", lambda e, d_=d_: e.activation(out=bet[:, :, d_ * 4:(d_ + 1) * 4], in_=ABS5[:, :, d_, 1, :], func=AF.Sigmoid),
                 reads=[R_abs], writes=[R_g])
        P.op("act", lambda e: e.activation(out=TM1[:, :, :], in_=TM1[:, :, :], func=AF.Exp), reads=[], writes=[R_g])
        P.op("act", lambda e: e.activation(out=TM1[:, :, :], in_=TM1[:, :, :], func=AF.Ln, bias=onec[:, :], scale=1.0), reads=[R_c], writes=[R_g])
        P.op("dve", lambda e: e.tensor_tensor(out=gsm[:, :, :], in0=TM1[:, :, :], in1=small[:, 0:8].unsqueeze(1).to_broadcast([128, 16, 8]), op=ALU.mult),
             reads=[R_c], writes=[R_g])
        P.op("dve", lambda e: e.tensor_scalar(out=nbet[:, :, :], in0=bet[:, :, :], scalar1=-1.0, scalar2=None, op0=ALU.mult), reads=[], writes=[R_g])

        wi = load_winp(2064, 512)
        TW = T + 8
        P.op("pool", lambda e: e.memset(UB[:, :], 0.0), reads=[], writes=[R_acc])
        P.op("pool", lambda e: e.memset(SB2[:, :], 0.0), reads=[], writes=[R_raw, R_nw])
        P.op("pool", lambda e: e.memset(PB3[:, :], 0.0), reads=[], writes=[R_pb3])

        def evac_u(tb, pb):
            P.op("act", lambda e, tb=tb, pb=pb: e.activation(out=UB[:, 16 + tb * 512: 16 + (tb + 1) * 512], in_=psb(pb), func=AF.Copy),
                 reads=[], writes=[R_ps[pb], R_acc])

        for g, w in enumerate((2, 4, 8, 16)):
            lo = w // 2
            hi = w - lo - 1
            proj_chunk(wi, g * 128, evac_u)
            s_prev, r_prev = UB, R_acc
            sh = 1
            for lv in range({2: 1, 4: 2, 8: 3, 16: 4}[w]):
                dstb, rdst = (SB2, R_raw) if lv % 2 == 0 else (PB3, R_pb3)
                P.op("dve", lambda e, s_prev=s_prev, dstb=dstb, sh=sh: e.tensor_tensor(out=dstb[:, 16:16 + TW], in0=s_prev[:, 16:16 + TW], in1=s_prev[:, 16 - sh:16 - sh + TW], op=ALU.add),
                     reads=[r_prev], writes=[rdst])
                s_prev, r_prev = dstb, rdst
                sh *= 2
            P.op("dve", lambda e, s_prev=s_prev, hi=hi, w=w: e.scalar_tensor_tensor(out=DIFB[:, :], in0=s_prev[:, 16 + hi:16 + hi + T], scalar=1.0 / w, in1=UB[:, 16:16 + T],
                                                                                   op0=ALU.mult, op1=ALU.subtract),
                 reads=[r_prev, R_acc], writes=[R_difb])
            P.op("dve", lambda e, s_prev=s_prev, hi=hi, lo=lo, g=g: e.tensor_tensor(out=small[:, 16:16 + lo], in0=s_prev[:, 16 + hi:16 + hi + lo], in1=invc[:, g * 16:g * 16 + lo], op=ALU.mult),
                 reads=[r_prev, R_c], writes=[R_small])
            P.op("dve", lambda e, lo=lo: e.tensor_tensor(out=DIFB[:, 0:lo], in0=small[:, 16:16 + lo], in1=UB[:, 16:16 + lo], op=ALU.subtract),
                 reads=[R_small, R_acc], writes=[R_difb])
            if hi > 0:
                P.op("dve", lambda e, s_prev=s_prev, hi=hi, g=g: e.tensor_tensor(out=small[:, 32:32 + hi], in0=s_prev[:, 16 + T:16 + T + hi], in1=invc[:, g * 16 + 8:g * 16 + 8 + hi], op=ALU.mult),
                     reads=[r_prev, R_c], writes=[R_small])
                P.op("dve", lambda e, hi=hi: e.tensor_tensor(out=DIFB[:, T - hi:T], in0=small[:, 32:32 + hi], in1=UB[:, 16 + T - hi:16 + T], op=ALU.subtract),
                     reads=[R_small, R_acc], writes=[R_difb])
            for tb in range(4):
                pb = 4 + tb % 2
                P.op("pe", lambda e, tb=tb, pb=pb, g=g: e.matmul(psb(pb), pwb[:, g, :], DIFB[:, tb * 512:(tb + 1) * 512], start=True, stop=True),
                     reads=[R_difb, R_c], writes=[R_ps[pb]])
                P.op("act", lambda e, tb=tb, pb=pb, g=g: e.activation(out=OPT[:, g, tb * 512:(tb + 1) * 512], in_=psb(pb), func=AF.Identity, scale=psc[:, g:g + 1]),
                     reads=[R_c], writes=[R_ps[pb], R_opt[g]])
        P.barrier()

        if DEBUG:
            ds_dbg = newds("ds_dbg")
            for name, ap, shape, dt in (("d_qt", QT, [128, H * T], BF16), ("d_kt", KT, [128, H * T], BF16),
                                        ("d_ktok", KTOK, [128, NTC * H * 128], BF16), ("d_vtok", VTOK, [128, NTC * H * 128], BF16),
                                        ("d_zs", ZS, [128, NTC * 512], BF16), ("d_opt", OPT, [128, 4 * T], BF16)):
                dd = nc.dram_tensor(name, shape, dt, kind="ExternalOutput").ap()
                flat = ap
                if len(ap.shape) == 3:
                    flat = ap.rearrange("p a b -> p (a b)")
                elif len(ap.shape) == 4:
                    flat = ap.rearrange("p a b c -> p (a b c)")
                out_toks.append(P.dma("sp", ds_dbg, lambda e, dd=dd, flat=flat: e.dma_start(out=dd, in_=flat)))
            for name, tl in (("d_gsm", gsm), ("d_bet", bet)):
                dd = nc.dram_tensor(name, [128, 128], F32, kind="ExternalOutput").ap()
                out_toks.append(P.dma("sp", ds_dbg, lambda e, dd=dd, tl=tl: e.dma_start(out=dd, in_=tl[:, :, :].rearrange("p a b -> p (a b)"))))

        NEU = F32
        OACC = view(OFF_A, [128, NTC, H, 128], F32)

        def make_ctx(base, limit, banks):
            o2 = [base]

            def tv(shape, dt):
                n = 1
                for s_ in shape[1:]:
                    n *= s_
                nb = n * (4 if dt == F32 else 2)
                off = o2[0]
                o2[0] += nb
                assert o2[0] <= limit, (o2[0], limit)
                return view(off, shape, dt)
            c = {}
            for nm in ("MG", "EM", "EMS", "BYV"):
                c[nm] = tv([128, 4, 128], F32)
            for nm in ("AK", "ATK", "RRK"):
                c[nm] = [tv([128, 4, 128], NEU) for _ in range(2)]
            for nm in ("QKT", "NTB", "EK", "KDEC", "YWT", "VNEW"):
                c[nm] = tv([128, 4, 128], BF16)
            for nm in ("MG", "EM", "EMS", "BYV", "QKT", "NTB", "EK", "KDEC", "YWT", "VNEW"):
                c["R_" + nm] = R()
            for nm in ("AK", "ATK", "RRK"):
                c["R_" + nm] = [R(), R()]
            c["banks"] = banks
            c["end"] = o2[0]
            return c

        ctxs = [make_ctx(OFF_A + 32 * K, OFF_A + 64 * K, (0, 1, 2, 3)), make_ctx(OFF_F, ARENA_B, (4, 5, 6, 7))]
        SST = view(ctxs[0]["end"], [128, 8, 128], F32)
        SBF = view(ctxs[0]["end"] + 4 * K, [128, 8, 128], BF16)
        assert ctxs[0]["end"] + 6 * K <= OFF_A + 64 * K
        R_oacc = [R() for _ in range(NTC)]
        R_s = [R(), R()]
        R_sbf = [R(), R()]

        for n in range(NTC):
            P.op("pool", lambda e, n=n: e.memset(OACC[:, n, :, :], 0.0), writes=[R_oacc[n]])
        P.op("pool", lambda e: e.memset(SST[:, :, :], 0.0), writes=R_s)
        P.op("pool", lambda e: e.memset(SBF[:, :, :], 0.0), writes=R_sbf)

        EV = psb(0)[:, 0:384].rearrange("p (a b) -> p a b", b=24)
        for d_ in range(2):
            sfx = "_f" if d_ == 0 else "_b"
            for j, lh in enumerate((cst["ut" + sfx], cst["sl" + sfx], cst["ones"])):
                c0 = j * 8 + d_ * 4
                P.op("pe", lambda e, d_=d_, lh=lh, c0=c0: e.matmul(EV[:, :, c0:c0 + 4], lh[:, :], gsm[:, :, d_ * 4:(d_ + 1) * 4], start=True, stop=True),
                     reads=[R_c, R_g], writes=[R_ps[0]])
        P.op("act", lambda e: e.activation(out=exps[:, :, :], in_=EV, func=AF.Exp), reads=[], writes=[R_ps[0], R_exps])

        def v4(i):
            return psb(i).rearrange("p (h c) -> p h c", h=4)

        identbc = ident[:, :].unsqueeze(1).to_broadcast([128, 4, 128])

        def group(n, d_, c):
            sfx = "_f" if d_ == 0 else "_b"
            ut, sl, neg, st = cst["ut" + sfx], cst["sl" + sfx], cst["neg" + sfx], cst["str" + sfx]
            ns = slice(n * 128, (n + 1) * 128)
            c4 = d_ * 4
            b0, b1, b2, b3 = c["banks"]
            MG, EM, EMS, BYV, AK, ATK, RRK = c["MG"], c["EM"], c["EMS"], c["BYV"], c["AK"], c["ATK"], c["RRK"]
            QKT, NTB, EK, KDEC, YWT, VNEW = c["QKT"], c["NTB"], c["EK"], c["KDEC"], c["YWT"], c["VNEW"]
            R_mg, R_em, R_ems, R_byv, R_ak, R_atk, R_rrk = c["R_MG"], c["R_EM"], c["R_EMS"], c["R_BYV"], c["R_AK"], c["R_ATK"], c["R_RRK"]
            R_qkt, R_ntb, R_ek, R_kdec, R_ywt, R_vnew = c["R_QKT"], c["R_NTB"], c["R_EK"], c["R_KDEC"], c["R_YWT"], c["R_VNEW"]
            for h in range(4):
                P.op("pool", lambda e, h=h: e.tensor_scalar(out=MG[:, h, :], in0=sl[:, :], scalar1=gsm[:, n, c4 + h:c4 + h + 1], scalar2=1.0, op0=ALU.mult, op1=ALU.mult),
                     reads=[R_c, R_g], writes=[R_mg])
            yield
            DV = v4(b1)
            for h in range(4):
                P.op("pe", lambda e, h=h: e.matmul(DV[:, h, :], MG[:, h, :], ut[:, :], start=True, stop=False), reads=[R_mg, R_c], writes=[R_ps[b1]])
                P.op("pe", lambda e, h=h: e.matmul(DV[:, h, :], ident[:, :], neg[:, :], start=False, stop=True), reads=[R_c], writes=[R_ps[b1]])
            yield
            P.op("act", lambda e: e.activation(out=EM[:, :, :], in_=DV, func=AF.Exp), reads=[], writes=[R_ps[b1], R_em])
            P.op("pool", lambda e: e.tensor_tensor(out=EMS[:, :, :], in0=EM[:, :, :], in1=st[:, :].unsqueeze(1).to_broadcast([128, 4, 128]), op=ALU.mult),
                 reads=[R_em, R_c], writes=[R_ems])
            yield
            for hp in range(2):
                V4 = psb(b0).rearrange("p (h k c) -> p h k c", h=2, k=2)
                for hl in range(2):
                    h = hp * 2 + hl
                    P.op("pe", lambda e, h=h, hl=hl: e.matmul(V4[:, hl, 0, :], KT[:, h, ns], KT[:, h, ns], start=True, stop=True), reads=[R_kt[h]], writes=[R_ps[b0]])
                    P.op("pe", lambda e, h=h, hl=hl: e.matmul(V4[:, hl, 1, :], KT[:, h, ns], QT[:, h, ns], start=True, stop=True), reads=[R_kt[h], R_qt[h]], writes=[R_ps[b0]])
                yield
                for hl in range(2):
                    h = hp * 2 + hl
                    P.op("dve", lambda e, h=h, hl=hl: e.scalar_tensor_tensor(out=AK[0][:, h, :], in0=V4[:, hl, 0, :], scalar=nbet[:, n, c4 + h:c4 + h + 1], in1=EMS[:, h, :],
                                                                             op0=ALU.mult, op1=ALU.mult),
                         reads=[R_g, R_ems], writes=[R_ps[b0], R_ak[0]])
                P.op("dve", lambda e, hp=hp: e.tensor_tensor(out=QKT[:, 2 * hp:2 * hp + 2, :], in0=V4[:, :, 1, :], in1=EM[:, 2 * hp:2 * hp + 2, :], op=ALU.mult),
                     reads=[R_em], writes=[R_ps[b0], R_qkt])
                yield
            TV = psb(b1, BF16)[:, 0:512].rearrange("p (h c) -> p h c", h=4) if NEU == BF16 else v4(b1)
            idn = identb if NEU == BF16 else ident
            for h in range(4):
                P.op("pe", lambda e, h=h: e.transpose(TV[:, h, :], AK[0][:, h, :], idn[:, :]), reads=[R_ak[0], R_c], writes=[R_ps[b1]])
            yield
            P.op("act", lambda e: e.activation(out=ATK[0][:, :, :], in_=TV, func=AF.Copy), reads=[], writes=[R_ps[b1], R_atk[0]])
            P.op("pool", lambda e: e.tensor_tensor(out=RRK[0][:, :, :], in0=AK[0][:, :, :], in1=identbc, op=ALU.add), reads=[R_ak[0], R_c], writes=[R_rrk[0]])
            yield
            cur = 0
            AV, ATV, RV = v4(b2), v4(b3), v4(b1)
            for k in range(1, 7):
                nxt = 1 - cur
                for h in range(4):
                    P.op("pe", lambda e, h=h, cur=cur: e.matmul(ATV[:, h, :], AK[cur][:, h, :], ATK[cur][:, h, :], start=True, stop=True),
                         reads=[R_ak[cur], R_atk[cur]], writes=[R_ps[b3]])
                if k < 6:
                    for h in range(4):
                        P.op("pe", lambda e, h=h, cur=cur: e.matmul(AV[:, h, :], ATK[cur][:, h, :], AK[cur][:, h, :], start=True, stop=True),
                             reads=[R_ak[cur], R_atk[cur]], writes=[R_ps[b2]])
                yield
                P.op("act", lambda e, nxt=nxt: e.activation(out=ATK[nxt][:, :, :], in_=ATV, func=AF.Copy), reads=[], writes=[R_ps[b3], R_atk[nxt]])
                if k < 6:
                    P.op("dve", lambda e, nxt=nxt: e.tensor_copy(out=AK[nxt][:, :, :], in_=AV), reads=[], writes=[R_ps[b2], R_ak[nxt]])
                yield
                for h in range(4):
                    P.op("pe", lambda e, h=h, cur=cur, nxt=nxt: e.matmul(RV[:, h, :], ATK[nxt][:, h, :], RRK[cur][:, h, :], start=True, stop=True),
                         reads=[R_atk[nxt], R_rrk[cur]], writes=[R_ps[b1]])
                yield
                if k < 6:
                    P.op("dve", lambda e, cur=cur, nxt=nxt: e.tensor_tensor(out=RRK[nxt][:, :, :], in0=RV, in1=RRK[cur][:, :, :], op=ALU.add),
                         reads=[R_rrk[cur]], writes=[R_ps[b1], R_rrk[nxt]])
                else:
                    P.op("dve", lambda e, cur=cur: e.tensor_tensor(out=NTB[:, :, :], in0=RV, in1=RRK[cur][:, :, :], op=ALU.add),
                         reads=[R_rrk[cur]], writes=[R_ps[b1], R_ntb])
                cur = nxt
                yield
            for h in range(4):
                P.op("act", lambda e, h=h: e.activation(out=EK[:, h, :], in_=KTOK[:, n, h, :], func=AF.Identity, scale=exps[:, n, c4 + h:c4 + h + 1]),
                     reads=[R_ktok, R_exps], writes=[R_ek])
                P.op("act", lambda e, h=h: e.activation(out=KDEC[:, h, :], in_=KTOK[:, n, h, :], func=AF.Identity, scale=exps[:, n, 8 + c4 + h:8 + c4 + h + 1]),
                     reads=[R_ktok, R_exps], writes=[R_kdec])
            yield
            YVV, YWV = v4(b0), v4(b2)
            for h in range(4):
                P.op("pe", lambda e, h=h: e.matmul(YVV[:, h, :], NTB[:, h, :], VTOK[:, n, h, :], start=True, stop=True), reads=[R_ntb, R_vtok], writes=[R_ps[b0]])
            for h in range(4):
                P.op("pe", lambda e, h=h: e.matmul(YWV[:, h, :], EK[:, h, :], NTB[:, h, :], start=True, stop=True), reads=[R_ntb, R_ek], writes=[R_ps[b2]])
            yield
            for h in range(4):
                P.op("act", lambda e, h=h: e.activation(out=BYV[:, h, :], in_=YVV[:, h, :], func=AF.Identity, scale=bet[:, n, c4 + h:c4 + h + 1]),
                     reads=[R_g], writes=[R_ps[b0], R_byv])
            P.op("act", lambda e: e.activation(out=YWT[:, :, :], in_=YWV, func=AF.Copy), reads=[], writes=[R_ps[b2], R_ywt])
            yield
            P1V, O1V, O2V, SUV = v4(b1), v4(b3), v4(b2), v4(b0)
            for h in range(4):
                P.op("pe", lambda e, h=h: e.matmul(P1V[:, h, :], YWT[:, h, :], SBF[:, c4 + h, :], start=True, stop=True), reads=[R_ywt, R_sbf[d_]], writes=[R_ps[b1]])
            for h in range(4):
                P.op("pe", lambda e, h=h: e.matmul(O1V[:, h, :], QT[:, h, ns], SBF[:, c4 + h, :], start=True, stop=True), reads=[R_qt[h], R_sbf[d_]], writes=[R_ps[b3]])
            yield
            for h in range(4):
                P.op("dve", lambda e, h=h: e.scalar_tensor_tensor(out=VNEW[:, h, :], in0=P1V[:, h, :], scalar=nbet[:, n, c4 + h:c4 + h + 1], in1=BYV[:, h, :],
                                                                  op0=ALU.mult, op1=ALU.add),
                     reads=[R_g, R_byv], writes=[R_ps[b1], R_vnew])
            yield
            for h in range(4):
                P.op("pe", lambda e, h=h: e.matmul(O2V[:, h, :], QKT[:, h, :], VNEW[:, h, :], start=True, stop=True), reads=[R_qkt, R_vnew], writes=[R_ps[b2]])
            for h in range(4):
                P.op("pe", lambda e, h=h: e.matmul(SUV[:, h, :], KDEC[:, h, :], VNEW[:, h, :], start=True, stop=True), reads=[R_kdec, R_vnew], writes=[R_ps[b0]])
            yield
            for h in range(4):
                P.op("dve", lambda e, h=h: e.scalar_tensor_tensor(out=SST[:, c4 + h, :], in0=SST[:, c4 + h, :], scalar=exps[:, n, 16 + c4 + h:16 + c4 + h + 1], in1=SUV[:, h, :],
                                                                  op0=ALU.mult, op1=ALU.add),
                     reads=[R_exps], writes=[R_ps[b0], R_s[d_]])
            P.op("act", lambda e: e.activation(out=SBF[:, c4:c4 + 4, :], in_=SST[:, c4:c4 + 4, :], func=AF.Copy), reads=[R_s[d_]], writes=[R_sbf[d_]])
            yield
            for h in range(4):
                P.op("dve", lambda e, h=h: e.scalar_tensor_tensor(out=OACC[:, n, h, :], in0=O1V[:, h, :], scalar=exps[:, n, c4 + h:c4 + h + 1], in1=OACC[:, n, h, :],
                                                                  op0=ALU.mult, op1=ALU.add),
                     reads=[R_exps], writes=[R_ps[b3], R_oacc[n]])
            P.op("dve", lambda e: e.tensor_tensor(out=OACC[:, n, :, :], in0=O2V, in1=OACC[:, n, :, :], op=ALU.add), reads=[], writes=[R_ps[b2], R_oacc[n]])
            yield

        for i in range(NTC):
            precast(14)
            gens = [group(i, 0, ctxs[0]), group(NTC - 1 - i, 1, ctxs[1])]
            while gens:
                for g_ in list(gens):
                    try:
                        next(g_)
                    except StopIteration:
                        gens.remove(g_)
        P.barrier()

        OT = view(OFF_B, [128, 4, T], BF16)
        TMPO = view(OFF_A + 32 * K, [128, 4, 128], F32)
        OGB = view(OFF_A + 34 * K, [128, 512], BF16)
        R_ot = [R() for _ in range(NTC)]
        R_tmpo = R()
        R_ogb = R()
        for n in range(NTC):
            P.op("dve", lambda e, n=n: e.tensor_tensor(out=TMPO[:, :, :], in0=OACC[:, n, :, :], in1=OACC[:, n, :, :], op=ALU.mult), reads=[R_oacc[n]], writes=[R_tmpo])
            P.op("dve", lambda e: e.tensor_reduce(out=small[:, 40:44], in_=TMPO[:, :, :], axis=AX.X, op=ALU.add), reads=[R_tmpo], writes=[R_small])
            P.op("act", lambda e: e.activation(out=small[:, 44:48], in_=small[:, 40:44], func=AF.Sqrt, bias=epsc[:, :], scale=1.0 / 128), reads=[R_c], writes=[R_small])
            P.op("dve", lambda e: e.reciprocal(out=small[:, 48:52], in_=small[:, 44:48]), reads=[], writes=[R_small])
            P.op("dve", lambda e, n=n: e.tensor_tensor(out=TMPO[:, :, :], in0=OACC[:, n, :, :], in1=small[:, 48:52].unsqueeze(2).to_broadcast([128, 4, 128]), op=ALU.mult),
                 reads=[R_oacc[n], R_small], writes=[R_tmpo])
            P.op("dve", lambda e, n=n: e.tensor_tensor(out=OGB[:, :], in0=TMPO[:, :, :].rearrange("p h d -> p (h d)"), in1=ZS[:, n, :], op=ALU.mult),
                 reads=[R_tmpo, R_zs[n]], writes=[R_ogb])
            pb = n % 2
            for h in range(4):
                P.op("pe", lambda e, h=h, pb=pb: e.transpose(psb(pb, BF16)[:, h * 128:(h + 1) * 128], OGB[:, h * 128:(h + 1) * 128], identb[:, :]),
                     reads=[R_ogb, R_c], writes=[R_ps[pb]])
            P.op("act", lambda e, n=n, pb=pb: e.activation(out=OT[:, :, n * 128:(n + 1) * 128], in_=psb(pb, BF16)[:, 0:512].rearrange("p (a b) -> p a b", b=128), func=AF.Copy),
                 reads=[], writes=[R_ps[pb], R_ot[n]])
        P.barrier()
        if DEBUG:
            dd = nc.dram_tensor("d_ot", [128, 4 * T], BF16, kind="ExternalOutput").ap()
            out_toks.append(P.dma("sp", ds_dbg, lambda e, dd=dd: e.dma_start(out=dd, in_=OT[:, :, :].rearrange("p a b -> p (a b)"))))

        WOUT = view(OFF_B + 16 * K, [128, 8, D], BF16)
        HACC = view(OFF_A, [128, NTC, D], F32)
        XN2 = view(OFF_C, [128, NTC, D], BF16)
        NWBC2 = view(OFF_D, [128, D], F32)
        XN2F = view(OFF_D + 4 * K, [128, D], F32)
        XN2T = view(OFF_D + 8 * K, [128, 8, 128], F32)
        R_wout = R()
        R_hacc = [R() for _ in range(NTC)]
        R_xn2 = [R() for _ in range(NTC)]
        R_nw2 = R()
        R_xn2f = R()
        R_xn2t = R()
        P.dma("pool", ds_w[2], lambda e: e.dma_start(out=WOUT[:, :, :], in_=wout_d.rearrange("(c p) n -> p c n", p=128)), writes=[R_wout])
        P.dma("sp", ds_c, lambda e: e.dma_start(out=NWBC2[:, :], in_=nw_d[1]), writes=[R_nw2])
        XN2Fs = [XN2F, view(OFF_F + 8 * K, [128, D], F32)]
        XN2Ts = [XN2T, view(OFF_F + 12 * K, [128, 8, 128], F32)]
        R_xn2fs = [R_xn2f, R()]
        R_xn2ts = [R_xn2t, R()]

        scr_toks = []
        R_sma = [R(), R()]
        R_smb = R()
        def s3_a(tc):
            b = tc % 2
            ts_ = slice(tc * 128, (tc + 1) * 128)
            XF, R_xf = XN2Fs[b], R_xn2fs[b]
            XTt, R_xt2 = XN2Ts[b], R_xn2ts[b]
            P.dma("sp", ds_x[b], lambda e: e.dma_start(out=XT[b][:, :], in_=x_d[tc * 128:(tc + 1) * 128, :]), writes=[R_xt[b]])
            for half in range(2):
                pb = 2 * (tc % 2) + half
                for cc in range(8):
                    lh = OT[:, cc, ts_] if cc < 4 else OPT[:, cc - 4, ts_]
                    rr = R_ot[tc] if cc < 4 else R_opt[cc - 4]
                    P.op("pe", lambda e, cc=cc, half=half, pb=pb, lh=lh: e.matmul(psb(pb), lh, WOUT[:, cc, half * 512:(half + 1) * 512], start=(cc == 0), stop=(cc == 7)),
                         reads=[rr, R_wout], writes=[R_ps[pb]])
                P.op("dve", lambda e, half=half, pb=pb: e.tensor_tensor(out=HACC[:, tc, half * 512:(half + 1) * 512], in0=psb(pb), in1=XT[b][:, half * 512:(half + 1) * 512], op=ALU.add),
                     reads=[R_xt[b]], writes=[R_ps[pb], R_hacc[tc]])

        def s3_n(tc):
            if not MOE:
                return
            b = tc % 2
            XF, R_xf = XN2Fs[b], R_xn2fs[b]
            XTt, R_xt2 = XN2Ts[b], R_xn2ts[b]
            sc = 56 + 3 * b
            P.op("act", lambda e: e.activation(out=XF[:, :], in_=HACC[:, tc, :], func=AF.Square, accum_out=small[:, sc:sc + 1]),
                 reads=[R_hacc[tc]], writes=[R_xf, R_sma[b]])
            P.op("act", lambda e: e.activation(out=small[:, sc + 1:sc + 2], in_=small[:, sc:sc + 1], func=AF.Ln, bias=epsc[:, :], scale=1.0 / D), reads=[R_c], writes=[R_sma[b]])
            P.op("act", lambda e: e.activation(out=small[:, sc + 2:sc + 3], in_=small[:, sc + 1:sc + 2], func=AF.Exp, scale=-0.5), reads=[], writes=[R_sma[b]])
            P.op("dve", lambda e: e.scalar_tensor_tensor(out=XF[:, :], in0=HACC[:, tc, :], scalar=small[:, sc + 2:sc + 3], in1=NWBC2[:, :], op0=ALU.mult, op1=ALU.mult),
                 reads=[R_hacc[tc], R_sma[b], R_nw2], writes=[R_xf])
            P.op("pool", lambda e: e.tensor_copy(out=XN2[:, tc, :], in_=XF[:, :]), reads=[R_xf], writes=[R_xn2[tc]])
            scr_toks.append(P.dma("sp", ds_s[b], lambda e: e.dma_start(out=xn2_d[tc * 128:(tc + 1) * 128, :], in_=XN2[:, tc, :]), reads=[R_xn2[tc]]))
            for dc in range(8):
                pb = 4 + dc // 4
                P.op("pe", lambda e, dc=dc, pb=pb: e.transpose(psb(pb)[:, (dc % 4) * 128:(dc % 4 + 1) * 128], XF[:, dc * 128:(dc + 1) * 128], ident[:, :]),
                     reads=[R_xf, R_c], writes=[R_ps[pb]])
            for hb in range(2):
                P.op("act", lambda e, hb=hb: e.activation(out=XTt[:, hb * 4:(hb + 1) * 4, :], in_=psb(4 + hb).rearrange("p (a b) -> p a b", b=128), func=AF.Copy),
                     reads=[], writes=[R_ps[4 + hb], R_xt2])

        def s3_b(tc):
            if not MOE:
                return
            b = tc % 2
            XTt, R_xt2 = XN2Ts[b], R_xn2ts[b]
            LG = psb(6 + b)[:, 0:16]
            for dc in range(8):
                P.op("pe", lambda e, dc=dc: e.matmul(LG, XTt[:, dc, :], rws[:, dc, :], start=(dc == 0), stop=(dc == 7)), reads=[R_xt2, R_c], writes=[R_ps[6 + b]])
            P.op("dve", lambda e: e.tensor_reduce(out=small[:, 52:53], in_=LG, axis=AX.X, op=ALU.max), reads=[], writes=[R_ps[6 + b], R_smb])
            P.op("dve", lambda e: e.tensor_scalar(out=small[:, 53:54], in0=small[:, 52:53], scalar1=-1.0, scalar2=None, op0=ALU.mult), reads=[], writes=[R_smb])
            P.op("act", lambda e: e.activation(out=prob[:, tc, :], in_=LG, func=AF.Exp, bias=small[:, 53:54], scale=1.0, accum_out=small[:, 54:55]),
                 reads=[R_smb], writes=[R_ps[6 + b], R_prob, R_smb])
            P.op("dve", lambda e: e.reciprocal(out=small[:, 55:56], in_=small[:, 54:55]), reads=[], writes=[R_smb])
            P.op("dve", lambda e: e.tensor_scalar(out=prob[:, tc, :], in0=prob[:, tc, :], scalar1=small[:, 55:56], scalar2=None, op0=ALU.mult), reads=[R_smb], writes=[R_prob])

        s3_a(0)
        for tc in range(NTC + 1):
            if tc + 1 < NTC:
                s3_a(tc + 1)
            if tc < NTC:
                precast(3)
                s3_n(tc)
            if tc >= 1:
                s3_b(tc - 1)
        P.barrier()
        if MOE:
            PF = view(OFF_B, [128, T], F32)
            WORK = view(OFF_B + 8 * K, [128, T], F32)
            SELT = view(OFF_B + 16 * K, [128, T], F32)
            M8 = view(OFF_B + 24 * K, [128, 8], F32)
            TH = view(OFF_B + 24 * K + 64, [128, 1], F32)
            R_pf, R_work, R_selt, R_m8, R_th = R(), R(), R(), R(), R()
            slot_off = [OFF_F, OFF_F + 12 * K, OFF_E + 4 * K, OFF_B + 20 * K]
            NSLOT = len(slot_off)
            WGs = [view(o_, [128, 8, 256], BF16) for o_ in slot_off]
            WUs = [view(o_ + 4 * K, [128, 8, 256], BF16) for o_ in slot_off]
            WDs = [view(o_ + 8 * K, [128, 2, D], BF16) for o_ in slot_off]
            R_wg = [R() for _ in range(NSLOT)]
            R_wu = [R() for _ in range(NSLOT)]
            R_wd = [R() for _ in range(NSLOT)]

            def load_piece(pi):
                e_, pc = pi // 8, pi % 8
                sl_ = pi % NSLOT
                f0 = pc * 256
                if e_ >= E - NPC:
                    i_ = e_ - (E - NPC)
                    if pc == 0:
                        precast(10 ** 6)
                        P.wait_all("pool", pc_toks[i_])
                    P.dma("pool", ds_w[3 * sl_], lambda e, i_=i_, sl_=sl_, f0=f0: e.dma_start(out=WGs[sl_][:, :, :], in_=wgb_d[i_].rearrange("(c p) f -> p c f", p=128)[:, :, f0:f0 + 256]),
                          writes=[R_wg[sl_]])
                    P.dma("pool", ds_w[3 * sl_ + 1], lambda e, i_=i_, sl_=sl_, f0=f0: e.dma_start(out=WUs[sl_][:, :, :], in_=wub_d[i_].rearrange("(c p) f -> p c f", p=128)[:, :, f0:f0 + 256]),
                          writes=[R_wu[sl_]])
                    P.dma("pool", ds_w[3 * sl_ + 2], lambda e, i_=i_, sl_=sl_, f0=f0: e.dma_start(out=WDs[sl_][:, :, :], in_=wdb_d[i_, f0:f0 + 256, :].rearrange("(c p) d -> p c d", p=128)),
                          writes=[R_wd[sl_]])
                    return
                P.dma("pool", ds_w[3 * sl_], lambda e, e_=e_, sl_=sl_, f0=f0: e.dma_start(out=WGs[sl_][:, :, :], in_=wg_d[e_].rearrange("(c p) f -> p c f", p=128)[:, :, f0:f0 + 256]),
                      writes=[R_wg[sl_]])
                P.dma("pool", ds_w[3 * sl_ + 1], lambda e, e_=e_, sl_=sl_, f0=f0: e.dma_start(out=WUs[sl_][:, :, :], in_=wu_d[e_].rearrange("(c p) f -> p c f", p=128)[:, :, f0:f0 + 256]),
                      writes=[R_wu[sl_]])
                P.dma("pool", ds_w[3 * sl_ + 2], lambda e, e_=e_, sl_=sl_, f0=f0: e.dma_start(out=WDs[sl_][:, :, :], in_=wd_d[e_, f0:f0 + 256, :].rearrange("(c p) d -> p c d", p=128)),
                      writes=[R_wd[sl_]])

            for pi in range(3):
                load_piece(pi)

            for tc in range(NTC):
                b = tc // 4
                P.op("pe", lambda e, tc=tc, b=b: e.transpose(psb(b)[0:16, (tc % 4) * 128:(tc % 4 + 1) * 128], prob[:, tc, :], ident[:, :]),
                     reads=[R_prob, R_c], writes=[R_ps[b]])
            for b in range(4):
                P.op("act", lambda e, b=b: e.activation(out=PF[0:16, b * 512:(b + 1) * 512], in_=psb(b)[0:16, :], func=AF.Copy), reads=[], writes=[R_ps[b], R_pf])
                P.op("dve", lambda e, b=b: e.tensor_copy(out=WORK[0:16, b * 512:(b + 1) * 512], in_=psb(b)[0:16, :]), reads=[], writes=[R_ps[b], R_work])
            for rnd in range(CAP // 8):
                P.op("dve", lambda e: e.max(out=M8[0:16, :], in_=WORK[0:16, :]), reads=[R_work], writes=[R_m8])
                if rnd < CAP // 8 - 1:
                    P.op("dve", lambda e: e.match_replace(out=WORK[0:16, :], in_to_replace=M8[0:16, :], in_values=WORK[0:16, :], imm_value=-1.0),
                         reads=[R_m8], writes=[R_work])
            P.op("dve", lambda e: e.tensor_reduce(out=TH[0:16, :], in_=M8[0:16, :], axis=AX.X, op=ALU.min), reads=[R_m8], writes=[R_th])
            P.op("dve", lambda e: e.tensor_scalar(out=SELT[0:16, :], in0=PF[0:16, :], scalar1=TH[0:16, 0:1], scalar2=None, op0=ALU.is_ge),
                 reads=[R_pf, R_th], writes=[R_selt])
            SV = psb(4)[:, 0:256].rearrange("p (a b) -> p a b", b=16)
            for tc in range(NTC):
                P.op("pe", lambda e, tc=tc: e.transpose(SV[:, tc, :], SELT[0:16, tc * 128:(tc + 1) * 128], ident[0:16, 0:16]),
                     reads=[R_selt, R_c], writes=[R_ps[4]])
            P.op("act", lambda e: e.activation(out=sel[:, :, :], in_=SV, func=AF.Copy), reads=[], writes=[R_ps[4], R_sel])
            P.op("dve", lambda e: e.tensor_copy(out=selb[:, :, :], in_=SV), reads=[], writes=[R_ps[4], R_sel])
            PV = psb(5)[:, 0:256].rearrange("p (a b) -> p a b", b=16)
            for tc in range(NTC):
                mms = [(onesb, t2) for t2 in range(tc)] + [(ltb, tc)]
                for i_, (lh, t2) in enumerate(mms):
                    P.op("pe", lambda e, tc=tc, lh=lh, t2=t2, i_=i_, nmm=len(mms): e.matmul(PV[:, tc, :], lh[:, :], selb[:, t2, :], start=(i_ == 0), stop=(i_ == nmm - 1)),
                         reads=[R_sel, R_c], writes=[R_ps[5]])
            P.op("dve", lambda e: e.scalar_tensor_tensor(out=posm[:, :, :], in0=PV, scalar=1.0, in1=sel[:, :, :], op0=ALU.add, op1=ALU.mult),
                 reads=[R_sel], writes=[R_ps[5], R_posm])
            P.op("dve", lambda e: e.tensor_scalar(out=posm[:, :, :], in0=posm[:, :, :], scalar1=-1.0, scalar2=None, op0=ALU.add), reads=[], writes=[R_posm])
            P.barrier()
            if DEBUG:
                for name, tl in (("d_prob", prob), ("d_sel", sel), ("d_posm", posm)):
                    dd = nc.dram_tensor(name, [128, 256], F32, kind="ExternalOutput").ap()
                    out_toks.append(P.dma("sp", ds_dbg, lambda e, dd=dd, tl=tl: e.dma_start(out=dd, in_=tl[:, :, :].rearrange("p a b -> p (a b)"))))

            load_piece(3)
            PE_ = view(OFF_B, [128, NTC, 256], BF16)
            PTE = view(OFF_B + 8 * K, [128, 2, T], BF16)
            XG = view(OFF_D, [128, 8, 256], BF16)
            HTT = view(OFF_D + 4 * K, [128, 16, 256], BF16)
            SGs = [view(OFF_D + 12 * K + i * K, [128, 256], F32) for i in range(2)]
            TMPS = [view(OFF_B + 16 * K + i * 2 * K, [128, 512], F32) for i in range(2)]
            R_tmps = [R(), R()]
            YG = view(OFF_E, [128, 2, D], BF16)
            R_pe, R_pte, R_xg, R_yg = R(), R(), R(), R()
            XGT = view(OFF_C, [128, 2, D], BF16)
            IDXF = view(OFF_C + 4 * K, [128, 2], F32)
            IDXU = view(OFF_C + 4 * K + 64, [128, 2], U32)
            IDX4 = view(OFF_C + 4 * K + 128, [128, 4], F32)
            R_xgt, R_idx = [R(), R()], R()
            R_scr = R()
            R_scr.w = scr_toks[-1] if False else None
            R_sgs = [R(), R()]
            R_htt = [R() for _ in range(16)]
            next_piece = [NSLOT]

            def build_p1(e_, tc):
                P.op("dve", lambda e, tc=tc, e_=e_: e.tensor_scalar(out=PE_[:, tc, :], in0=iota[:, :], scalar1=posm[:, tc, e_:e_ + 1], scalar2=None, op0=ALU.is_equal),
                     reads=[R_posm, R_c], writes=[R_pe])

            def build_p(e_):
                for tc in range(NTC):
                    build_p1(e_, tc)

            def gather_idx():
                IV = psb(7)[:, 0:4]
                for jc in range(2):
                    for tc in range(NTC):
                        P.op("pe", lambda e, jc=jc, tc=tc: e.matmul(IV[:, jc * 2:jc * 2 + 2], PE_[:, tc, jc * 128:(jc + 1) * 128], tokb[:, tc, :], start=(tc == 0), stop=(tc == NTC - 1)),
                             reads=[R_pe, R_c], writes=[R_ps[7]])
                P.op("dve", lambda e: e.tensor_copy(out=IDX4[:, :], in_=IV), reads=[], writes=[R_ps[7], R_idx])
                IV3 = IDX4[:, :].rearrange("p (j k) -> p j k", k=2)
                P.op("dve", lambda e: e.scalar_tensor_tensor(out=IDXF[:, :], in0=IV3[:, :, 0], scalar=128.0, in1=IV3[:, :, 1], op0=ALU.mult, op1=ALU.add),
                     reads=[], writes=[R_idx])
                P.op("dve", lambda e: e.tensor_copy(out=IDXU[:, :], in_=IDXF[:, :]), reads=[], writes=[R_idx])
                for jc in range(2):
                    P.dma("pool", ds_g[jc], lambda e, jc=jc: e.indirect_dma_start(out=XGT[:, jc, :], out_offset=None, in_=xn2_d[:, :],
                                                                                   in_offset=bass.IndirectOffsetOnAxis(ap=IDXU[:, jc:jc + 1], axis=0)),
                          reads=[R_idx], writes=[R_xgt[jc]])

            def gather():
                for jc in range(2):
                    bank = 6 + jc
                    for dc in range(8):
                        P.op("pe", lambda e, jc=jc, dc=dc, bank=bank: e.transpose(psb(bank, BF16)[:, dc * 128:(dc + 1) * 128], XGT[:, jc, dc * 128:(dc + 1) * 128], identb[:, :]),
                             reads=[R_xgt[jc], R_c], writes=[R_ps[bank]])
                    P.op("act", lambda e, jc=jc, bank=bank: e.activation(out=XG[:, :, jc * 128:(jc + 1) * 128], in_=psb(bank, BF16)[:, 0:1024].rearrange("p (a b) -> p a b", b=128), func=AF.Copy),
                         reads=[], writes=[R_ps[bank], R_xg])

            def transpose_p():
                for jc in range(2):
                    for th in range(2):
                        bank = 4 + (jc * 2 + th) % 2
                        for t8 in range(8):
                            tc = th * 8 + t8
                            P.op("pe", lambda e, jc=jc, tc=tc, t8=t8, bank=bank: e.transpose(psb(bank, BF16)[:, t8 * 128:(t8 + 1) * 128], PE_[:, tc, jc * 128:(jc + 1) * 128], identb[:, :]),
                                 reads=[R_pe, R_c], writes=[R_ps[bank]])
                        P.op("act", lambda e, jc=jc, th=th, bank=bank: e.activation(out=PTE[:, jc, th * 1024:(th + 1) * 1024], in_=psb(bank, BF16)[:, 0:1024], func=AF.Copy),
                             reads=[], writes=[R_ps[bank], R_pte])

            def down(e_, fc):
                sl_ = (e_ * 8 + fc // 2) % NSLOT
                sub = fc % 2
                for jc in range(2):
                    for half in range(2):
                        yb = jc * 2 + half
                        P.op("pe", lambda e, fc=fc, jc=jc, half=half, yb=yb, sl_=sl_, sub=sub: e.matmul(psb(yb), HTT[:, fc, jc * 128:(jc + 1) * 128], WDs[sl_][:, sub, half * 512:(half + 1) * 512],
                                                                                                    start=(fc == 0), stop=(fc == 15)),
                             reads=[R_htt[fc], R_wd[sl_]], writes=[R_ps[yb]])
                if sub == 1:
                    if next_piece[0] < E * 8:
                        load_piece(next_piece[0])
                        next_piece[0] += 1

            def ffn(e_):
                for fc in range(16):
                    sl_ = (e_ * 8 + fc // 2) % NSLOT
                    sub = fc % 2
                    hb = 4 + fc % 2
                    for wi_, (W, RW) in enumerate(((WGs, R_wg), (WUs, R_wu))):
                        for dc in range(8):
                            P.op("pe", lambda e, wi_=wi_, W=W, dc=dc, hb=hb, sl_=sl_, sub=sub: e.matmul(psb(hb)[:, wi_ * 256:(wi_ + 1) * 256], W[sl_][:, dc, sub * 128:(sub + 1) * 128], XG[:, dc, :],
                                                                                                    start=(dc == 0), stop=(dc == 7)),
                                 reads=[RW[sl_], R_xg], writes=[R_ps[hb]])
                    sgi = fc % 2
                    P.op("act", lambda e, hb=hb, sgi=sgi: e.activation(out=SGs[sgi][:, :], in_=psb(hb)[:, 0:256], func=AF.Silu), reads=[], writes=[R_ps[hb], R_sgs[sgi]])
                    P.op("dve", lambda e, hb=hb, fc=fc, sgi=sgi: e.tensor_tensor(out=HTT[:, fc, :], in0=SGs[sgi][:, :], in1=psb(hb)[:, 256:512], op=ALU.mult),
                         reads=[R_sgs[sgi]], writes=[R_ps[hb], R_htt[fc]])
                    if e_ + 1 < E:
                        if fc < 8:
                            build_p1(e_ + 1, 2 * fc)
                            build_p1(e_ + 1, 2 * fc + 1)
                        if fc == 9:
                            gather_idx()
                    if fc >= 2:
                        down(e_, fc - 2)
                down(e_, 14)
                down(e_, 15)
                for jc in range(2):
                    for half in range(2):
                        yb = jc * 2 + half
                        P.op("act", lambda e, jc=jc, half=half, yb=yb: e.activation(out=YG[:, jc, half * 512:(half + 1) * 512], in_=psb(yb), func=AF.Copy),
                             reads=[], writes=[R_ps[yb], R_yg])

            def scatter(e_):
                sbanks = (6, 7, 0, 1, 2, 3)
                for tc in range(NTC):
                    for half in range(2):
                        i_ = tc * 2 + half
                        bank = sbanks[i_ % len(sbanks)]
                        for jc in range(2):
                            P.op("pe", lambda e, tc=tc, half=half, jc=jc, bank=bank: e.matmul(psb(bank), PTE[:, jc, tc * 128:(tc + 1) * 128], YG[:, jc, half * 512:(half + 1) * 512],
                                                                                            start=(jc == 0), stop=(jc == 1)),
                                 reads=[R_pte, R_yg], writes=[R_ps[bank]])
                        if i_ % 2 == 0:
                            P.op("dve", lambda e, tc=tc, half=half, bank=bank, e_=e_: e.scalar_tensor_tensor(out=HACC[:, tc, half * 512:(half + 1) * 512], in0=psb(bank), scalar=prob[:, tc, e_:e_ + 1],
                                                                                                         in1=HACC[:, tc, half * 512:(half + 1) * 512], op0=ALU.mult, op1=ALU.add),
                                 reads=[R_prob], writes=[R_ps[bank], R_hacc[tc]])
                        else:
                            ti = (i_ // 2) % 2
                            P.op("act", lambda e, tc=tc, bank=bank, e_=e_, ti=ti: e.activation(out=TMPS[ti][:, :], in_=psb(bank), func=AF.Identity, scale=prob[:, tc, e_:e_ + 1]),
                                 reads=[R_prob], writes=[R_ps[bank], R_tmps[ti]])
                            P.op("pool", lambda e, tc=tc, half=half, ti=ti: e.tensor_tensor(out=HACC[:, tc, half * 512:(half + 1) * 512], in0=HACC[:, tc, half * 512:(half + 1) * 512],
                                                                                        in1=TMPS[ti][:, :], op=ALU.add),
                                 reads=[R_tmps[ti]], writes=[R_hacc[tc]])

            P.wait_all("pool", scr_toks)
            build_p(0)
            gather_idx()
            gather()
            for e_ in range(E):
                transpose_p()
                ffn(e_)
                scatter(e_)
                if e_ + 1 < E:
                    gather()
            P.barrier()
        NWF = view(OFF_D, [128, D], F32)
        OTL = [view(OFF_E + i * 4 * K, [128, D], F32) for i in range(2)]
        R_nwf = R()
        R_otl = [R(), R()]
        P.dma("sp", ds_c, lambda e: e.dma_start(out=NWF[:, :], in_=nw_d[2]), writes=[R_nwf])
        for tc in range(NTC):
            ob = tc % 2
            P.op("act", lambda e, ob=ob, tc=tc: e.activation(out=OTL[ob][:, :], in_=HACC[:, tc, :], func=AF.Square, accum_out=small[:, 8:9]),
                 reads=[R_hacc[tc]], writes=[R_otl[ob], R_small])
            P.op("act", lambda e: e.activation(out=small[:, 9:10], in_=small[:, 8:9], func=AF.Ln, bias=epsc[:, :], scale=1.0 / D),
                 reads=[R_c], writes=[R_small])
            P.op("act", lambda e: e.activation(out=small[:, 10:11], in_=small[:, 9:10], func=AF.Exp, scale=-0.5), reads=[], writes=[R_small])
            P.op("dve", lambda e, ob=ob, tc=tc: e.scalar_tensor_tensor(out=OTL[ob][:, :], in0=HACC[:, tc, :], scalar=small[:, 10:11], in1=NWF[:, :], op0=ALU.mult, op1=ALU.mult),
                 reads=[R_hacc[tc], R_small, R_nwf], writes=[R_otl[ob]])
            out_toks.append(P.dma("sp", ds_o[ob], lambda e, tc=tc, ob=ob: e.dma_start(out=out_d[tc * 128:(tc + 1) * 128, :], in_=OTL[ob][:, :]), reads=[R_otl[ob]]))
        P.wait_all("sp", out_toks)

        with nc.Block() as block:
            @block.tensor
            def _(e):
                for f in P.q["pe"]:
                    f(e)

            @block.scalar
            def _(e):
                for f in P.q["act"]:
                    f(e)

            @block.vector
            def _(e):
                for f in P.q["dve"]:
                    f(e)

            @block.gpsimd
            def _(e):
                for f in P.q["pool"]:
                    f(e)

            @block.sync
            def _(e):
                for f in P.q["sp"]:
                    f(e)
    return nc


DEBUG = False
MOE = True


def make_in_maps(inputs):
    c = host_consts()
    f = lambda a: np.ascontiguousarray(np.asarray(a, dtype=np.float32))
    x = f(inputs["x"])
    shared = {
        "w_in": f(inputs["w_in"][0]),
        "conv_wP": f(np.asarray(inputs["conv_w"][0]).T.reshape(12, 128, 5).transpose(1, 0, 2).reshape(128, 60)),
        "abp": f(np.tile(np.concatenate([np.asarray(inputs["a_log_fwd"][0]), np.asarray(inputs["a_log_bwd"][0]),
                                         np.asarray(inputs["dt_bias_fwd"][0]), np.asarray(inputs["dt_bias_bwd"][0])])[None, :], (128, 1))),
        "nw0": f(np.tile(np.asarray(inputs["norm_mix_w"][0])[None, :], (128, 1))),
        "nw1": f(np.tile(np.asarray(inputs["norm_ffn_w"][0])[None, :], (128, 1))),
        "nw2": f(np.tile(np.asarray(inputs["norm_final_w"])[None, :], (128, 1))),
        "hnw": f(np.tile(np.asarray(inputs["head_norm_w"][0])[None, :], (128, 1))),
        "pool_wP": f(np.asarray(inputs["pool_w"][0]).transpose(1, 0, 2).reshape(128, 512)),
        "pool_scT": f(np.asarray(inputs["pool_scale"][0]).reshape(4, 128).T),
        "w_out": f(inputs["w_out"][0]),
        "router_wP": f(np.asarray(inputs["router_w"][0]).reshape(8, 128, 16).transpose(1, 0, 2).reshape(128, 128)),
        "wg": f(inputs["expert_w_gate"][0]),
        "wu": f(inputs["expert_w_up"][0]),
        "wd": f(inputs["expert_w_down"][0]),
        "c_iota": c["iota"],
        "c_tok": c["tok"],
        "c_invc": c["invc"],
    }
    for n in CONST_NAMES:
        shared["c_" + n] = c[n]
    maps = []
    for b in range(8):
        m = dict(shared)
        m["x"] = np.ascontiguousarray(x[b])
        maps.append(m)
    return maps


def kernel(**inputs):
    nc = build_nc()
    in_maps = make_in_maps(inputs)
    res = run_bass_kernel_spmd(nc, in_maps, core_ids=list(range(8)))
    out = np.stack([np.asarray(r["out"], dtype=np.float32) for r in res.results], axis=0)
    return out
```
